# Optimizing a Trainium2 kernel written in Bass

```python
import math
import jax, jax.numpy as jnp
from jax import lax
import numpy as np

D_MODEL = 1024
BATCH = 16
SEQ = 256
DEPTH = 1
DEC_BATCH = 2
DEC_SEQ = 1024
PAST_LEN = 256

GRID_W = 64
D_INNER = 2 * D_MODEL
HEAD_DIM = 64
N_HEADS = D_INNER // HEAD_DIM
N_GROUPS = 8
HEADS_PER_GROUP = N_HEADS // N_GROUPS
D_STATE = 128
SSM_CONV = 5
CHUNK = 128
D_XBC = D_INNER + 2 * N_GROUPS * D_STATE
D_CONF = D_MODEL
CONF_KERNEL = 31
N_EXPERTS = 32
TOP_K = 4
D_EXPERT = D_MODEL
SWIGLU_LIMIT = 7.0
SWIGLU_ALPHA = 1.702
D_IN_PROJ = D_INNER + D_XBC + 2 * N_HEADS + 2 * D_CONF + 2 * D_MODEL
EPS = 1e-6

kernel_name = "hybrid_ssd_conformer_moe_diffusion_step"


def _rmsnorm(x, w):
    xf = x.astype(jnp.float32)
    xf = xf * lax.rsqrt(jnp.mean(xf * xf, axis=-1, keepdims=True) + EPS)
    return xf.astype(x.dtype) * w


def _layernorm(x, w, b):
    xf = x.astype(jnp.float32)
    mu = jnp.mean(xf, axis=-1, keepdims=True)
    xc = xf - mu
    var = jnp.mean(xc * xc, axis=-1, keepdims=True)
    return (xc * lax.rsqrt(var + EPS)).astype(x.dtype) * w + b


def _dwconv(x, w, b):
    k = w.shape[0]
    y = lax.conv_general_dilated(
        x, w[:, None, :].astype(x.dtype), window_strides=(1,), padding=[(k // 2, k // 2)],
        dimension_numbers=("NWC", "WIO", "NWC"), feature_group_count=x.shape[-1])
    return y + b


def _segsum(a):
    t = a.shape[-1]
    a_rep = jnp.broadcast_to(a[..., :, None], a.shape + (t,))
    a_rep = jnp.where(jnp.tril(jnp.ones((t, t), bool), -1), a_rep, 0.0)
    ss = jnp.cumsum(a_rep, axis=-2)
    return jnp.where(jnp.tril(jnp.ones((t, t), bool)), ss, -jnp.inf)


def _ssd_scan(x, dt, a, bm, cm, h0):
    bsz, seq = x.shape[:2]
    nc = seq // CHUNK
    g, r = N_GROUPS, HEADS_PER_GROUP
    xs = (x * dt[..., None]).reshape(bsz, nc, CHUNK, g, r, HEAD_DIM)
    bs = bm.reshape(bsz, nc, CHUNK, g, D_STATE)
    cs = cm.reshape(bsz, nc, CHUNK, g, D_STATE)
    adt = (dt * a).reshape(bsz, nc, CHUNK, g, r).transpose(0, 3, 4, 1, 2)
    a_cum = jnp.cumsum(adt, axis=-1)
    lmat = jnp.exp(_segsum(adt))
    scores = jnp.einsum("bclgn,bcsgn->bgcls", cs, bs)
    y_diag = jnp.einsum("bgrcls,bcsgrp->bclgrp", scores[:, :, None] * lmat, xs)
    decay_states = jnp.exp(a_cum[..., -1:] - a_cum)
    states = jnp.einsum("bcsgn,bgrcs,bcsgrp->bcgrpn", bs, decay_states, xs)
    h0r = h0.reshape(bsz, g, r, HEAD_DIM, D_STATE)
    states = jnp.concatenate([h0r[:, None], states], axis=1)
    chunk_tot = jnp.pad(a_cum[..., -1], ((0, 0), (0, 0), (0, 0), (1, 0)))
    decay_chunk = jnp.exp(_segsum(chunk_tot))
    new_states = jnp.einsum("bgrzc,bcgrpn->bzgrpn", decay_chunk, states)
    prev_states, h_final = new_states[:, :-1], new_states[:, -1]
    y_off = jnp.einsum("bclgn,bcgrpn,bgrcl->bclgrp", cs, prev_states, jnp.exp(a_cum))
    y = (y_diag + y_off).reshape(bsz, seq, N_HEADS, HEAD_DIM)
    return y, h_final.reshape(bsz, N_HEADS, HEAD_DIM, D_STATE)


def _mixer(h, h0, grid_rows, lp):
    bsz, seq, _ = h.shape
    f32 = jnp.float32
    proj = h @ lp["w_in"]
    z, xbc, dt_raw, glu_in, gate_raw = jnp.split(
        proj, (D_INNER, D_INNER + D_XBC, D_INNER + D_XBC + 2 * N_HEADS,
               D_INNER + D_XBC + 2 * N_HEADS + 2 * D_CONF), axis=-1)
    xbc = jax.nn.silu(_dwconv(xbc, lp["ssm_conv_w"], lp["ssm_conv_b"]))
    xs, bm, cm = jnp.split(xbc, (D_INNER, D_INNER + N_GROUPS * D_STATE), axis=-1)
    xs = xs.reshape(bsz, seq, N_HEADS, HEAD_DIM).astype(f32)
    bm = bm.reshape(bsz, seq, N_GROUPS, D_STATE).astype(f32)
    cm = cm.reshape(bsz, seq, N_GROUPS, D_STATE).astype(f32)
    dt = jax.nn.softplus(dt_raw.reshape(bsz, seq, 2, N_HEADS).astype(f32) + lp["dt_bias"].astype(f32))
    a = -jnp.exp(lp["a_log"].astype(f32))
    y_f, h_f = _ssd_scan(xs, dt[:, :, 0], a[0], bm, cm, h0[:, 0])
    y_b, h_b = _ssd_scan(jnp.flip(xs, 1), jnp.flip(dt[:, :, 1], 1), a[1],
                         jnp.flip(bm, 1), jnp.flip(cm, 1), h0[:, 1])
    y = y_f + jnp.flip(y_b, 1) + xs * lp["d_skip"].astype(f32)[:, None]
    y = y.reshape(bsz, seq, D_INNER).astype(h.dtype) * jax.nn.silu(z)
    y = _rmsnorm(y.reshape(bsz, seq, N_GROUPS, D_INNER // N_GROUPS),
                 lp["ssm_norm_w"].reshape(N_GROUPS, D_INNER // N_GROUPS)).reshape(bsz, seq, D_INNER)
    o_ssd = y @ lp["w_ssd_out"]
    a_glu, b_glu = jnp.split(glu_in, 2, axis=-1)
    u = a_glu * jax.nn.sigmoid(b_glu)
    if grid_rows is None:
        u = _dwconv(u, lp["conf_dw_w"], lp["conf_dw_b"])
    else:
        u = _dwconv(u.reshape(bsz * grid_rows, GRID_W, D_CONF), lp["conf_dw_w"],
                    lp["conf_dw_b"]).reshape(bsz, seq, D_CONF)
    u = jax.nn.silu(_layernorm(u, lp["conf_ln_w"], lp["conf_ln_b"]))
    o_conf = u @ lp["w_conf_out"] + lp["b_conf_out"]
    g_ssd, g_conf = jnp.split(jax.nn.sigmoid(gate_raw + lp["b_gate"]), 2, axis=-1)
    out = (g_ssd * o_ssd + g_conf * o_conf) @ lp["w_o"]
    return out, jnp.stack([h_f, h_b], axis=1)


def _moe(h, lp):
    bsz, seq, d = h.shape
    t = h.reshape(bsz * seq, d)
    logits = (t @ lp["w_router"] + lp["b_router"]).astype(jnp.float32)
    top_vals, top_idx = lax.top_k(logits, TOP_K)
    probs = jax.nn.softmax(top_vals, axis=-1)
    combine = jnp.sum(jax.nn.one_hot(top_idx, N_EXPERTS, dtype=jnp.float32) * probs[..., None], axis=1)

    def expert_step(acc, e):
        w_gu_e, b_gu_e, w_down_e, b_down_e, w_tok = e
        g, u = jnp.split(t @ w_gu_e + b_gu_e, 2, axis=-1)
        g = jnp.minimum(g, SWIGLU_LIMIT)
        u = jnp.clip(u, -SWIGLU_LIMIT, SWIGLU_LIMIT)
        act = (u + 1.0) * g * jax.nn.sigmoid(SWIGLU_ALPHA * g)
        out = act @ w_down_e + b_down_e
        return acc + w_tok[:, None].astype(out.dtype) * out, None

    acc, _ = lax.scan(expert_step, jnp.zeros_like(t),
                      (lp["w_gu"], lp["b_gu"], lp["w_down"], lp["b_down"], combine.T))
    return acc.reshape(bsz, seq, d)


def _block(x, mod, h0, grid_rows, lp):
    sh1, sc1, g1, sh2, sc2, g2 = jnp.split(mod, 6, axis=-1)
    h = _rmsnorm(x, lp["norm_mix"]) * (1.0 + sc1) + sh1
    o, h_fin = _mixer(h, h0, grid_rows, lp)
    x = x + g1 * o
    h = _rmsnorm(x, lp["norm_ffn"]) * (1.0 + sc2) + sh2
    x = x + g2 * _moe(h, lp)
    return x, h_fin


def setup_inputs(seed: int = 0) -> dict:
    key = jax.random.key(seed)
    ks = jax.random.split(key, 40)
    f32 = jnp.float32

    def nrm(k, shape, scale):
        return jax.random.normal(k, shape, f32) * scale

    dt0 = jnp.exp(jax.random.uniform(ks[10], (DEPTH, 2, N_HEADS), f32,
                                     minval=math.log(1e-3), maxval=math.log(1e-1)))
    return {
        "x_prompt": nrm(ks[0], (BATCH, SEQ, D_MODEL), 1.0),
        "x_sample": nrm(ks[1], (DEC_BATCH, DEC_SEQ, D_MODEL), 1.0),
        "state_ssm": nrm(ks[2], (DEC_BATCH, DEPTH, 2, N_HEADS, HEAD_DIM, D_STATE), 0.1),
        "c": nrm(ks[3], (DEC_BATCH, D_MODEL), 1.0),
        "c_ctx": nrm(ks[4], (D_MODEL,), 1.0),
        "w_ada": nrm(ks[5], (DEPTH, D_MODEL, 6 * D_MODEL), 0.5 * D_MODEL ** -0.5),
        "b_ada": nrm(ks[6], (DEPTH, 6 * D_MODEL), 0.02),
        "norm_mix": 1.0 + nrm(ks[7], (DEPTH, D_MODEL), 0.02),
        "norm_ffn": 1.0 + nrm(ks[8], (DEPTH, D_MODEL), 0.02),
        "w_in": nrm(ks[9], (DEPTH, D_MODEL, D_IN_PROJ), D_MODEL ** -0.5),
        "ssm_conv_w": nrm(ks[11], (DEPTH, SSM_CONV, D_XBC), SSM_CONV ** -0.5),
        "ssm_conv_b": nrm(ks[12], (DEPTH, D_XBC), 0.02),
        "dt_bias": dt0 + jnp.log(-jnp.expm1(-dt0)),
        "a_log": jnp.log(jax.random.uniform(ks[13], (DEPTH, 2, N_HEADS), f32, minval=1.0, maxval=16.0)),
        "d_skip": 1.0 + nrm(ks[14], (DEPTH, N_HEADS), 0.1),
        "ssm_norm_w": 1.0 + nrm(ks[15], (DEPTH, D_INNER), 0.02),
        "w_ssd_out": nrm(ks[16], (DEPTH, D_INNER, D_MODEL), D_INNER ** -0.5),
        "conf_dw_w": nrm(ks[17], (DEPTH, CONF_KERNEL, D_CONF), CONF_KERNEL ** -0.5),
        "conf_dw_b": nrm(ks[18], (DEPTH, D_CONF), 0.02),
        "conf_ln_w": 1.0 + nrm(ks[19], (DEPTH, D_CONF), 0.02),
        "conf_ln_b": nrm(ks[20], (DEPTH, D_CONF), 0.02),
        "w_conf_out": nrm(ks[21], (DEPTH, D_CONF, D_MODEL), D_CONF ** -0.5),
        "b_conf_out": nrm(ks[22], (DEPTH, D_MODEL), 0.02),
        "b_gate": nrm(ks[23], (DEPTH, 2 * D_MODEL), 0.02),
        "w_o": nrm(ks[24], (DEPTH, D_MODEL, D_MODEL), D_MODEL ** -0.5),
        "w_router": nrm(ks[25], (DEPTH, D_MODEL, N_EXPERTS), D_MODEL ** -0.5),
        "b_router": nrm(ks[26], (DEPTH, N_EXPERTS), 0.01),
        "w_gu": nrm(ks[27], (DEPTH, N_EXPERTS, D_MODEL, 2 * D_EXPERT), D_MODEL ** -0.5),
        "b_gu": nrm(ks[28], (DEPTH, N_EXPERTS, 2 * D_EXPERT), 0.02),
        "w_down": nrm(ks[29], (DEPTH, N_EXPERTS, D_EXPERT, D_MODEL), D_EXPERT ** -0.5),
        "b_down": nrm(ks[30], (DEPTH, N_EXPERTS, D_MODEL), 0.02),
        "norm_final": 1.0 + nrm(ks[31], (D_MODEL,), 0.02),
    }


def reference(x_prompt, x_sample, state_ssm, c, c_ctx, w_ada, b_ada, norm_mix, norm_ffn, w_in,
              ssm_conv_w, ssm_conv_b, dt_bias, a_log, d_skip, ssm_norm_w, w_ssd_out,
              conf_dw_w, conf_dw_b, conf_ln_w, conf_ln_b, w_conf_out, b_conf_out, b_gate, w_o,
              w_router, b_router, w_gu, b_gu, w_down, b_down, norm_final):
    n_ctx = x_prompt.shape[0]
    grid_rows = x_sample.shape[1] // GRID_W
    xc, xl = x_prompt, x_sample
    new_states = []
    for l in range(DEPTH):
        lp = {
            "norm_mix": norm_mix[l], "norm_ffn": norm_ffn[l], "w_in": w_in[l],
            "ssm_conv_w": ssm_conv_w[l], "ssm_conv_b": ssm_conv_b[l], "dt_bias": dt_bias[l],
            "a_log": a_log[l], "d_skip": d_skip[l], "ssm_norm_w": ssm_norm_w[l],
            "w_ssd_out": w_ssd_out[l], "conf_dw_w": conf_dw_w[l], "conf_dw_b": conf_dw_b[l],
            "conf_ln_w": conf_ln_w[l], "conf_ln_b": conf_ln_b[l], "w_conf_out": w_conf_out[l],
            "b_conf_out": b_conf_out[l], "b_gate": b_gate[l], "w_o": w_o[l],
            "w_router": w_router[l], "b_router": b_router[l], "w_gu": w_gu[l], "b_gu": b_gu[l],
            "w_down": w_down[l], "b_down": b_down[l],
        }
        mod_ctx = (jax.nn.silu(c_ctx) @ w_ada[l] + b_ada[l])[None, None, :]
        mod_lat = (jax.nn.silu(c) @ w_ada[l] + b_ada[l])[:, None, :]
        h0_ctx = jnp.zeros((n_ctx, 2, N_HEADS, HEAD_DIM, D_STATE), jnp.float32)
        xc, st_ctx = _block(xc, mod_ctx, h0_ctx, None, lp)
        new_states.append(st_ctx)
        xl, _ = _block(xl, mod_lat, state_ssm[:, l].astype(jnp.float32), grid_rows, lp)
    y_prompt = _rmsnorm(xc, norm_final)
    y_sample = _rmsnorm(xl, norm_final)
    new_state_ssm = jnp.stack(new_states, axis=1)
    return (y_prompt, y_sample, new_state_ssm)
```

```python
import numpy as np
from contextlib import ExitStack
import concourse.bass as bass
import concourse.mybir as mybir
from concourse.bass_utils import run_bass_kernel_spmd

F32, BF16 = mybir.dt.float32, mybir.dt.bfloat16
AF = mybir.ActivationFunctionType
ALU = mybir.AluOpType
AX = mybir.AxisListType

T = 768
A = 1536
EPS = 1e-6
NU = 5
SAME_SYNC = True
N_EXP = 32
DBG = None


def _par_layout():
    off = {}
    n = 0
    def add(name, w):
        nonlocal n
        off[name] = (n, w)
        n += w
    add("norm_mix", 8); add("norm_ffn", 8); add("norm_final", 8); add("b_ada", 48)
    add("conv_w", 32 * 5); add("conv_b", 32); add("ssm_norm_w", 16)
    add("cdw_w", 8 * 31); add("cdw_b", 8); add("ln_w", 8); add("ln_b", 8); add("b_conf_out", 8)
    add("b_gate", 16); add("b_gu", 32 * 16)
    add("dt_bias", 64); add("a_log", 64); add("d_skip", 32); add("b_router", 32)
    return off, n

PAR, NPAR = _par_layout()


def _fm(v, ntile):
    return np.ascontiguousarray(np.asarray(v, np.float32).reshape(ntile, 128).T)


def pack_params(inp):
    P = np.zeros((128, NPAR), np.float32)
    def put(name, arr):
        o, w = PAR[name]
        assert arr.shape == (128, w), (name, arr.shape, w)
        P[:, o:o + w] = arr
    put("norm_mix", _fm(inp["norm_mix"][0], 8)); put("norm_ffn", _fm(inp["norm_ffn"][0], 8))
    put("norm_final", _fm(inp["norm_final"], 8)); put("b_ada", _fm(inp["b_ada"][0], 48))
    cw = np.asarray(inp["ssm_conv_w"][0], np.float32)
    put("conv_w", np.ascontiguousarray(cw.reshape(5, 32, 128).transpose(2, 1, 0).reshape(128, 160)))
    put("conv_b", _fm(inp["ssm_conv_b"][0], 32)); put("ssm_norm_w", _fm(inp["ssm_norm_w"][0], 16))
    dw = np.asarray(inp["conf_dw_w"][0], np.float32)
    put("cdw_w", np.ascontiguousarray(dw.reshape(31, 8, 128).transpose(2, 1, 0).reshape(128, 248)))
    put("cdw_b", _fm(inp["conf_dw_b"][0], 8)); put("ln_w", _fm(inp["conf_ln_w"][0], 8))
    put("ln_b", _fm(inp["conf_ln_b"][0], 8)); put("b_conf_out", _fm(inp["b_conf_out"][0], 8))
    put("b_gate", _fm(inp["b_gate"][0], 16))
    bg = np.asarray(inp["b_gu"][0], np.float32)
    put("b_gu", np.ascontiguousarray(bg.reshape(32, 16, 128).transpose(2, 0, 1).reshape(128, 512)))
    put("dt_bias", np.broadcast_to(np.asarray(inp["dt_bias"][0], np.float32).reshape(1, 64), (128, 64)))
    put("a_log", np.broadcast_to(np.asarray(inp["a_log"][0], np.float32).reshape(1, 64), (128, 64)))
    put("d_skip", np.broadcast_to(np.asarray(inp["d_skip"][0], np.float32).reshape(1, 32), (128, 32)))
    put("b_router", np.broadcast_to(np.asarray(inp["b_router"][0], np.float32).reshape(1, 32), (128, 32)))
    return P


def make_consts():
    k = np.arange(128)[:, None]
    s = np.arange(128)[None, :]
    C = np.zeros((128, 8, 128), np.float32)
    C[:, 0] = (k == s); C[:, 1] = (k <= s); C[:, 2] = (k >= s); C[:, 3] = (k > s); C[:, 4] = (k < s)
    C[:, 5] = 1.0
    C[:, 6] = (s < 64); C[:, 7] = (s >= 64)
    return C.reshape(128, 1024)


class _Trk:
    __slots__ = ("lw", "rd")
    def __init__(self):
        self.lw = None
        self.rd = {}


class Buf:
    def __init__(self, name, share=None):
        self.name = name
        self.t = share.t if share is not None else _Trk()
        self.dsem = None
        self.dn = 0


ENGS = ["pe", "act", "dve", "pool", "sp"]


class Prog:
    def __init__(self):
        self.ops = {e: [] for e in ENGS}

    def op(self, eng, fn, rd=(), wr=(), dma=None):
        deps = set()
        for b in rd:
            if b.t.lw is not None:
                deps.add(b.t.lw)
        for b in wr:
            if b.t.lw is not None:
                deps.add(b.t.lw)
            deps.update(b.t.rd.values())
        idx = len(self.ops[eng])
        if dma is not None:
            dma.dn += 1
            tok = ("dma", dma, dma.dn)
            key = ("dma", id(dma))
        else:
            tok = (eng, idx)
            key = eng
        for b in rd:
            if getattr(b, "is_psum", False) and eng != "pe":
                others = [k for k in b.t.rd if isinstance(k, str) and k not in ("pe", eng)]
                if others:
                    raise AssertionError("PSUM bank %s read by %s and %s between writes" % (b.name, eng, others))
            b.t.rd[key] = tok
        for b in wr:
            b.t.lw = tok
            b.t.rd = {}
        self.ops[eng].append((fn, deps, tok, dma))

    def finalize(self):
        needed = set()
        for e in ENGS:
            for fn, deps, tok, dma in self.ops[e]:
                for d in deps:
                    if d[0] != "dma":
                        if d[0] == e and (e == "pe" or not SAME_SYNC):
                            continue
                        needed.add(d)
        self.needed = needed
        self.sigval = {}
        for e in ENGS:
            c = 0
            for i, (fn, deps, tok, dma) in enumerate(self.ops[e]):
                if tok in needed:
                    c += 1
                    self.sigval[tok] = c

    def emit(self, e, E, sems):
        known = {}
        for fn, deps, tok, dma in self.ops[e]:
            waits = {}
            for d in deps:
                if d[0] == "dma":
                    sem, val = d[1].dsem, 16 * d[2]
                else:
                    if d[0] == e and (e == "pe" or not SAME_SYNC):
                        continue
                    sem, val = sems[d[0]], self.sigval[d]
                k = id(sem)
                if known.get(k, 0) >= val:
                    continue
                if k not in waits or waits[k][1] < val:
                    waits[k] = (sem, val)
            for k, (sem, val) in waits.items():
                E.wait_ge(sem, val)
                known[k] = val
            if fn is None:
                continue
            ins = fn(E)
            if dma is not None:
                ins.then_inc(dma.dsem, 16)
            elif tok in self.needed:
                ins.then_inc(sems[e], 1)


SW_ALPHA = 1.702
SW_LIM = 7.0
BLK = ((0, 512, 0), (512, 256, 1))


def build(dbg=None):
    nc = bass.Bass("TRN2", target_bir_lowering=False)
    P = Prog()
    es = ExitStack()

    def dram(name, shape, kind="ExternalInput", dt=F32):
        return nc.dram_tensor(name, list(shape), dt, kind=kind).ap()

    x_all = dram("x_all", [A, 1024])
    cond_d = dram("cond_fm", [128, 16])
    st0_d = dram("state0", [4096, 128])
    masks_d = dram("masks", [128, 20])
    params_d = dram("params", [128, NPAR])
    consts_d = dram("consts", [128, 1024])
    w_ada_d = dram("w_ada", [1024, 6144])
    w_in_d = dram("w_in", [1024, 10304])
    w_ssd_d = dram("w_ssd_out", [2048, 1024])
    w_conf_d = dram("w_conf_out", [1024, 1024])
    w_o_d = dram("w_o", [1024, 1024])
    w_rt_d = dram("w_router", [128, 256])
    w_gu_d = dram("w_gu", [N_EXP, 1024, 2048])
    w_dn_d = dram("w_down", [N_EXP, 1024, 1024])
    b_dn_d = dram("b_down", [N_EXP, 1024])
    y_out = dram("y_own", [T, 1024], kind="ExternalOutput")
    ns_out = dram("new_state", [2 * 2 * 2048, 128], kind="ExternalOutput")
    dbg_out = dram("dbg", [128, 8192], kind="ExternalOutput") if dbg else None

    def newsem(name):
        return es.enter_context(nc.semaphore(name))

    def sb(name, shape, dt=F32, dma=False):
        t = es.enter_context(nc.sbuf_tensor(name, list(shape), dt))
        b = Buf(name)
        if dma:
            b.dsem = newsem("d_" + name)
        return t, b

    sems = {e: newsem("s_" + e) for e in ENGS}

    def V(eng, method, rd, wr, *args, **kw):
        P.op(eng, lambda E: getattr(E, method)(*args, **kw), rd=rd, wr=wr)

    class Slab:
        def __init__(self, name, words):
            self.name = name
            self.words = words
            self.t = es.enter_context(nc.sbuf_tensor(name, [128, words], F32))
            self.ptr = 0
            self.tiles = []
            self.haz = {}
            self.n = 0

        def reset(self):
            for b in self.tiles:
                if b.t.lw is not None:
                    self.haz[("lw", id(b))] = b.t.lw
                for k, v in b.t.rd.items():
                    if k in self.haz and isinstance(k, str):
                        if self.haz[k][1] < v[1]:
                            self.haz[k] = v
                    else:
                        self.haz[k] = v
            self.tiles = []
            self.ptr = 0

        def carve(self, shape, dt=F32, dma=False):
            per = 1
            for d in shape[1:]:
                per *= d
            words = per if dt == F32 else (per + 1) // 2
            assert self.ptr + words <= self.words, (self.name, self.ptr, words, self.words)
            ap = self.t[:, self.ptr:self.ptr + words]
            self.ptr += words
            if dt != F32:
                ap = ap.bitcast(dt)
            if len(shape) == 3:
                ap = ap.rearrange("p (a b) -> p a b", a=shape[1])
            elif len(shape) == 4:
                ap = ap.rearrange("p (a b c) -> p a b c", a=shape[1], b=shape[2])
            self.n += 1
            b = Buf("%s_%d" % (self.name, self.n))
            b.t.rd = dict(self.haz)
            if dma:
                b.dsem = newsem("d_%s_%d" % (self.name, self.n))
            self.tiles.append(b)
            return ap, b

    cst, cst_b = sb("cst", [128, 8, 128], F32, dma=True)
    cstb, cstb_b = sb("cstb", [128, 8, 128], BF16)
    par, par_b = sb("par", [128, NPAR], F32, dma=True)
    msk, msk_b = sb("msk", [128, 20], F32, dma=True)
    small, small_b = sb("small", [128, 8], F32)
    negm_t, negm_b = sb("negm", [128, 2, 128], BF16)
    negm = negm_t[:]
    NUr = 4
    ring = [sb("ring%d" % i, [128, 8, 1024], BF16, dma=True) for i in range(NUr)]
    ring_i = [0]
    SL0 = Slab("SL0", 6144)
    SL1 = Slab("SL1", 6144)
    SL2 = Slab("SL2", 6144)
    SL3 = Slab("SL3", 6144)
    SLT = Slab("SLT", 7680)

    IDENT, TRI_LE, TRI_GE, TRI_GT, TRI_LT, ONES, HLO, HHI = [cst[:, i, :] for i in range(8)]
    IDENTB = cstb[:, 0, :]
    ONESB = cstb[:, 5, :]
    EPSC = small[:, 0:1]
    ONEC = small[:, 1:2]

    def pc(name, a=0, b=None):
        o, w = PAR[name]
        if b is None:
            b = w
        return par[:, o + a:o + b]

    psum = []
    for i in range(8):
        t = es.enter_context(nc.psum_tensor("ps%d" % i, [128, 512], F32))
        psum.append((t, Buf("ps%d" % i)))
        psum[-1][1].is_psum = True

    def PS(i):
        return psum[i]

    def next_ring():
        i = ring_i[0] % NUr
        ring_i[0] += 1
        return ring[i]

    def load_unit(w2d, c0, ncols, k0=0, nk=8):
        t, b = next_ring()
        src = w2d[k0 * 128:(k0 + nk) * 128, c0:c0 + ncols].rearrange("(kc p) c -> p kc c", p=128)
        P.op("pool", lambda E: E.dma_start(out=t[:, 0:nk, 0:ncols], in_=src), wr=[b], dma=b)
        return t, b

    def load_unit2(w2d, c0, c1, n=512):
        t, b = next_ring()
        for idx, cc in enumerate((c0, c1)):
            src = w2d[:, cc:cc + n].rearrange("(kc p) c -> p kc c", p=128)
            dst = t[:, :, idx * n:(idx + 1) * n]
            if idx == 0:
                P.op("pool", (lambda E, dst=dst, src=src: E.dma_start(out=dst, in_=src)), wr=[b], dma=b)
            else:
                P.op("pool", (lambda E, dst=dst, src=src: E.dma_start(out=dst, in_=src)), dma=b)
                b.t.lw = P.ops["pool"][-1][2]
        return t, b

    def dma_in(tile_ap, b, src, eng="sp"):
        P.op(eng, lambda E: E.dma_start(out=tile_ap, in_=src), wr=[b], dma=b)

    dbg_b = Buf("dbgout")
    if dbg:
        dbg_b.dsem = newsem("d_dbg")
    dbg_col = [0]

    def dump(ap2d, b, n):
        c0 = dbg_col[0]
        dbg_col[0] += n
        dst = dbg_out[:, c0:c0 + n]
        P.op("pool", lambda E: E.dma_start(out=dst, in_=ap2d), rd=[b], dma=dbg_b)
        return c0

    def end_dbg():
        P.op("sp", None, wr=[dbg_b])
        return finish(nc, P, es, sems)

    def rsqrt_from(out_ap, out_b, in_ap, in_b, scale, eng_rd=()):
        V("act", "activation", [in_b, small_b] + list(eng_rd), [out_b], out=out_ap, in_=in_ap, func=AF.Ln,
          scale=scale, bias=EPSC)
        V("act", "activation", [out_b], [out_b], out=out_ap, in_=out_ap, func=AF.Exp, scale=-0.5)

    dma_in(cst[:], cst_b, consts_d.rearrange("p (a b) -> p a b", a=8))
    dma_in(par[:], par_b, params_d)
    dma_in(msk[:], msk_b, masks_d)
    V("dve", "tensor_copy", [cst_b], [cstb_b], out=cstb[:], in_=cst[:])
    V("dve", "memset", [], [small_b], small[:, 0:1], EPS)
    V("dve", "memset", [], [small_b], small[:, 1:2], 1.0)

    cond, cond_b = sb("cond", [128, 8, 2], F32, dma=True)
    scond, scond_b = sb("scond", [128, 8, 2], BF16)
    mod, mod_b = sb("mod", [128, 48, 2], F32)
    modA, modA_b = sb("modA", [128, 2, 8, 2], F32)
    dma_in(cond[:], cond_b, cond_d.rearrange("p (j c) -> p j c", c=2))
    V("act", "activation", [cond_b], [scond_b], out=scond[:], in_=cond[:], func=AF.Silu)
    mod1_b = Buf("mod1")
    modA1_b = Buf("modA1")

    def ada_units(u0, u1, bank, mb_, which_list, mab_):
        pt, pb = PS(bank)
        for u in range(u0, u1):
            ut, ub = load_unit(w_ada_d, u * 1024, 1024)
            for jt in range(8):
                col = (u * 8 + jt) * 2
                for k in range(8):
                    V("pe", "matmul", [ub, scond_b], [pb], pt[:, col:col + 2],
                      lhsT=ut[:, k, jt * 128:(jt + 1) * 128], rhs=scond[:, k, :], start=(k == 0), stop=(k == 7))
        j0, j1 = u0 * 8, u1 * 8
        V("dve", "tensor_tensor", [pb, par_b], [mb_], out=mod[:, j0:j1, :],
          in0=pt[:, 2 * j0:2 * j1].rearrange("p (j c) -> p j c", c=2),
          in1=pc("b_ada", j0, j1).unsqueeze(2).to_broadcast([128, j1 - j0, 2]), op=ALU.add)
        for which, nm, jj0 in which_list:
            V("dve", "tensor_scalar", [mb_], [mab_], out=modA[:, which], in0=mod[:, jj0:jj0 + 8, :],
              scalar1=1.0, scalar2=None, op0=ALU.add)
            V("dve", "tensor_tensor", [mab_, par_b], [mab_], out=modA[:, which], in0=modA[:, which],
              in1=pc(nm).unsqueeze(2).to_broadcast([128, 8, 2]), op=ALU.mult)

    ada_units(0, 2, 0, mod1_b, [(0, "norm_mix", 8)], modA1_b)

    def A1(j, c): return modA[:, 0, j, c:c + 1]
    def S1(j, c): return mod[:, j, c:c + 1]
    def G1(j, c): return mod[:, 16 + j, c:c + 1]
    def A2(j, c): return modA[:, 1, j, c:c + 1]
    def S2(j, c): return mod[:, 24 + j, c:c + 1]
    def G2(j, c): return mod[:, 40 + j, c:c + 1]

    hT, hT_all = SL0.carve([128, 8, A], BF16)
    hTb = [Buf("hT_c%d" % i) for i in range(12)]
    for b_ in hTb:
        SL0.tiles.append(b_)
    xt = [SL2.carve([128, 1024], F32, dma=True) for _ in range(2)]
    xn = [SL2.carve([128, 1024], F32) for _ in range(2)]
    junk, junk_b = SL2.carve([128, 1024], BF16)
    ssq, ssq_b = sb("ssq", [128, 12], F32)
    rstd, rstd_b = sb("rstd", [128, 12], F32)
    V("dve", "memset", [], [ssq_b], ssq[:], 0.0)

    def transpose_x_tile(xnt, xnb, evac):
        for hb in range(2):
            pt, pb = PS(1 + hb)
            for jj in range(4):
                j = hb * 4 + jj
                V("pe", "transpose", [xnb, cst_b], [pb], out=pt[:, jj * 128:(jj + 1) * 128],
                  in_=xnt[:, j * 128:(j + 1) * 128], identity=IDENT)
            for jj in range(4):
                evac(hb * 4 + jj, pt[:, jj * 128:(jj + 1) * 128], pb)

    for i in range(12):
        xtt, xtb = xt[i % 2]
        xnt, xnb = xn[i % 2]
        cnd = 0 if i < 4 else 1
        dma_in(xtt, xtb, x_all[i * 128:(i + 1) * 128, :])
        V("act", "activation", [xtb], [junk_b, ssq_b], out=junk, in_=xtt, func=AF.Square,
          accum_out=ssq[:, i:i + 1])
        rsqrt_from(rstd[:, i:i + 1], rstd_b, ssq[:, i:i + 1], ssq_b, 1.0 / 1024)
        V("act", "activation", [xtb, rstd_b], [xnb], out=xnt, in_=xtt, func=AF.Copy, scale=rstd[:, i:i + 1])

        def evac(j, pap, pb, i=i, cnd=cnd):
            dst = hT[:, j, i * 128:(i + 1) * 128]
            if j < 4:
                V("dve", "tensor_scalar", [pb, modA1_b, mod1_b], [hTb[i]], out=dst, in0=pap,
                  scalar1=A1(j, cnd), scalar2=S1(j, cnd), op0=ALU.mult, op1=ALU.add)
            else:
                V("act", "activation", [pb, modA1_b, mod1_b], [hTb[i]], out=dst, in_=pap, func=AF.Identity,
                  scale=A1(j, cnd), bias=S1(j, cnd))
        transpose_x_tile(xnt, xnb, evac)

    ada_units(2, 6, 7, mod_b, [(1, "norm_ffn", 32)], modA_b)

    if dbg == "hT":
        dump(hT[:, 0, :], hT_all, 1536) if False else None
        for b_ in hTb:
            pass
        P.op("pool", lambda E: E.dma_start(out=dbg_out[:, 0:1536], in_=hT[:, 0, :]), rd=hTb, dma=dbg_b)
        P.op("pool", lambda E: E.dma_start(out=dbg_out[:, 1536:3072], in_=hT[:, 7, :]), rd=hTb, dma=dbg_b)
        P.op("pool", lambda E: E.dma_start(out=dbg_out[:, 3072:3168], in_=mod[:].rearrange("p j c -> p (j c)")),
             rd=[mod_b], dma=dbg_b)
        return end_dbg()

    SL2.reset()
    dtv, dtv_b = SL3.carve([128, 12, 64])
    raw, raw_b = SL3.carve([128, 12, 192])
    Eown, Eown_b = SL3.carve([128, 6, 192])
    wdec, wdec_b = SL3.carve([128, 6, 64])
    wrest, wrest_b = SL3.carve([128, 6, 64])
    lw, lw_b = SL3.carve([128, 6, 64])
    wfin, wfin_b = SL3.carve([128, 4, 32])
    ptot, ptot_b = SL3.carve([128, 64])
    aneg, aneg_b = SL3.carve([128, 64])
    negcs_t, negcs_b = sb("negcs", [128, 6, 64], F32)
    negcs = negcs_t[:]
    cs_hi, cs_hi_b = SL3.carve([128, 6, 64], BF16)
    cs_lo, cs_lo_b = SL3.carve([128, 6, 64], BF16)
    adt, adt_b = SLT.carve([128, 12, 64])
    t64, t64_b = SLT.carve([128, 12, 64])
    lpu, lpu_b = SLT.carve([128, 12, 64])
    lpl, lpl_b = SLT.carve([128, 12, 64])

    dtu, dtu_b = load_unit(w_in_d, 6144, 64)
    for i in range(12):
        pt, pb = PS(3 + (i // 8))
        col = (i % 8) * 64
        for k in range(8):
            V("pe", "matmul", [hTb[i], dtu_b], [pb], pt[:, col:col + 64], lhsT=hT[:, k, i * 128:(i + 1) * 128],
              rhs=dtu[:, k, 0:64], start=(k == 0), stop=(k == 7))
    V("dve", "tensor_tensor", [PS(3)[1], par_b], [dtv_b], out=dtv[:, 0:8, :],
      in0=PS(3)[0][:, 0:512].rearrange("p (i c) -> p i c", c=64),
      in1=pc("dt_bias").unsqueeze(1).to_broadcast([128, 8, 64]), op=ALU.add)
    V("dve", "tensor_tensor", [PS(4)[1], par_b], [dtv_b], out=dtv[:, 8:12, :],
      in0=PS(4)[0][:, 0:256].rearrange("p (i c) -> p i c", c=64),
      in1=pc("dt_bias").unsqueeze(1).to_broadcast([128, 4, 64]), op=ALU.add)
    V("act", "activation", [dtv_b], [t64_b], out=t64, in_=dtv, func=AF.Abs)
    V("act", "activation", [t64_b], [t64_b], out=t64, in_=t64, func=AF.Exp, scale=-1.0)
    V("dve", "tensor_scalar", [t64_b], [lpu_b], out=lpu, in0=t64, scalar1=1.0, scalar2=None, op0=ALU.add)
    V("act", "activation", [lpu_b], [lpl_b], out=lpl, in_=lpu, func=AF.Ln)
    V("dve", "tensor_scalar", [lpu_b], [lpu_b], out=lpu, in0=lpu, scalar1=-1.0, scalar2=1e-30, op0=ALU.add,
      op1=ALU.add)
    V("dve", "reciprocal", [lpu_b], [lpu_b], out=lpu, in_=lpu)
    V("dve", "scalar_tensor_tensor", [lpl_b, lpu_b], [lpl_b], out=lpl, in0=lpl, scalar=1e-30, in1=lpu,
      op0=ALU.add, op1=ALU.mult)
    V("dve", "tensor_tensor", [t64_b, lpl_b], [t64_b], out=t64, in0=t64, in1=lpl, op=ALU.mult)
    V("dve", "scalar_tensor_tensor", [dtv_b, t64_b], [dtv_b], out=dtv, in0=dtv, scalar=0.0, in1=t64,
      op0=ALU.max, op1=ALU.add)
    if dbg == "dt1":
        dump(dtv.rearrange("p a b -> p (a b)"), dtv_b, 768)
        return end_dbg()
    V("dve", "tensor_tensor", [dtv_b, msk_b], [dtv_b], out=dtv[:, 6:12, 0:32], in0=dtv[:, 6:12, 0:32],
      in1=msk[:, 8:14].unsqueeze(2).to_broadcast([128, 6, 32]), op=ALU.mult)
    V("dve", "tensor_tensor", [dtv_b, msk_b], [dtv_b], out=dtv[:, 6:12, 32:64], in0=dtv[:, 6:12, 32:64],
      in1=msk[:, 14:20].unsqueeze(2).to_broadcast([128, 6, 32]), op=ALU.mult)
    V("act", "activation", [par_b], [aneg_b], out=aneg, in_=pc("a_log"), func=AF.Exp)
    V("dve", "scalar_tensor_tensor", [dtv_b, aneg_b], [adt_b], out=adt, in0=dtv, scalar=-1.0,
      in1=aneg.unsqueeze(1).to_broadcast([128, 12, 64]), op0=ALU.mult, op1=ALU.mult)
    adt_hi, adt_hi_b = SLT.carve([128, 12, 64], BF16)
    adt_lo, adt_lo_b = SLT.carve([128, 12, 64], BF16)
    V("dve", "tensor_copy", [adt_b], [adt_hi_b], out=adt_hi, in_=adt)
    V("dve", "tensor_tensor", [adt_b, adt_hi_b], [adt_lo_b], out=adt_lo, in0=adt, in1=adt_hi, op=ALU.subtract)
    if dbg == "dt2":
        dump(adt.rearrange("p a b -> p (a b)"), adt_b, 768)
        dump(adt_lo.rearrange("p a b -> p (a b)"), adt_lo_b, 768)
        return end_dbg()
    for i in range(12):
        pt, pb = PS(5 + i % 2)
        for (c0, c1, tri, a0, a1) in ((0, 32, 1, 0, 32), (32, 64, 2, 32, 64), (64, 96, 3, 0, 32),
                                      (96, 128, 4, 32, 64), (128, 192, 5, 0, 64)):
            V("pe", "matmul", [adt_hi_b, cstb_b], [pb], pt[:, c0:c1], lhsT=cstb[:, tri, :],
              rhs=adt_hi[:, i, a0:a1], start=True, stop=False)
            V("pe", "matmul", [adt_lo_b, cstb_b], [pb], pt[:, c0:c1], lhsT=cstb[:, tri, :],
              rhs=adt_lo[:, i, a0:a1], start=False, stop=True)
        V("dve", "tensor_copy", [pb], [raw_b], out=raw[:, i, :], in_=pt[:, 0:192])
        if i < 6:
            V("act", "activation", [raw_b], [Eown_b], out=Eown[:, i, :], in_=raw[:, i, :], func=AF.Exp)
    if dbg == "dt3":
        dump(raw.rearrange("p a b -> p (a b)"), raw_b, 2304)
        dump(Eown.rearrange("p a b -> p (a b)"), Eown_b, 1152)
        return end_dbg()
    V("dve", "tensor_tensor", [dtv_b, Eown_b], [wdec_b], out=wdec, in0=dtv[:, 0:6, :], in1=Eown[:, :, 64:128],
      op=ALU.mult)
    V("dve", "tensor_scalar", [raw_b], [negcs_b], out=negcs, in0=raw[:, 0:6, 0:64], scalar1=-1.0, scalar2=None,
      op0=ALU.mult)
    V("dve", "tensor_scalar", [cst_b], [negm_b], out=negm, in0=cst[:, 3:5, :], scalar1=-65536.0, scalar2=None,
      op0=ALU.mult)
    V("dve", "tensor_copy", [raw_b], [cs_hi_b], out=cs_hi, in_=raw[:, 0:6, 0:64])
    V("dve", "tensor_tensor", [raw_b, cs_hi_b], [cs_lo_b], out=cs_lo, in0=raw[:, 0:6, 0:64], in1=cs_hi,
      op=ALU.subtract)
    V("dve", "memset", [], [lw_b], lw, 0.0)
    for i in range(10, 5, -1):
        V("dve", "tensor_tensor", [lw_b, raw_b], [lw_b], out=lw[:, i - 6, 0:32], in0=lw[:, i - 5, 0:32],
          in1=raw[:, i + 1, 128:160], op=ALU.add)
    for i in range(7, 12):
        V("dve", "tensor_tensor", [lw_b, raw_b], [lw_b], out=lw[:, i - 6, 32:64], in0=lw[:, i - 7, 32:64],
          in1=raw[:, i - 1, 160:192], op=ALU.add)
    V("dve", "tensor_tensor", [lw_b, raw_b], [ptot_b], out=ptot[:, 0:32], in0=lw[:, 0, 0:32],
      in1=raw[:, 6, 128:160], op=ALU.add)
    V("dve", "tensor_tensor", [lw_b, raw_b], [ptot_b], out=ptot[:, 32:64], in0=lw[:, 5, 32:64],
      in1=raw[:, 11, 160:192], op=ALU.add)
    V("act", "activation", [ptot_b], [ptot_b], out=ptot, in_=ptot, func=AF.Exp)
    V("dve", "tensor_tensor", [lw_b, raw_b], [wrest_b], out=wrest, in0=lw, in1=raw[:, 6:12, 64:128], op=ALU.add)
    V("act", "activation", [wrest_b], [wrest_b], out=wrest, in_=wrest, func=AF.Exp)
    V("dve", "tensor_tensor", [wrest_b, dtv_b], [wrest_b], out=wrest, in0=wrest, in1=dtv[:, 6:12, :], op=ALU.mult)
    for s in range(2):
        c0, c1 = 2 * s, 2 * s + 1
        V("dve", "tensor_tensor", [wdec_b, Eown_b], [wfin_b], out=wfin[:, 2 * s, :], in0=wdec[:, c0, 0:32],
          in1=Eown[:, c1, 128:160], op=ALU.mult)
        V("dve", "tensor_tensor", [wdec_b, Eown_b], [wfin_b], out=wfin[:, 2 * s + 1, :], in0=wdec[:, c1, 32:64],
          in1=Eown[:, c0, 160:192], op=ALU.mult)

    if dbg == "dt":
        dump(dtv.rearrange("p a b -> p (a b)"), dtv_b, 768)
        dump(raw.rearrange("p a b -> p (a b)"), raw_b, 2304)
        dump(wdec.rearrange("p a b -> p (a b)"), wdec_b, 384)
        dump(wrest.rearrange("p a b -> p (a b)"), wrest_b, 384)
        dump(ptot, ptot_b, 64)
        dump(wfin.rearrange("p a b -> p (a b)"), wfin_b, 128)
        return end_dbg()

    yT, yT_b = SL1.carve([128, 16, T], BF16)
    yTb = [Buf("yT_p%d" % p) for p in range(4)]
    for b_ in yTb:
        SL1.tiles.append(b_)
    st0v = st0_d.rearrange("(d g t p) n -> g p d t n", d=2, g=8, t=2, p=128)
    stg_i = [0]
    out_bufs = []

    for p in range(4):
        SL2.reset()
        SLT.reset()
        x_tok, x_tok_b = SL2.carve([128, 6, 512], BF16)
        B_tok, B_tok_b = SL2.carve([128, 6, 256], BF16)
        BT, BT_b = SL2.carve([128, 2, T], BF16)
        CT, CT_b = SL2.carve([128, 2, T], BF16)
        H, H_b = SL2.carve([128, 2, 512], F32)
        ents = [SL2.carve([128, 512], BF16) for _ in range(4)]
        pres = [SLT.carve([128, 1576], BF16) for _ in range(2)]
        fms = [SLT.carve([128, A], BF16) for _ in range(2)]
        pc_i = [0]
        diags = [SLT.carve([128, 5, 128], BF16) for _ in range(2)]
        dg_i = [0]
        x_rest, x_rest_b = SLT.carve([128, 6, 256], BF16)
        B_rest, B_rest_b = SLT.carve([128, 6, 256], BF16)
        xsr = [SLT.carve([128, 256], BF16) for _ in range(4)]
        h0t, h0t_b = SLT.carve([128, 2, 2, 128], F32, dma=True)
        htmp, htmp_b = SLT.carve([128, 512], F32)
        for (pre_, pre_b_) in pres:
            preP_ = pre_[:, 0:520].rearrange("p (s t) -> p s t", s=2)
            V("dve", "memset", [], [pre_b_], preP_[:, :, 0:2], 0.0)
            V("dve", "memset", [], [pre_b_], preP_[:, :, 258:260], 0.0)

        xu = load_unit(w_in_d, 2048 + 512 * p, 512)
        bu = load_unit(w_in_d, 4096 + 256 * p, 256)
        cu = load_unit(w_in_d, 5120 + 256 * p, 256)

        def proj_conv(unit, jt, cidx, sil):
            ut, ub = unit
            pre, pre_b = pres[pc_i[0] % 2]
            fm, fm_b = fms[pc_i[0] % 2]
            pc_i[0] += 1
            preP = pre[:, 0:520].rearrange("p (s t) -> p s t", s=2)
            preS = pre[:, 520:1576].rearrange("p (s t) -> p s t", s=8)
            for tb in range(3):
                pt, pb = PS((0, 1, 5)[tb])
                for k in range(8):
                    V("pe", "matmul", [ub] + hTb[4 * tb:4 * tb + 4], [pb], pt[:, :],
                      lhsT=ut[:, k, jt * 128:(jt + 1) * 128], rhs=hT[:, k, tb * 512:(tb + 1) * 512],
                      start=(k == 0), stop=(k == 7))
                if tb == 0:
                    dst = preP[:, :, 2:258]
                    src = pt[:, :].rearrange("p (s t) -> p s t", s=2)
                else:
                    dst = preS[:, 4 * (tb - 1):4 * tb, 2:130]
                    src = pt[:, :].rearrange("p (s t) -> p s t", s=4)
                V("act", "activation", [pb], [pre_b], out=dst, in_=src, func=AF.Copy)
            V("dve", "tensor_tensor", [pre_b, msk_b], [pre_b], out=preS[:, 1:8, 0:2], in0=preS[:, 0:7, 128:130],
              in1=msk[:, 1:8].unsqueeze(2).to_broadcast([128, 7, 2]), op=ALU.mult)
            V("dve", "tensor_scalar", [pre_b, msk_b], [pre_b], out=preS[:, 0, 0:2], in0=preS[:, 7, 128:130],
              scalar1=msk[:, 0:1], scalar2=None, op0=ALU.mult)
            V("dve", "tensor_tensor", [pre_b, msk_b], [pre_b], out=preS[:, 0:7, 130:132], in0=preS[:, 1:8, 2:4],
              in1=msk[:, 1:8].unsqueeze(2).to_broadcast([128, 7, 2]), op=ALU.mult)
            V("dve", "tensor_scalar", [pre_b, msk_b], [pre_b], out=preS[:, 7, 130:132], in0=preS[:, 0, 2:4],
              scalar1=msk[:, 0:1], scalar2=None, op0=ALU.mult)
            o, _w = PAR["conv_w"]
            dg, dgb = diags[dg_i[0] % 2]
            dg_i[0] += 1
            for k in range(5):
                V("dve", "tensor_scalar", [cstb_b, par_b], [dgb], out=dg[:, k, :], in0=IDENTB,
                  scalar1=par[:, o + cidx * 5 + k:o + cidx * 5 + k + 1], scalar2=None, op0=ALU.mult)
            for tb in range(3):
                pt, pb = PS((6, 7, 4)[tb])
                for k in range(5):
                    if tb == 0:
                        rhs = preP[:, :, k:k + 256]
                    else:
                        rhs = preS[:, 4 * (tb - 1):4 * tb, k:k + 128]
                    V("pe", "matmul", [pre_b, dgb], [pb], pt[:, :], lhsT=dg[:, k, :], rhs=rhs,
                      start=(k == 0), stop=(k == 4))
                sil(tb, pt, pb, fm, fm_b)
            return fm, fm_b

        def transposes_to(fm, fm_b, dst_own, dst_own_b, dst_rest, dst_rest_b):
            for half, (dst, dstb) in enumerate(((dst_own, dst_own_b), (dst_rest, dst_rest_b))):
                pt, pb = PS(2 + half)
                ptb = pt[:, :].bitcast(BF16)
                for cc in range(6):
                    c = half * 6 + cc
                    V("pe", "transpose", [fm_b, cstb_b], [pb], out=ptb[:, cc * 128:(cc + 1) * 128],
                      in_=fm[:, c * 128:(c + 1) * 128], identity=IDENTB)
                V("act", "activation", [pb], [dstb], out=dst,
                  in_=ptb[:, 0:768].rearrange("p (c n) -> p c n", c=6), func=AF.Copy)

        cb_o = PAR["conv_b"][0]
        for gl in range(2):
            cidx = 16 + 2 * p + gl
            def sil(tb, pt, pb, fm, fm_b, cidx=cidx):
                V("act", "activation", [pb, par_b], [fm_b], out=fm[:, tb * 512:(tb + 1) * 512], in_=pt[:, :],
                  func=AF.Silu, bias=par[:, cb_o + cidx:cb_o + cidx + 1], scale=1.0)
            fm, fm_b = proj_conv(bu, gl, cidx, sil)
            V("pool", "tensor_copy", [fm_b], [BT_b], out=BT[:, gl, :], in_=fm[:, 0:T])
            transposes_to(fm, fm_b, B_tok[:, :, gl * 128:(gl + 1) * 128], B_tok_b, B_rest[:, :, gl * 128:(gl + 1) * 128],
                          B_rest_b)
        for gl in range(2):
            cidx = 24 + 2 * p + gl
            def sil(tb, pt, pb, fm, fm_b, cidx=cidx, gl=gl):
                if tb == 0:
                    V("act", "activation", [pb, par_b], [CT_b], out=CT[:, gl, 0:512], in_=pt[:, :], func=AF.Silu,
                      bias=par[:, cb_o + cidx:cb_o + cidx + 1], scale=1.0)
                elif tb == 1:
                    V("act", "activation", [pb, par_b], [CT_b], out=CT[:, gl, 512:768], in_=pt[:, 0:256],
                      func=AF.Silu, bias=par[:, cb_o + cidx:cb_o + cidx + 1], scale=1.0)
            proj_conv(cu, gl, cidx, sil)
        for xl in range(4):
            cidx = 4 * p + xl
            gl = xl // 2
            g = 2 * p + gl
            def sil(tb, pt, pb, fm, fm_b, cidx=cidx):
                V("act", "activation", [pb, par_b], [fm_b], out=fm[:, tb * 512:(tb + 1) * 512], in_=pt[:, :],
                  func=AF.Silu, bias=par[:, cb_o + cidx:cb_o + cidx + 1], scale=1.0)
            fm, fm_b = proj_conv(xu, xl, cidx, sil)
            transposes_to(fm, fm_b, x_tok[:, :, xl * 128:(xl + 1) * 128], x_tok_b,
                          x_rest[:, :, (xl % 2) * 128:(xl % 2 + 1) * 128], x_rest_b)
            if xl % 2 == 1:
                pt4, pb4 = PS(4)
                ri = 0
                for d in range(2):
                    for i in range(6, 12):
                        xs_t, xs_b = xsr[ri % 4]
                        ri += 1
                        V("dve", "tensor_tensor", [x_rest_b, wrest_b], [xs_b],
                          out=xs_t.rearrange("p (h q) -> p h q", h=4),
                          in0=x_rest[:, i - 6, :].rearrange("p (h q) -> p h q", h=4),
                          in1=wrest[:, i - 6, d * 32 + 4 * g:d * 32 + 4 * g + 4].unsqueeze(2).to_broadcast(
                              [128, 4, 64]), op=ALU.mult)
                        V("pe", "matmul", [B_rest_b, xs_b], [pb4], pt4[:, d * 256:(d + 1) * 256],
                          lhsT=B_rest[:, i - 6, gl * 128:(gl + 1) * 128], rhs=xs_t, start=(i == 6), stop=(i == 11))
                for d in range(2):
                    dma_in(h0t[:, d], h0t_b, st0v[g][:, d])
                pt5, pb5 = PS(5)
                for d in range(2):
                    for t_ in range(2):
                        V("pe", "transpose", [h0t_b, cst_b], [pb5],
                          out=pt5[:, (d * 2 + t_) * 128:(d * 2 + t_ + 1) * 128], in_=h0t[:, d, t_, :],
                          identity=IDENT)
                V("dve", "tensor_tensor", [pb5, ptot_b], [htmp_b],
                  out=htmp.rearrange("p (d h q) -> p d h q", d=2, h=4),
                  in0=pt5[:, :].rearrange("p (d h q) -> p d h q", d=2, h=4),
                  in1=ptot.rearrange("p (d h) -> p d h", d=2)[:, :, 4 * g:4 * g + 4].unsqueeze(3).to_broadcast(
                      [128, 2, 4, 64]), op=ALU.mult)
                V("dve", "tensor_tensor", [pb4, htmp_b], [H_b], out=H[:, :, gl * 256:(gl + 1) * 256],
                  in0=pt4[:, :].rearrange("p (d c) -> p d c", d=2), in1=htmp.rearrange("p (d c) -> p d c", d=2),
                  op=ALU.add)

        if dbg == "chain" and p == 0:
            dump(x_tok.rearrange("p a b -> p (a b)"), x_tok_b, 3072)
            dump(B_tok.rearrange("p a b -> p (a b)"), B_tok_b, 1536)
            dump(H.rearrange("p a b -> p (a b)"), H_b, 1024)
            dump(CT[:, 0, :], CT_b, 768)
            dump(BT[:, 1, :], BT_b, 768)
            return end_dbg()

        SLT.reset()
        xs_pool = [SLT.carve([128, 512], BF16) for _ in range(4)]
        xs_i = [0]
        scss = [SLT.carve([128, 2, 128], F32) for _ in range(2)]
        lt4s = [SLT.carve([128, 4, 128], BF16) for _ in range(4)]
        lt4bufs = [[Buf("lt4_%d_%d" % (a_, b_)) for b_ in range(4)] for a_ in range(4)]
        for a_ in range(4):
            for b_ in lt4bufs[a_]:
                b_.t.rd = dict(lt4s[a_][1].t.rd)
                SLT.tiles.append(b_)
        mt4s = [SLT.carve([128, 4, 128], BF16) for _ in range(4)]
        yaccs = [SLT.carve([128, 512], F32) for _ in range(2)]
        ytmp, ytmp_b = SLT.carve([128, 512], F32)
        ybfs = [SLT.carve([128, 512], BF16) for _ in range(2)]
        yc_i = [0]
        stg = [SLT.carve([128, 4, 128], F32, dma=True) for _ in range(2)]

        def hb8(ap2d):
            return ap2d.rearrange("p (h q) -> p h q", h=8)

        def bc8(ap8):
            return ap8.unsqueeze(2).to_broadcast([128, 8, 64])

        def make_xs(c, wap, wbufs):
            t_, b_ = xs_pool[xs_i[0] % 4]
            xs_i[0] += 1
            V("dve", "tensor_tensor", [x_tok_b] + wbufs, [b_], out=hb8(t_), in0=hb8(x_tok[:, c, :]), in1=bc8(wap),
              op=ALU.mult)
            return t_, b_

        def state_mm(c, xs):
            pt, pb = PS(6)
            for gl in range(2):
                V("pe", "matmul", [B_tok_b, xs[1]], [pb], pt[:, gl * 256:(gl + 1) * 256],
                  lhsT=B_tok[:, c, gl * 128:(gl + 1) * 128], rhs=xs[0][:, gl * 256:(gl + 1) * 256],
                  start=True, stop=True)
            return pt, pb

        def wd(c, d):
            return wdec[:, c, d * 32 + 8 * p:d * 32 + 8 * p + 8]

        def ent_from_state(c, d, ent, hdir=None, etot_c=None):
            xs = make_xs(c, wd(c, d), [wdec_b])
            pt, pb = state_mm(c, xs)
            if hdir is None:
                V("act", "activation", [pb], [ent[1]], out=ent[0], in_=pt[:, :], func=AF.Copy)
            else:
                V("dve", "tensor_tensor", [H_b, Eown_b], [ytmp_b], out=hb8(ytmp), in0=hb8(H[:, hdir, :]),
                  in1=bc8(Eown[:, etot_c, 128 + hdir * 32 + 8 * p:128 + hdir * 32 + 8 * p + 8]), op=ALU.mult)
                V("dve", "tensor_tensor", [pb, ytmp_b], [ent[1]], out=ent[0], in0=pt[:, :], in1=ytmp, op=ALU.add)

        def finals(s, d):
            c0, c1 = 2 * s, 2 * s + 1
            if d == 0:
                xa = make_xs(c0, wfin[:, 2 * s, 8 * p:8 * p + 8], [wfin_b]); ca = c0
                xb = make_xs(c1, wd(c1, 0), [wdec_b]); cb = c1
            else:
                xa = make_xs(c1, wfin[:, 2 * s + 1, 8 * p:8 * p + 8], [wfin_b]); ca = c1
                xb = make_xs(c0, wd(c0, 1), [wdec_b]); cb = c0
            pt, pb = PS(5)
            for i in range(4):
                gl = i // 2
                V("pe", "matmul", [xa[1], B_tok_b], [pb], pt[:, i * 128:(i + 1) * 128],
                  lhsT=xa[0][:, i * 128:(i + 1) * 128], rhs=B_tok[:, ca, gl * 128:(gl + 1) * 128],
                  start=True, stop=False)
                V("pe", "matmul", [xb[1], B_tok_b], [pb], pt[:, i * 128:(i + 1) * 128],
                  lhsT=xb[0][:, i * 128:(i + 1) * 128], rhs=B_tok[:, cb, gl * 128:(gl + 1) * 128],
                  start=False, stop=True)
            st_, sb_ = stg[stg_i[0] % 2]
            stg_i[0] += 1
            V("act", "activation", [pb], [sb_], out=st_, in_=pt[:, :].rearrange("p (i n) -> p i n", i=4),
              func=AF.Copy)
            r0 = (s * 2 + d) * 2048 + 512 * p
            dst = ns_out[r0:r0 + 512, :].rearrange("(i r) n -> r i n", r=128)
            P.op("sp", (lambda E, dst=dst, st_=st_: E.dma_start(out=dst, in_=st_)), rd=[sb_], dma=sb_)
            if sb_ not in out_bufs:
                out_bufs.append(sb_)

        yn_i = [0]

        def y_front(c, ent_f, ent_b):
            scs, scs_b = scss[yc_i[0] % 2]
            yacc, yacc_b = yaccs[yc_i[0] % 2]
            ybf, ybf_b = ybfs[yc_i[0] % 2]
            pt3, pb3 = PS((3, 7)[yc_i[0] % 2])
            yc_i[0] += 1
            pt0, pb0 = PS(0)
            for gl in range(2):
                V("pe", "matmul", [BT_b, CT_b], [pb0], pt0[:, gl * 128:(gl + 1) * 128],
                  lhsT=BT[:, gl, c * 128:(c + 1) * 128], rhs=CT[:, gl, c * 128:(c + 1) * 128], start=True, stop=True)
            V("act", "activation", [pb0], [scs_b], out=scs, in_=pt0[:, 0:256].rearrange("p (g n) -> p g n", g=2),
              func=AF.Copy)
            xsd = [make_xs(c, dtv[:, c, d * 32 + 8 * p:d * 32 + 8 * p + 8], [dtv_b]) for d in range(2)]
            for gl in range(2):
                mts = []
                for d in range(2):
                    n = yn_i[0]
                    yn_i[0] += 1
                    ptS, pbS = PS((1, 2)[n % 2])
                    lt4, _ = lt4s[n % 4]
                    ltb = lt4bufs[n % 4]
                    mt4, mt4b = mt4s[n % 4]
                    mts.append((mt4, mt4b))
                    for hh in range(4):
                        hl = gl * 4 + hh
                        ci = d * 32 + 8 * p + hl
                        dst = ptS[:, hh * 128:(hh + 1) * 128]
                        V("pe", "matmul", [cs_hi_b, cstb_b], [pbS], dst,
                          lhsT=cs_hi[:, c, ci:ci + 1].to_broadcast([128, 128]), rhs=IDENTB, start=True, stop=False)
                        V("pe", "matmul", [cs_lo_b, cstb_b], [pbS], dst,
                          lhsT=cs_lo[:, c, ci:ci + 1].to_broadcast([128, 128]), rhs=IDENTB, start=False, stop=False)
                        V("pe", "matmul", [negm_b, cstb_b], [pbS], dst, lhsT=IDENTB, rhs=negm[:, d, :],
                          start=False, stop=True)
                    for hh in range(4):
                        hl = gl * 4 + hh
                        ci = d * 32 + 8 * p + hl
                        V("act", "activation", [pbS, negcs_b], [ltb[hh]], out=lt4[:, hh, :],
                          in_=ptS[:, hh * 128:(hh + 1) * 128], func=AF.Exp, bias=negcs[:, c, ci:ci + 1], scale=1.0)
                    V("dve", "tensor_tensor", ltb + [scs_b], [mt4b], out=mt4, in0=lt4,
                      in1=scs[:, gl, :].unsqueeze(1).to_broadcast([128, 4, 128]), op=ALU.mult)
                for hh in range(4):
                    hl = gl * 4 + hh
                    for d in range(2):
                        V("pe", "matmul", [mts[d][1], xsd[d][1]], [pb3], pt3[:, hl * 64:(hl + 1) * 64],
                          lhsT=mts[d][0][:, hh, :], rhs=xsd[d][0][:, hl * 64:(hl + 1) * 64],
                          start=(d == 0), stop=(d == 1))
            return (c, ent_f, ent_b, yacc, yacc_b, ybf, ybf_b, pt3, pb3)

        def y_tail(ctx):
            (c, ent_f, ent_b, yacc, yacc_b, ybf, ybf_b, pt3, pb3) = ctx
            for d, ent in ((0, ent_f), (1, ent_b)):
                if ent is None:
                    continue
                ptO, pbO = PS(4 + d)
                for gl in range(2):
                    V("pe", "matmul", [CT_b, ent[1]], [pbO], ptO[:, gl * 256:(gl + 1) * 256],
                      lhsT=CT[:, gl, c * 128:(c + 1) * 128], rhs=ent[0][:, gl * 256:(gl + 1) * 256],
                      start=True, stop=True)
            V("dve", "tensor_tensor", [x_tok_b, par_b], [yacc_b], out=hb8(yacc), in0=hb8(x_tok[:, c, :]),
              in1=bc8(pc("d_skip")[:, 8 * p:8 * p + 8]), op=ALU.mult)
            V("dve", "tensor_tensor", [pb3, yacc_b], [yacc_b], out=yacc, in0=pt3[:, :], in1=yacc, op=ALU.add)
            for d, ent in ((0, ent_f), (1, ent_b)):
                if ent is None:
                    continue
                ptO, pbO = PS(4 + d)
                V("dve", "tensor_tensor", [pbO, Eown_b], [ytmp_b], out=hb8(ytmp), in0=hb8(ptO[:, :]),
                  in1=bc8(Eown[:, c, d * 32 + 8 * p:d * 32 + 8 * p + 8]), op=ALU.mult)
                V("dve", "tensor_tensor", [ytmp_b, yacc_b], [yacc_b], out=yacc, in0=yacc, in1=ytmp, op=ALU.add)
            V("act", "activation", [yacc_b], [ybf_b], out=ybf, in_=yacc, func=AF.Copy)
            pt6, pb6 = PS(6)
            pt6b = pt6[:, :].bitcast(BF16)
            for j in range(4):
                V("pe", "transpose", [ybf_b, cstb_b], [pb6], out=pt6b[:, j * 128:(j + 1) * 128],
                  in_=ybf[:, j * 128:(j + 1) * 128], identity=IDENTB)
            V("act", "activation", [pb6], [yTb[p]], out=yT[:, 4 * p:4 * p + 4, c * 128:(c + 1) * 128],
              in_=pt6b[:, 0:512].rearrange("p (j n) -> p j n", j=4), func=AF.Copy)

        jobs = []
        for s in range(2):
            c0, c1 = 2 * s, 2 * s + 1
            ent_a, ent_bb = ents[2 * s], ents[2 * s + 1]

            def pre(s=s, c0=c0, c1=c1, ent_a=ent_a, ent_bb=ent_bb):
                ent_from_state(c0, 0, ent_a)
                ent_from_state(c1, 1, ent_bb)
                finals(s, 0)
                finals(s, 1)
            jobs.append((pre, c0, None, ent_bb))
            jobs.append((None, c1, ent_a, None))
        e0, e1, e2, e3 = ents

        def pre4():
            V("act", "activation", [H_b], [e0[1]], out=e0[0], in_=H[:, 0, :], func=AF.Copy)
            ent_from_state(5, 1, e1, hdir=1, etot_c=5)

        def pre5():
            ent_from_state(4, 0, e2, hdir=0, etot_c=4)
            V("act", "activation", [H_b], [e3[1]], out=e3[0], in_=H[:, 1, :], func=AF.Copy)
        jobs.append((pre4, 4, e0, e1))
        jobs.append((pre5, 5, e2, e3))
        prev_ctx = None
        for (pre, c, ef, eb_) in jobs:
            if pre is not None:
                pre()
            ctx = y_front(c, ef, eb_)
            if prev_ctx is not None:
                y_tail(prev_ctx)
            prev_ctx = ctx
        y_tail(prev_ctx)

    if dbg == "ssd":
        dump(yT[:, 0, :], yT_b, 768)
        dump(yT[:, 5, :], yT_b, 768)
        dump(yT[:, 15, :], yT_b, 768)
        P.ops["pool"][-1][1].update([b_.t.lw for b_ in yTb if b_.t.lw is not None])
        P.ops["pool"][-2][1].update([b_.t.lw for b_ in yTb if b_.t.lw is not None])
        P.ops["pool"][-3][1].update([b_.t.lw for b_ in yTb if b_.t.lw is not None])
        return end_dbg()

    SLT.reset(); SL2.reset(); SL3.reset()
    yrd = [yT_b] + yTb

    def hbufs(bi):
        return hTb[0:4] if bi == 0 else hTb[4:6]

    pf_i = [0]

    def proj_fm(units, jt, rhs_tile, rhs_bufs_fn, evac):
        par_ = pf_i[0] % 2
        pf_i[0] += 1
        nk = 8 * len(units)
        for bi, (t0, n, cnd) in enumerate(BLK):
            pt, pb = PS(par_ * 2 + bi)
            kk = 0
            for (ut, ub) in units:
                for k in range(8):
                    V("pe", "matmul", [ub] + rhs_bufs_fn(bi), [pb], pt[:, 0:n],
                      lhsT=ut[:, k, jt * 128:(jt + 1) * 128], rhs=rhs_tile[:, kk, t0:t0 + n],
                      start=(kk == 0), stop=(kk == nk - 1))
                    kk += 1
            evac(bi, t0, n, cnd, pt[:, 0:n], pb)

    gs, gs_b = SLT.carve([128, 8, T], BF16)
    gc, gc_b = SLT.carve([128, 8, T], BF16)
    bg_o = PAR["b_gate"][0]
    for (c0, gt, gb, joff) in ((8256, gs, gs_b, 0), (9280, gc, gc_b, 8)):
        gu_ = load_unit(w_in_d, c0, 1024)
        for j in range(8):
            def evac(bi, t0, n, cnd, pap, pb, j=j, gt=gt, gb=gb, joff=joff):
                V("act", "activation", [pb, par_b], [gb], out=gt[:, j, t0:t0 + n], in_=pap, func=AF.Sigmoid,
                  bias=par[:, bg_o + joff + j:bg_o + joff + j + 1], scale=1.0)
            proj_fm([gu_], j, hT, hbufs, evac)

    szs = [SL3.carve([128, T]) for _ in range(2)]
    sqs = [SL3.carve([128, T], BF16) for _ in range(2)]
    rgs = [SL3.carve([128, T]) for _ in range(2)]
    zus = [None, None]
    for j in range(16):
        if j % 8 == 0:
            zus[j // 8] = load_unit(w_in_d, (j // 8) * 1024, 1024)
        zu = zus[j // 8]
        sz, szb = szs[j % 2]
        sq, sqb = sqs[j % 2]

        def evac(bi, t0, n, cnd, pap, pb, sz=sz, szb=szb):
            V("act", "activation", [pb], [szb], out=sz[:, t0:t0 + n], in_=pap, func=AF.Silu)
        proj_fm([zu], j % 8, hT, hbufs, evac)
        V("dve", "tensor_tensor", yrd + [szb], [yT_b], out=yT[:, j, :], in0=yT[:, j, :], in1=sz, op=ALU.mult)
        V("act", "activation", [yT_b], [sqb], out=sq, in_=yT[:, j, :], func=AF.Square)
        for bi, (t0, n, cnd) in enumerate(BLK):
            V("pe", "matmul", [sqb, cstb_b], [PS(4 + bi)[1]], PS(4 + bi)[0][:, 0:n], lhsT=ONESB,
              rhs=sq[:, t0:t0 + n], start=(j % 2 == 0), stop=(j % 2 == 1))
        if j % 2 == 1:
            rg, rgb = rgs[(j // 2) % 2]
            for bi, (t0, n, cnd) in enumerate(BLK):
                rsqrt_from(rg[:, t0:t0 + n], rgb, PS(4 + bi)[0][:, 0:n], PS(4 + bi)[1], 1.0 / 256)
            for jj in (j - 1, j):
                V("dve", "scalar_tensor_tensor", [yT_b, rgb, par_b], [yT_b], out=yT[:, jj, :], in0=yT[:, jj, :],
                  scalar=pc("ssm_norm_w")[:, jj:jj + 1], in1=rg, op0=ALU.mult, op1=ALU.mult)

    if dbg == "yn":
        dump(yT[:, 0, :], yT_b, 768)
        dump(yT[:, 9, :], yT_b, 768)
        dump(gs[:, 3, :], gs_b, 768)
        dump(gc[:, 7, :], gc_b, 768)
        return end_dbg()

    mT, mT_b = SL2.carve([128, 8, T])
    so1 = load_unit(w_ssd_d, 0, 1024, k0=0)
    so2 = load_unit(w_ssd_d, 0, 1024, k0=8)
    for dd in range(8):
        def evac(bi, t0, n, cnd, pap, pb, dd=dd):
            V("dve", "tensor_tensor", [pb, gs_b], [mT_b], out=mT[:, dd, t0:t0 + n], in0=pap,
              in1=gs[:, dd, t0:t0 + n], op=ALU.mult)
        proj_fm([so1, so2], dd, yT, lambda bi: [yT_b], evac)

    SL1.reset()
    SL3.reset()
    uc, uc_b = SL3.carve([128, 8, T])
    upres = [SL1.carve([128, 948], BF16) for _ in range(2)]
    sg, sg_b = SL1.carve([128, T])
    dg31s = [SL1.carve([128, 31, 128], BF16) for _ in range(2)]
    for (up, upb) in upres:
        V("dve", "memset", [], [upb], up, 0.0)
    au = load_unit(w_in_d, 6208, 1024)
    bu_ = load_unit(w_in_d, 7232, 1024)
    cw_o = PAR["cdw_w"][0]
    cdb_o = PAR["cdw_b"][0]
    for j in range(8):
        up, upb = upres[j % 2]
        upP = up[:, 0:572].rearrange("p (s t) -> p s t", s=2)
        upS = up[:, 572:948].rearrange("p (s t) -> p s t", s=4)
        for bi, (t0, n, cnd) in enumerate(BLK):
            pa, pab = PS(bi)
            pbt, pbb = PS(2 + bi)
            for k in range(8):
                V("pe", "matmul", [au[1]] + hbufs(bi), [pab], pa[:, 0:n], lhsT=au[0][:, k, j * 128:(j + 1) * 128],
                  rhs=hT[:, k, t0:t0 + n], start=(k == 0), stop=(k == 7))
            for k in range(8):
                V("pe", "matmul", [bu_[1]] + hbufs(bi), [pbb], pbt[:, 0:n],
                  lhsT=bu_[0][:, k, j * 128:(j + 1) * 128], rhs=hT[:, k, t0:t0 + n], start=(k == 0), stop=(k == 7))
            V("act", "activation", [pbb], [sg_b], out=sg[:, t0:t0 + n], in_=pbt[:, 0:n], func=AF.Sigmoid)
            if bi == 0:
                V("dve", "tensor_tensor", [pab, sg_b], [upb], out=upP[:, :, 15:271],
                  in0=pa[:, 0:512].rearrange("p (s t) -> p s t", s=2),
                  in1=sg[:, 0:512].rearrange("p (s t) -> p s t", s=2), op=ALU.mult)
            else:
                V("dve", "tensor_tensor", [pab, sg_b], [upb], out=upS[:, :, 15:79],
                  in0=pa[:, 0:256].rearrange("p (s t) -> p s t", s=4),
                  in1=sg[:, 512:768].rearrange("p (s t) -> p s t", s=4), op=ALU.mult)
        dg, dgb = dg31s[j % 2]
        dgks = [Buf("dg31_%d_%d" % (j, k)) for k in range(31)]
        for k in range(31):
            dgks[k].t.rd = dict(dgb.t.rd)
            if dgb.t.lw is not None:
                dgks[k].t.rd[("lw", id(dgb))] = dgb.t.lw
            SL1.tiles.append(dgks[k])
            wc_ = par[:, cw_o + j * 31 + k:cw_o + j * 31 + k + 1]
            if k % 2 == 0:
                V("dve", "tensor_scalar", [cstb_b, par_b], [dgks[k]], out=dg[:, k, :], in0=IDENTB,
                  scalar1=wc_, scalar2=None, op0=ALU.mult)
            else:
                V("act", "activation", [cstb_b, par_b], [dgks[k]], out=dg[:, k, :], in_=IDENTB, func=AF.Copy,
                  scale=wc_)
        for bi, (t0, n, cnd) in enumerate(BLK):
            pt, pb = PS(4 + 2 * (j % 2) + bi)
            for k in range(31):
                rhs = upP[:, :, k:k + 256] if bi == 0 else upS[:, :, k:k + 64]
                V("pe", "matmul", [upb, dgks[k]], [pb], pt[:, 0:n], lhsT=dg[:, k, :], rhs=rhs,
                  start=(k == 0), stop=(k == 30))
                dgb.t.rd["pe"] = dgks[k].t.rd.get("pe", dgb.t.rd.get("pe"))
            V("act", "activation", [pb, par_b], [uc_b], out=uc[:, j, t0:t0 + n], in_=pt[:, 0:n], func=AF.Identity,
              bias=par[:, cdb_o + j:cdb_o + j + 1], scale=1.0)

    SL1.reset()
    un, un_b = SL1.carve([128, 8, T], BF16)
    lnr, lnr_b = SL1.carve([128, T])
    t1s = [SL1.carve([128, T]) for _ in range(2)]
    ucb_, ucbb = SLT.carve([128, T], BF16)
    sq2, sq2b = SLT.carve([128, T], BF16)
    mu, mu_b = SLT.carve([128, T])
    for j in range(8):
        V("act", "activation", [uc_b], [ucbb], out=ucb_, in_=uc[:, j, :], func=AF.Copy)
        V("act", "activation", [uc_b], [sq2b], out=sq2, in_=uc[:, j, :], func=AF.Square)
        for bi, (t0, n, cnd) in enumerate(BLK):
            V("pe", "matmul", [ucbb, cstb_b], [PS(4 + bi)[1]], PS(4 + bi)[0][:, 0:n], lhsT=ONESB,
              rhs=ucb_[:, t0:t0 + n], start=(j == 0), stop=(j == 7))
            V("pe", "matmul", [sq2b, cstb_b], [PS(6 + bi)[1]], PS(6 + bi)[0][:, 0:n], lhsT=ONESB,
              rhs=sq2[:, t0:t0 + n], start=(j == 0), stop=(j == 7))
    for bi, (t0, n, cnd) in enumerate(BLK):
        V("act", "activation", [PS(4 + bi)[1]], [mu_b], out=mu[:, t0:t0 + n], in_=PS(4 + bi)[0][:, 0:n],
          func=AF.Copy, scale=1.0 / 1024)
    V("dve", "tensor_tensor", [mu_b], [lnr_b], out=lnr, in0=mu, in1=mu, op=ALU.mult)
    for bi, (t0, n, cnd) in enumerate(BLK):
        V("dve", "scalar_tensor_tensor", [PS(6 + bi)[1], lnr_b], [lnr_b], out=lnr[:, t0:t0 + n],
          in0=PS(6 + bi)[0][:, 0:n], scalar=1.0 / 1024, in1=lnr[:, t0:t0 + n], op0=ALU.mult, op1=ALU.subtract)
    rsqrt_from(lnr, lnr_b, lnr, lnr_b, 1.0)
    for j in range(8):
        t1, t1b = t1s[j % 2]
        V("dve", "tensor_tensor", [uc_b, mu_b], [t1b], out=t1, in0=uc[:, j, :], in1=mu, op=ALU.subtract)
        V("dve", "tensor_tensor", [t1b, lnr_b], [t1b], out=t1, in0=t1, in1=lnr, op=ALU.mult)
        V("act", "activation", [t1b, par_b], [un_b], out=un[:, j, :], in_=t1, func=AF.Silu,
          scale=pc("ln_w")[:, j:j + 1], bias=pc("ln_b")[:, j:j + 1])
    cuo = load_unit(w_conf_d, 0, 1024)
    for dd in range(8):
        t1, t1b = t1s[dd % 2]

        def evac(bi, t0, n, cnd, pap, pb, dd=dd, t1=t1, t1b=t1b):
            V("dve", "scalar_tensor_tensor", [pb, gc_b, par_b], [t1b], out=t1[:, t0:t0 + n], in0=pap,
              scalar=pc("b_conf_out")[:, dd:dd + 1], in1=gc[:, dd, t0:t0 + n], op0=ALU.add, op1=ALU.mult)
            V("dve", "tensor_tensor", [t1b, mT_b], [mT_b], out=mT[:, dd, t0:t0 + n], in0=mT[:, dd, t0:t0 + n],
              in1=t1[:, t0:t0 + n], op=ALU.add)
        proj_fm([cuo], dd, un, lambda bi: [un_b], evac)

    SLT.reset()
    mb, mb_b = SLT.carve([128, 8, T], BF16)
    xts = [SLT.carve([128, 1024], F32, dma=True) for _ in range(2)]
    V("act", "activation", [mT_b], [mb_b], out=mb, in_=mT, func=AF.Copy)
    SL3.reset()
    x1T, x1T_b = SL3.carve([128, 8, T])
    for i in range(6):
        xtt, xtb = xts[i % 2]
        dma_in(xtt, xtb, x_all[i * 128:(i + 1) * 128, :])

        def evac(j, pap, pb, i=i):
            eng = "dve" if j < 4 else "act"
            if eng == "dve":
                V("dve", "tensor_copy", [pb], [x1T_b], out=x1T[:, j, i * 128:(i + 1) * 128], in_=pap)
            else:
                V("act", "activation", [pb], [x1T_b], out=x1T[:, j, i * 128:(i + 1) * 128], in_=pap, func=AF.Copy)
        transpose_x_tile(xtt, xtb, evac)
    wou = load_unit(w_o_d, 0, 1024)
    for dd in range(8):
        def evac(bi, t0, n, cnd, pap, pb, dd=dd):
            V("dve", "scalar_tensor_tensor", [pb, x1T_b, mod_b], [x1T_b], out=x1T[:, dd, t0:t0 + n], in0=pap,
              scalar=G1(dd, cnd), in1=x1T[:, dd, t0:t0 + n], op0=ALU.mult, op1=ALU.add)
        proj_fm([wou], dd, mb, lambda bi: [mb_b], evac)

    if dbg == "mix":
        for j in (0, 3, 7):
            dump(x1T[:, j, :], x1T_b, 768)
        return end_dbg()

    SL0.reset(); SL1.reset(); SL2.reset(); SLT.reset()
    acc, acc_b = SL0.carve([128, 8, T])
    acts = [SL2.carve([128, 8, T], BF16) for _ in range(2)]
    h2T, h2T_b = SLT.carve([128, 8, T], BF16)
    combT, combT_b = SLT.carve([128, T], BF16)
    rb, rb_b = SLT.carve([128, T])
    sqm, sqm_b = SLT.carve([128, T], BF16)
    lg, lg_b = SLT.carve([128, 6, 32])
    ex, ex_b = SLT.carve([128, 6, 32])
    mk, mk_b = SLT.carve([128, 6, 32])
    comb, comb_b = SLT.carve([128, 6, 32])
    cma_hi, cma_hi_b = SLT.carve([128, 6, 32], BF16)
    cma_lo, cma_lo_b = SLT.carve([128, 6, 32], BF16)
    m8, m8_b = SLT.carve([128, 6, 8])
    sm, sm_b = SLT.carve([128, 6])
    bdn, bdn_b = SLT.carve([128, 1024], BF16, dma=True)
    wrt, wrt_b = SLT.carve([128, 8, 32], F32, dma=True)
    wrt_hi, wrt_hi_b = SLT.carve([128, 8, 32], BF16)
    wrt_lo, wrt_lo_b = SLT.carve([128, 8, 32], BF16)
    h2lo, h2lo_b = SL1.carve([128, 8, T], BF16)
    h2fs = [SL1.carve([128, T]) for _ in range(2)]

    def rms_bc(src, src_b, scale):
        for j in range(8):
            V("act", "activation", [src_b], [sqm_b], out=sqm, in_=src[:, j, :], func=AF.Square)
            for bi, (t0, n, cnd) in enumerate(BLK):
                V("pe", "matmul", [sqm_b, cstb_b], [PS(4 + bi)[1]], PS(4 + bi)[0][:, 0:n], lhsT=ONESB,
                  rhs=sqm[:, t0:t0 + n], start=(j == 0), stop=(j == 7))
        for bi, (t0, n, cnd) in enumerate(BLK):
            rsqrt_from(rb[:, t0:t0 + n], rb_b, PS(4 + bi)[0][:, 0:n], PS(4 + bi)[1], scale)

    rms_bc(x1T, x1T_b, 1.0 / 1024)
    P.op("pool", lambda E: E.dma_start(out=bdn[0:32, :], in_=b_dn_d), wr=[bdn_b], dma=bdn_b)
    dma_in(wrt, wrt_b, w_rt_d.rearrange("p (k e) -> p k e", k=8))
    V("dve", "tensor_copy", [wrt_b], [wrt_hi_b], out=wrt_hi, in_=wrt)
    V("dve", "tensor_tensor", [wrt_b, wrt_hi_b], [wrt_lo_b], out=wrt_lo, in0=wrt, in1=wrt_hi, op=ALU.subtract)
    for j in range(8):
        h2f, h2fb = h2fs[j % 2]
        for bi, (t0, n, cnd) in enumerate(BLK):
            V("dve", "scalar_tensor_tensor", [x1T_b, rb_b, modA_b], [h2fb], out=h2f[:, t0:t0 + n],
              in0=x1T[:, j, t0:t0 + n], scalar=A2(j, cnd), in1=rb[:, t0:t0 + n], op0=ALU.mult, op1=ALU.mult)
            V("act", "activation", [h2fb, mod_b], [h2fb], out=h2f[:, t0:t0 + n], in_=h2f[:, t0:t0 + n],
              func=AF.Identity, bias=S2(j, cnd), scale=1.0)
        V("dve", "tensor_copy", [h2fb], [h2T_b], out=h2T[:, j, :], in_=h2f)
        V("dve", "tensor_tensor", [h2fb, h2T_b], [h2lo_b], out=h2lo[:, j, :], in0=h2f, in1=h2T[:, j, :],
          op=ALU.subtract)
    pt0, pb0 = PS(0)
    for i in range(6):
        n_mm = 0
        for k in range(8):
            for (lt_, ltb, rt_, rtb) in ((h2T, h2T_b, wrt_hi, wrt_hi_b), (h2T, h2T_b, wrt_lo, wrt_lo_b),
                                         (h2lo, h2lo_b, wrt_hi, wrt_hi_b)):
                V("pe", "matmul", [ltb, rtb], [pb0], pt0[:, i * 32:(i + 1) * 32],
                  lhsT=lt_[:, k, i * 128:(i + 1) * 128], rhs=rt_[:, k, :], start=(n_mm == 0), stop=(n_mm == 23))
                n_mm += 1
    V("dve", "tensor_tensor", [pb0, par_b], [lg_b], out=lg, in0=pt0[:, 0:192].rearrange("p (i e) -> p i e", i=6),
      in1=pc("b_router").unsqueeze(1).to_broadcast([128, 6, 32]), op=ALU.add)
    for i in range(6):
        V("dve", "max", [lg_b], [m8_b], out=m8[:, i, :], in_=lg[:, i, :])
    V("dve", "tensor_tensor", [lg_b, m8_b], [mk_b], out=mk, in0=lg, in1=m8[:, :, 3:4].to_broadcast([128, 6, 32]),
      op=ALU.is_ge)
    V("dve", "tensor_tensor", [lg_b, m8_b], [ex_b], out=ex, in0=lg, in1=m8[:, :, 0:1].to_broadcast([128, 6, 32]),
      op=ALU.subtract)
    V("act", "activation", [ex_b], [ex_b], out=ex, in_=ex, func=AF.Exp)
    V("dve", "tensor_tensor", [ex_b, mk_b], [ex_b], out=ex, in0=ex, in1=mk, op=ALU.mult)
    V("dve", "tensor_reduce", [ex_b], [sm_b], out=sm, in_=ex, axis=AX.X, op=ALU.add)
    V("dve", "reciprocal", [sm_b], [sm_b], out=sm, in_=sm)
    V("dve", "tensor_tensor", [ex_b, sm_b], [comb_b], out=comb, in0=ex,
      in1=sm.unsqueeze(2).to_broadcast([128, 6, 32]), op=ALU.mult)
    V("dve", "tensor_scalar", [comb_b], [ex_b], out=ex, in0=comb, scalar1=1.0 / SW_ALPHA, scalar2=None, op0=ALU.mult)
    V("dve", "tensor_copy", [ex_b], [cma_hi_b], out=cma_hi, in_=ex)
    V("dve", "tensor_tensor", [ex_b, cma_hi_b], [cma_lo_b], out=cma_lo, in0=ex, in1=cma_hi, op=ALU.subtract)
    for i in range(6):
        ptc, pbc = PS(1 + i // 4)
        V("pe", "transpose", [comb_b, cst_b], [pbc], out=ptc[0:32, (i % 4) * 128:(i % 4 + 1) * 128],
          in_=comb[:, i, :], identity=IDENT)
    V("act", "activation", [PS(1)[1]], [combT_b], out=combT[0:32, 0:512], in_=PS(1)[0][0:32, 0:512], func=AF.Copy)
    V("act", "activation", [PS(2)[1]], [combT_b], out=combT[0:32, 512:768], in_=PS(2)[0][0:32, 0:256], func=AF.Copy)
    for dd in range(8):
        for bi, (t0, n, cnd) in enumerate(BLK):
            pt, pb = PS(4 + (dd * 2 + bi) % 2)
            V("pe", "matmul", [bdn_b, combT_b], [pb], pt[:, 0:n], lhsT=bdn[0:32, dd * 128:(dd + 1) * 128],
              rhs=combT[0:32, t0:t0 + n], start=True, stop=True)
            V("act", "activation", [pb], [acc_b], out=acc[:, dd, t0:t0 + n], in_=pt[:, 0:n], func=AF.Copy)

    if dbg == "router":
        dump(comb.rearrange("p a b -> p (a b)"), comb_b, 192)
        dump(acc[:, 2, :], acc_b, 768)
        dump(h2T[:, 5, :], h2T_b, 768)
        return end_dbg()

    SL1.reset()
    gsbs = [SL1.carve([128, T]) for _ in range(2)]
    tts = [SL1.carve([128, T]) for _ in range(2)]
    ubs = [SL1.carve([128, T]) for _ in range(2)]
    cmbs = [SL1.carve([128, T]) for _ in range(2)]
    bgu_o = PAR["b_gu"][0]

    def make_cmb(e):
        cm_t, cm_b = cmbs[e % 2]
        for i in range(6):
            pt, pb = PS(6 + i // 4)
            dst = pt[:, (i % 4) * 128:(i % 4 + 1) * 128]
            V("pe", "matmul", [cma_hi_b, cstb_b], [pb], dst, lhsT=cma_hi[:, i, e:e + 1].to_broadcast([128, 128]),
              rhs=IDENTB, start=True, stop=False)
            V("pe", "matmul", [cma_lo_b, cstb_b], [pb], dst, lhsT=cma_lo[:, i, e:e + 1].to_broadcast([128, 128]),
              rhs=IDENTB, start=False, stop=True)
        V("act", "activation", [PS(6)[1]], [cm_b], out=cm_t[:, 0:512], in_=PS(6)[0][:, 0:512], func=AF.Copy)
        V("act", "activation", [PS(7)[1]], [cm_b], out=cm_t[:, 512:768], in_=PS(7)[0][:, 0:256], func=AF.Copy)
        return cm_t, cm_b

    jcount = [0]

    def expert_gu(e, jh, U, act, cm, down_iter=None):
        ut, ub_ = U
        act_t, act_b = act
        cm_t, cm_b = cm
        for jj in range(4):
            j = jh * 4 + jj
            s_ = jcount[0] % 2
            jcount[0] += 1
            gA, gAb = PS(3 * s_)
            uA, uAb = PS(3 * s_ + 1)
            gB, gBb = PS(3 * s_ + 2)
            for k in range(8):
                w_ = ut[:, k, jj * 128:(jj + 1) * 128]
                V("pe", "matmul", [ub_, h2T_b], [gAb], gA[:, 0:512], lhsT=w_, rhs=h2T[:, k, 0:512],
                  start=(k == 0), stop=(k == 7))
                V("pe", "matmul", [ub_, h2T_b], [gBb], gB[:, 0:256], lhsT=w_, rhs=h2T[:, k, 512:768],
                  start=(k == 0), stop=(k == 7))
            for k in range(8):
                w_ = ut[:, k, 512 + jj * 128:512 + (jj + 1) * 128]
                V("pe", "matmul", [ub_, h2T_b], [uAb], uA[:, 0:512], lhsT=w_, rhs=h2T[:, k, 0:512],
                  start=(k == 0), stop=(k == 7))
                V("pe", "matmul", [ub_, h2T_b], [gBb], gB[:, 256:512], lhsT=w_, rhs=h2T[:, k, 512:768],
                  start=(k == 0), stop=(k == 7))
            bg = par[:, bgu_o + e * 16 + j:bgu_o + e * 16 + j + 1]
            bu = par[:, bgu_o + e * 16 + 8 + j:bgu_o + e * 16 + 8 + j + 1]
            gsb, gsbb = gsbs[s_]
            tt, ttb = tts[s_]
            ubt, ubb = ubs[s_]
            V("dve", "tensor_scalar", [gAb, par_b], [gsbb], out=gsb[:, 0:512], in0=gA[:, 0:512], scalar1=bg,
              scalar2=SW_LIM, op0=ALU.add, op1=ALU.min)
            V("dve", "tensor_scalar", [gBb, par_b], [gsbb], out=gsb[:, 512:768], in0=gB[:, 0:256], scalar1=bg,
              scalar2=SW_LIM, op0=ALU.add, op1=ALU.min)
            V("act", "activation", [uAb, par_b], [ubb], out=ubt[:, 0:512], in_=uA[:, 0:512], func=AF.Identity,
              bias=bu, scale=1.0)
            V("dve", "tensor_scalar", [gBb, par_b], [ubb], out=ubt[:, 512:768], in0=gB[:, 256:512], scalar1=bu,
              scalar2=None, op0=ALU.add)
            V("act", "activation", [gsbb], [ttb], out=tt, in_=gsb, func=AF.Silu, scale=SW_ALPHA)
            V("dve", "tensor_scalar", [ubb], [ubb], out=ubt, in0=ubt, scalar1=SW_LIM,
              scalar2=-SW_LIM, op0=ALU.min, op1=ALU.max)
            V("dve", "scalar_tensor_tensor", [ubb, ttb], [ttb], out=tt, in0=ubt, scalar=1.0, in1=tt,
              op0=ALU.add, op1=ALU.mult)
            V("dve", "tensor_tensor", [ttb, cm_b], [act_b], out=act_t[:, j, :], in0=tt, in1=cm_t, op=ALU.mult)
            if down_iter is not None:
                for _ in range(4):
                    next(down_iter, None)

    dcount = [0]

    accb = [[Buf("acc_%d_%d" % (dd, bi)) for bi in range(2)] for dd in range(8)]
    for dd in range(8):
        for bi in range(2):
            accb[dd][bi].t.lw = acc_b.t.lw
            accb[dd][bi].t.rd = dict(acc_b.t.rd)
            SL0.tiles.append(accb[dd][bi])

    def expert_down_gen(UD, act):
        ut, ub_ = UD
        act_t, act_b = act
        for bi, (t0, n, cnd) in enumerate(BLK):
            for dd in range(8):
                pt, pb = PS(6 + dcount[0] % 2)
                dcount[0] += 1
                for k in range(8):
                    V("pe", "matmul", [ub_, act_b], [pb], pt[:, 0:n], lhsT=ut[:, k, dd * 128:(dd + 1) * 128],
                      rhs=act_t[:, k, t0:t0 + n], start=(k == 0), stop=(k == 7))
                V("dve", "tensor_tensor", [pb, accb[dd][bi]], [accb[dd][bi]], out=acc[:, dd, t0:t0 + n],
                  in0=pt[:, 0:n], in1=acc[:, dd, t0:t0 + n], op=ALU.add)
                yield

    def expert_down(UD, act):
        for _ in expert_down_gen(UD, act):
            pass

    n_exp = N_EXP
    if dbg and dbg.startswith("moe"):
        n_exp = int(dbg[3:])
    UA = load_unit2(w_gu_d[0], 0, 1024)
    UB = load_unit2(w_gu_d[0], 512, 1536)
    UD = load_unit(w_dn_d[0], 0, 1024)
    prev = None
    for e in range(n_exp):
        cm = make_cmb(e)
        act = acts[e % 2]
        dit = expert_down_gen(*prev) if prev is not None else None
        expert_gu(e, 0, UA, act, cm, dit)
        if dit is not None:
            for _ in dit:
                pass
        if e + 1 < n_exp:
            UA_n = load_unit2(w_gu_d[e + 1], 0, 1024)
            UB_n = load_unit2(w_gu_d[e + 1], 512, 1536)
        expert_gu(e, 1, UB, act, cm)
        prev = (UD, act)
        if e + 1 < n_exp:
            UD = load_unit(w_dn_d[e + 1], 0, 1024)
            UA, UB = UA_n, UB_n
    expert_down(*prev)

    for j in range(8):
        for bi, (t0, n, cnd) in enumerate(BLK):
            V("dve", "scalar_tensor_tensor", [accb[j][bi], x1T_b, mod_b], [x1T_b], out=x1T[:, j, t0:t0 + n],
              in0=acc[:, j, t0:t0 + n], scalar=G2(j, cnd), in1=x1T[:, j, t0:t0 + n], op0=ALU.mult, op1=ALU.add)
    rms_bc(x1T, x1T_b, 1.0 / 1024)
    for j in range(8):
        V("dve", "scalar_tensor_tensor", [x1T_b, rb_b, par_b], [x1T_b], out=x1T[:, j, :], in0=x1T[:, j, :],
          scalar=pc("norm_final")[:, j:j + 1], in1=rb, op0=ALU.mult, op1=ALU.mult)
    SL2.reset()
    ystg = [SL2.carve([128, 1024], F32, dma=True) for _ in range(2)]
    for i in range(6):
        ys, ysb = ystg[i % 2]
        for hb in range(2):
            pt, pb = PS(1 + hb)
            for jj in range(4):
                j = hb * 4 + jj
                V("pe", "transpose", [x1T_b, cst_b], [pb], out=pt[:, jj * 128:(jj + 1) * 128],
                  in_=x1T[:, j, i * 128:(i + 1) * 128], identity=IDENT)
            if hb == 0:
                V("act", "activation", [pb], [ysb], out=ys[:, 0:512], in_=pt[:, :], func=AF.Copy)
            else:
                V("dve", "tensor_copy", [pb], [ysb], out=ys[:, 512:1024], in_=pt[:, :])
        dst = y_out[i * 128:(i + 1) * 128, :]
        P.op("sp", (lambda E, dst=dst, ys=ys: E.dma_start(out=dst, in_=ys)), rd=[ysb], dma=ysb)
        if ysb not in out_bufs:
            out_bufs.append(ysb)
    P.op("sp", None, wr=out_bufs)
    return finish(nc, P, es, sems)


def finish(nc, P, es, sems):
    P.finalize()
    with nc.Block() as block:
        @block.tensor
        def _(E):
            P.emit("pe", E, sems)

        @block.scalar
        def _(E):
            P.emit("act", E, sems)

        @block.vector
        def _(E):
            P.emit("dve", E, sems)

        @block.gpsimd
        def _(E):
            P.emit("pool", E, sems)

        @block.sync
        def _(E):
            P.emit("sp", E, sems)
    es.close()
    return nc


def core_inputs(inp, core, shared):
    b, q = core // 4, core % 4
    xp = np.asarray(inp["x_prompt"], np.float32)
    xs = np.asarray(inp["x_sample"], np.float32)
    xrot = np.roll(xs[b], -256 * q, axis=0)
    x_all = np.ascontiguousarray(np.concatenate([xp[2 * core], xp[2 * core + 1], xrot], axis=0))
    cond = np.stack([_fm(inp["c_ctx"], 8), _fm(np.asarray(inp["c"])[b], 8)], axis=2).reshape(128, 16)
    st0 = np.ascontiguousarray(np.asarray(inp["state_ssm"], np.float32)[b, 0].reshape(4096, 128))
    m = np.zeros((20,), np.float32)
    m[0:8] = 1.0
    m[(8 - 2 * q) % 8] = 0.0
    for s in range(2, 8):
        orig = (s + 2 * q) % 8
        m[8 + (s - 2)] = 1.0 if orig < 2 * q else 0.0
        m[14 + (s - 2)] = 0.0 if orig < 2 * q else 1.0
    d = dict(shared)
    d.update({"x_all": x_all, "cond_fm": np.ascontiguousarray(cond),
              "state0": st0, "masks": np.ascontiguousarray(np.broadcast_to(m[None, :], (128, 20)))})
    return d


def shared_inputs(inp):
    f = lambda a: np.ascontiguousarray(np.asarray(a, np.float32))
    wr = f(inp["w_router"])[0]
    return {
        "params": pack_params(inp), "consts": make_consts(),
        "w_ada": f(inp["w_ada"])[0], "w_in": f(inp["w_in"])[0], "w_ssd_out": f(inp["w_ssd_out"])[0],
        "w_conf_out": f(inp["w_conf_out"])[0], "w_o": f(inp["w_o"])[0],
        "w_router": np.ascontiguousarray(wr.reshape(8, 128, 32).transpose(1, 0, 2).reshape(128, 256)),
        "w_gu": f(inp["w_gu"])[0], "w_down": f(inp["w_down"])[0], "b_down": f(inp["b_down"])[0],
    }


def kernel(**inp):
    nc = build()
    shared = shared_inputs(inp)
    in_maps = [core_inputs(inp, c, shared) for c in range(8)]
    res = run_bass_kernel_spmd(nc, in_maps, core_ids=list(range(8)))
    y_prompt = np.zeros((16, 256, 1024), np.float32)
    y_sample = np.zeros((2, 1024, 1024), np.float32)
    new_state = np.zeros((16, 1, 2, 32, 64, 128), np.float32)
    for c in range(8):
        r = res.results[c]
        b, q = c // 4, c % 4
        y = r["y_own"]
        y_prompt[2 * c] = y[0:256]
        y_prompt[2 * c + 1] = y[256:512]
        y_sample[b, 256 * q:256 * q + 256] = y[512:768]
        ns = r["new_state"].reshape(2, 2, 32, 64, 128)
        new_state[2 * c, 0] = ns[0]
        new_state[2 * c + 1, 0] = ns[1]
    return (y_prompt, y_sample, new_state)
```

```python
import numpy as np
from contextlib import ExitStack
import concourse.bass as bass
import concourse.mybir as mybir
from concourse.bass_utils import run_bass_kernel_spmd

F32, BF16 = mybir.dt.float32, mybir.dt.bfloat16
AF = mybir.ActivationFunctionType
ALU = mybir.AluOpType
AX = mybir.AxisListType

T = 768
A = 1536
EPS = 1e-6
NU = 5
SAME_SYNC = True
N_EXP = 32
DBG = None


def _par_layout():
    off = {}
    n = 0
    def add(name, w):
        nonlocal n
        off[name] = (n, w)
        n += w
    add("norm_mix", 8); add("norm_ffn", 8); add("norm_final", 8); add("b_ada", 48)
    add("conv_w", 32 * 5); add("conv_b", 32); add("ssm_norm_w", 16)
    add("cdw_w", 8 * 31); add("cdw_b", 8); add("ln_w", 8); add("ln_b", 8); add("b_conf_out", 8)
    add("b_gate", 16); add("b_gu", 32 * 16)
    add("dt_bias", 64); add("a_log", 64); add("d_skip", 32); add("b_router", 32)
    return off, n

PAR, NPAR = _par_layout()


def _fm(v, ntile):
    return np.ascontiguousarray(np.asarray(v, np.float32).reshape(ntile, 128).T)


def pack_params(inp):
    P = np.zeros((128, NPAR), np.float32)
    def put(name, arr):
        o, w = PAR[name]
        assert arr.shape == (128, w), (name, arr.shape, w)
        P[:, o:o + w] = arr
    put("norm_mix", _fm(inp["norm_mix"][0], 8)); put("norm_ffn", _fm(inp["norm_ffn"][0], 8))
    put("norm_final", _fm(inp["norm_final"], 8)); put("b_ada", _fm(inp["b_ada"][0], 48))
    cw = np.asarray(inp["ssm_conv_w"][0], np.float32)
    put("conv_w", np.ascontiguousarray(cw.reshape(5, 32, 128).transpose(2, 1, 0).reshape(128, 160)))
    put("conv_b", _fm(inp["ssm_conv_b"][0], 32)); put("ssm_norm_w", _fm(inp["ssm_norm_w"][0], 16))
    dw = np.asarray(inp["conf_dw_w"][0], np.float32)
    put("cdw_w", np.ascontiguousarray(dw.reshape(31, 8, 128).transpose(2, 1, 0).reshape(128, 248)))
    put("cdw_b", _fm(inp["conf_dw_b"][0], 8)); put("ln_w", _fm(inp["conf_ln_w"][0], 8))
    put("ln_b", _fm(inp["conf_ln_b"][0], 8)); put("b_conf_out", _fm(inp["b_conf_out"][0], 8))
    put("b_gate", _fm(inp["b_gate"][0], 16))
    bg = np.asarray(inp["b_gu"][0], np.float32)
    put("b_gu", np.ascontiguousarray(bg.reshape(32, 16, 128).transpose(2, 0, 1).reshape(128, 512)))
    put("dt_bias", np.broadcast_to(np.asarray(inp["dt_bias"][0], np.float32).reshape(1, 64), (128, 64)))
    put("a_log", np.broadcast_to(np.asarray(inp["a_log"][0], np.float32).reshape(1, 64), (128, 64)))
    put("d_skip", np.broadcast_to(np.asarray(inp["d_skip"][0], np.float32).reshape(1, 32), (128, 32)))
    put("b_router", np.broadcast_to(np.asarray(inp["b_router"][0], np.float32).reshape(1, 32), (128, 32)))
    return P


def make_consts():
    k = np.arange(128)[:, None]
    s = np.arange(128)[None, :]
    C = np.zeros((128, 8, 128), np.float32)
    C[:, 0] = (k == s); C[:, 1] = (k <= s); C[:, 2] = (k >= s); C[:, 3] = (k > s); C[:, 4] = (k < s)
    C[:, 5] = 1.0
    C[:, 6] = (s < 64); C[:, 7] = (s >= 64)
    return C.reshape(128, 1024)


class _Trk:
    __slots__ = ("lw", "rd")
    def __init__(self):
        self.lw = None
        self.rd = {}


class Buf:
    def __init__(self, name, share=None):
        self.name = name
        self.t = share.t if share is not None else _Trk()
        self.dsem = None
        self.dn = 0


ENGS = ["pe", "act", "dve", "pool", "sp"]


class Prog:
    def __init__(self):
        self.ops = {e: [] for e in ENGS}

    def op(self, eng, fn, rd=(), wr=(), dma=None):
        deps = set()
        for b in rd:
            if b.t.lw is not None:
                deps.add(b.t.lw)
        for b in wr:
            if b.t.lw is not None:
                deps.add(b.t.lw)
            deps.update(b.t.rd.values())
        idx = len(self.ops[eng])
        if dma is not None:
            dma.dn += 1
            tok = ("dma", dma, dma.dn)
            key = ("dma", id(dma))
        else:
            tok = (eng, idx)
            key = eng
        for b in rd:
            if getattr(b, "is_psum", False) and eng != "pe":
                others = [k for k in b.t.rd if isinstance(k, str) and k not in ("pe", eng)]
                if others:
                    raise AssertionError("PSUM bank %s read by %s and %s between writes" % (b.name, eng, others))
            b.t.rd[key] = tok
        for b in wr:
            b.t.lw = tok
            b.t.rd = {}
        self.ops[eng].append((fn, deps, tok, dma))

    def finalize(self):
        needed = set()
        for e in ENGS:
            for fn, deps, tok, dma in self.ops[e]:
                for d in deps:
                    if d[0] != "dma":
                        if d[0] == e and (e == "pe" or not SAME_SYNC):
                            continue
                        needed.add(d)
        self.needed = needed
        self.sigval = {}
        for e in ENGS:
            c = 0
            for i, (fn, deps, tok, dma) in enumerate(self.ops[e]):
                if tok in needed:
                    c += 1
                    self.sigval[tok] = c

    def emit(self, e, E, sems):
        known = {}
        for fn, deps, tok, dma in self.ops[e]:
            waits = {}
            for d in deps:
                if d[0] == "dma":
                    sem, val = d[1].dsem, 16 * d[2]
                else:
                    if d[0] == e and (e == "pe" or not SAME_SYNC):
                        continue
                    sem, val = sems[d[0]], self.sigval[d]
                k = id(sem)
                if known.get(k, 0) >= val:
                    continue
                if k not in waits or waits[k][1] < val:
                    waits[k] = (sem, val)
            for k, (sem, val) in waits.items():
                E.wait_ge(sem, val)
                known[k] = val
            if fn is None:
                continue
            ins = fn(E)
            if dma is not None:
                ins.then_inc(dma.dsem, 16)
            elif tok in self.needed:
                ins.then_inc(sems[e], 1)


SW_ALPHA = 1.702
SW_LIM = 7.0
BLK = ((0, 512, 0), (512, 256, 1))


def build(dbg=None):
    nc = bass.Bass("TRN2", target_bir_lowering=False)
    P = Prog()
    es = ExitStack()

    def dram(name, shape, kind="ExternalInput", dt=F32):
        return nc.dram_tensor(name, list(shape), dt, kind=kind).ap()

    x_all = dram("x_all", [A, 1024])
    cond_d = dram("cond_fm", [128, 16])
    st0_d = dram("state0", [4096, 128])
    masks_d = dram("masks", [128, 20])
    params_d = dram("params", [128, NPAR])
    consts_d = dram("consts", [128, 1024])
    w_ada_d = dram("w_ada", [1024, 6144])
    w_in_d = dram("w_in", [1024, 10304])
    w_ssd_d = dram("w_ssd_out", [2048, 1024])
    w_conf_d = dram("w_conf_out", [1024, 1024])
    w_o_d = dram("w_o", [1024, 1024])
    w_rt_d = dram("w_router", [128, 256])
    w_gu_d = dram("w_gu", [N_EXP, 1024, 2048])
    w_dn_d = dram("w_down", [N_EXP, 1024, 1024])
    b_dn_d = dram("b_down", [N_EXP, 1024])
    y_out = dram("y_own", [T, 1024], kind="ExternalOutput")
    ns_out = dram("new_state", [2 * 2 * 2048, 128], kind="ExternalOutput")
    dbg_out = dram("dbg", [128, 8192], kind="ExternalOutput") if dbg else None

    def newsem(name):
        return es.enter_context(nc.semaphore(name))

    def sb(name, shape, dt=F32, dma=False):
        t = es.enter_context(nc.sbuf_tensor(name, list(shape), dt))
        b = Buf(name)
        if dma:
            b.dsem = newsem("d_" + name)
        return t, b

    sems = {e: newsem("s_" + e) for e in ENGS}

    def V(eng, method, rd, wr, *args, **kw):
        P.op(eng, lambda E: getattr(E, method)(*args, **kw), rd=rd, wr=wr)

    class Slab:
        def __init__(self, name, words):
            self.name = name
            self.words = words
            self.t = es.enter_context(nc.sbuf_tensor(name, [128, words], F32))
            self.ptr = 0
            self.tiles = []
            self.haz = {}
            self.n = 0

        def reset(self):
            for b in self.tiles:
                if b.t.lw is not None:
                    self.haz[("lw", id(b))] = b.t.lw
                for k, v in b.t.rd.items():
                    if k in self.haz and isinstance(k, str):
                        if self.haz[k][1] < v[1]:
                            self.haz[k] = v
                    else:
                        self.haz[k] = v
            self.tiles = []
            self.ptr = 0

        def carve(self, shape, dt=F32, dma=False):
            per = 1
            for d in shape[1:]:
                per *= d
            words = per if dt == F32 else (per + 1) // 2
            assert self.ptr + words <= self.words, (self.name, self.ptr, words, self.words)
            ap = self.t[:, self.ptr:self.ptr + words]
            self.ptr += words
            if dt != F32:
                ap = ap.bitcast(dt)
            if len(shape) == 3:
                ap = ap.rearrange("p (a b) -> p a b", a=shape[1])
            elif len(shape) == 4:
                ap = ap.rearrange("p (a b c) -> p a b c", a=shape[1], b=shape[2])
            self.n += 1
            b = Buf("%s_%d" % (self.name, self.n))
            b.t.rd = dict(self.haz)
            if dma:
                b.dsem = newsem("d_%s_%d" % (self.name, self.n))
            self.tiles.append(b)
            return ap, b

    cst, cst_b = sb("cst", [128, 8, 128], F32, dma=True)
    cstb, cstb_b = sb("cstb", [128, 8, 128], BF16)
    par, par_b = sb("par", [128, NPAR], F32, dma=True)
    msk, msk_b = sb("msk", [128, 20], F32, dma=True)
    small, small_b = sb("small", [128, 8], F32)
    negm_t, negm_b = sb("negm", [128, 2, 128], BF16)
    negm = negm_t[:]
    NUr = 4
    ring = [sb("ring%d" % i, [128, 8, 1024], BF16, dma=True) for i in range(NUr)]
    ring_i = [0]
    SL0 = Slab("SL0", 6144)
    SL1 = Slab("SL1", 6144)
    SL2 = Slab("SL2", 6144)
    SL3 = Slab("SL3", 6144)
    SLT = Slab("SLT", 7680)

    IDENT, TRI_LE, TRI_GE, TRI_GT, TRI_LT, ONES, HLO, HHI = [cst[:, i, :] for i in range(8)]
    IDENTB = cstb[:, 0, :]
    ONESB = cstb[:, 5, :]
    EPSC = small[:, 0:1]
    ONEC = small[:, 1:2]

    def pc(name, a=0, b=None):
        o, w = PAR[name]
        if b is None:
            b = w
        return par[:, o + a:o + b]

    psum = []
    for i in range(8):
        t = es.enter_context(nc.psum_tensor("ps%d" % i, [128, 512], F32))
        psum.append((t, Buf("ps%d" % i)))
        psum[-1][1].is_psum = True

    def PS(i):
        return psum[i]

    def next_ring():
        i = ring_i[0] % NUr
        ring_i[0] += 1
        return ring[i]

    def load_unit(w2d, c0, ncols, k0=0, nk=8):
        t, b = next_ring()
        src = w2d[k0 * 128:(k0 + nk) * 128, c0:c0 + ncols].rearrange("(kc p) c -> p kc c", p=128)
        P.op("pool", lambda E: E.dma_start(out=t[:, 0:nk, 0:ncols], in_=src), wr=[b], dma=b)
        return t, b

    def load_unit2(w2d, c0, c1, n=512):
        t, b = next_ring()
        for idx, cc in enumerate((c0, c1)):
            src = w2d[:, cc:cc + n].rearrange("(kc p) c -> p kc c", p=128)
            dst = t[:, :, idx * n:(idx + 1) * n]
            if idx == 0:
                P.op("pool", (lambda E, dst=dst, src=src: E.dma_start(out=dst, in_=src)), wr=[b], dma=b)
            else:
                P.op("pool", (lambda E, dst=dst, src=src: E.dma_start(out=dst, in_=src)), dma=b)
                b.t.lw = P.ops["pool"][-1][2]
        return t, b

    def dma_in(tile_ap, b, src, eng="sp"):
        P.op(eng, lambda E: E.dma_start(out=tile_ap, in_=src), wr=[b], dma=b)

    dbg_b = Buf("dbgout")
    if dbg:
        dbg_b.dsem = newsem("d_dbg")
    dbg_col = [0]

    def dump(ap2d, b, n):
        c0 = dbg_col[0]
        dbg_col[0] += n
        dst = dbg_out[:, c0:c0 + n]
        P.op("pool", lambda E: E.dma_start(out=dst, in_=ap2d), rd=[b], dma=dbg_b)
        return c0

    def end_dbg():
        P.op("sp", None, wr=[dbg_b])
        return finish(nc, P, es, sems)

    def rsqrt_from(out_ap, out_b, in_ap, in_b, scale, eng_rd=()):
        V("act", "activation", [in_b, small_b] + list(eng_rd), [out_b], out=out_ap, in_=in_ap, func=AF.Ln,
          scale=scale, bias=EPSC)
        V("act", "activation", [out_b], [out_b], out=out_ap, in_=out_ap, func=AF.Exp, scale=-0.5)

    dma_in(cst[:], cst_b, consts_d.rearrange("p (a b) -> p a b", a=8))
    dma_in(par[:], par_b, params_d)
    dma_in(msk[:], msk_b, masks_d)
    V("dve", "tensor_copy", [cst_b], [cstb_b], out=cstb[:], in_=cst[:])
    V("dve", "memset", [], [small_b], small[:, 0:1], EPS)
    V("dve", "memset", [], [small_b], small[:, 1:2], 1.0)

    cond, cond_b = sb("cond", [128, 8, 2], F32, dma=True)
    scond, scond_b = sb("scond", [128, 8, 2], BF16)
    mod, mod_b = sb("mod", [128, 48, 2], F32)
    modA, modA_b = sb("modA", [128, 2, 8, 2], F32)
    dma_in(cond[:], cond_b, cond_d.rearrange("p (j c) -> p j c", c=2))
    V("act", "activation", [cond_b], [scond_b], out=scond[:], in_=cond[:], func=AF.Silu)
    mod1_b = Buf("mod1")
    modA1_b = Buf("modA1")

    def ada_units(u0, u1, bank, mb_, which_list, mab_):
        pt, pb = PS(bank)
        for u in range(u0, u1):
            ut, ub = load_unit(w_ada_d, u * 1024, 1024)
            for jt in range(8):
                col = (u * 8 + jt) * 2
                for k in range(8):
                    V("pe", "matmul", [ub, scond_b], [pb], pt[:, col:col + 2],
                      lhsT=ut[:, k, jt * 128:(jt + 1) * 128], rhs=scond[:, k, :], start=(k == 0), stop=(k == 7))
        j0, j1 = u0 * 8, u1 * 8
        V("dve", "tensor_tensor", [pb, par_b], [mb_], out=mod[:, j0:j1, :],
          in0=pt[:, 2 * j0:2 * j1].rearrange("p (j c) -> p j c", c=2),
          in1=pc("b_ada", j0, j1).unsqueeze(2).to_broadcast([128, j1 - j0, 2]), op=ALU.add)
        for which, nm, jj0 in which_list:
            V("dve", "tensor_scalar", [mb_], [mab_], out=modA[:, which], in0=mod[:, jj0:jj0 + 8, :],
              scalar1=1.0, scalar2=None, op0=ALU.add)
            V("dve", "tensor_tensor", [mab_, par_b], [mab_], out=modA[:, which], in0=modA[:, which],
              in1=pc(nm).unsqueeze(2).to_broadcast([128, 8, 2]), op=ALU.mult)

    ada_units(0, 2, 0, mod1_b, [(0, "norm_mix", 8)], modA1_b)

    def A1(j, c): return modA[:, 0, j, c:c + 1]
    def S1(j, c): return mod[:, j, c:c + 1]
    def G1(j, c): return mod[:, 16 + j, c:c + 1]
    def A2(j, c): return modA[:, 1, j, c:c + 1]
    def S2(j, c): return mod[:, 24 + j, c:c + 1]
    def G2(j, c): return mod[:, 40 + j, c:c + 1]

    hT, hT_all = SL0.carve([128, 8, A], BF16)
    hTb = [Buf("hT_c%d" % i) for i in range(12)]
    for b_ in hTb:
        SL0.tiles.append(b_)
    xt = [SL2.carve([128, 1024], F32, dma=True) for _ in range(2)]
    xn = [SL2.carve([128, 1024], F32) for _ in range(2)]
    junk, junk_b = SL2.carve([128, 1024], BF16)
    ssq, ssq_b = sb("ssq", [128, 12], F32)
    rstd, rstd_b = sb("rstd", [128, 12], F32)
    V("dve", "memset", [], [ssq_b], ssq[:], 0.0)

    def transpose_x_tile(xnt, xnb, evac):
        for hb in range(2):
            pt, pb = PS(1 + hb)
            for jj in range(4):
                j = hb * 4 + jj
                V("pe", "transpose", [xnb, cst_b], [pb], out=pt[:, jj * 128:(jj + 1) * 128],
                  in_=xnt[:, j * 128:(j + 1) * 128], identity=IDENT)
            for jj in range(4):
                evac(hb * 4 + jj, pt[:, jj * 128:(jj + 1) * 128], pb)

    for i in range(12):
        xtt, xtb = xt[i % 2]
        xnt, xnb = xn[i % 2]
        cnd = 0 if i < 4 else 1
        dma_in(xtt, xtb, x_all[i * 128:(i + 1) * 128, :])
        V("act", "activation", [xtb], [junk_b, ssq_b], out=junk, in_=xtt, func=AF.Square,
          accum_out=ssq[:, i:i + 1])
        rsqrt_from(rstd[:, i:i + 1], rstd_b, ssq[:, i:i + 1], ssq_b, 1.0 / 1024)
        V("act", "activation", [xtb, rstd_b], [xnb], out=xnt, in_=xtt, func=AF.Copy, scale=rstd[:, i:i + 1])

        def evac(j, pap, pb, i=i, cnd=cnd):
            dst = hT[:, j, i * 128:(i + 1) * 128]
            if j < 4:
                V("dve", "tensor_scalar", [pb, modA1_b, mod1_b], [hTb[i]], out=dst, in0=pap,
                  scalar1=A1(j, cnd), scalar2=S1(j, cnd), op0=ALU.mult, op1=ALU.add)
            else:
                V("act", "activation", [pb, modA1_b, mod1_b], [hTb[i]], out=dst, in_=pap, func=AF.Identity,
                  scale=A1(j, cnd), bias=S1(j, cnd))
        transpose_x_tile(xnt, xnb, evac)

    ada_units(2, 6, 7, mod_b, [(1, "norm_ffn", 32)], modA_b)

    if dbg == "hT":
        dump(hT[:, 0, :], hT_all, 1536) if False else None
        for b_ in hTb:
            pass
        P.op("pool", lambda E: E.dma_start(out=dbg_out[:, 0:1536], in_=hT[:, 0, :]), rd=hTb, dma=dbg_b)
        P.op("pool", lambda E: E.dma_start(out=dbg_out[:, 1536:3072], in_=hT[:, 7, :]), rd=hTb, dma=dbg_b)
        P.op("pool", lambda E: E.dma_start(out=dbg_out[:, 3072:3168], in_=mod[:].rearrange("p j c -> p (j c)")),
             rd=[mod_b], dma=dbg_b)
        return end_dbg()

    SL2.reset()
    dtv, dtv_b = SL3.carve([128, 12, 64])
    raw, raw_b = SL3.carve([128, 12, 192])
    Eown, Eown_b = SL3.carve([128, 6, 192])
    wdec, wdec_b = SL3.carve([128, 6, 64])
    wrest, wrest_b = SL3.carve([128, 6, 64])
    lw, lw_b = SL3.carve([128, 6, 64])
    wfin, wfin_b = SL3.carve([128, 4, 32])
    ptot, ptot_b = SL3.carve([128, 64])
    aneg, aneg_b = SL3.carve([128, 64])
    negcs_t, negcs_b = sb("negcs", [128, 6, 64], F32)
    negcs = negcs_t[:]
    cs_hi, cs_hi_b = SL3.carve([128, 6, 64], BF16)
    cs_lo, cs_lo_b = SL3.carve([128, 6, 64], BF16)
    adt, adt_b = SLT.carve([128, 12, 64])
    t64, t64_b = SLT.carve([128, 12, 64])
    lpu, lpu_b = SLT.carve([128, 12, 64])
    lpl, lpl_b = SLT.carve([128, 12, 64])

    dtu, dtu_b = load_unit(w_in_d, 6144, 64)
    for i in range(12):
        pt, pb = PS(3 + (i // 8))
        col = (i % 8) * 64
        for k in range(8):
            V("pe", "matmul", [hTb[i], dtu_b], [pb], pt[:, col:col + 64], lhsT=hT[:, k, i * 128:(i + 1) * 128],
              rhs=dtu[:, k, 0:64], start=(k == 0), stop=(k == 7))
    V("dve", "tensor_tensor", [PS(3)[1], par_b], [dtv_b], out=dtv[:, 0:8, :],
      in0=PS(3)[0][:, 0:512].rearrange("p (i c) -> p i c", c=64),
      in1=pc("dt_bias").unsqueeze(1).to_broadcast([128, 8, 64]), op=ALU.add)
    V("dve", "tensor_tensor", [PS(4)[1], par_b], [dtv_b], out=dtv[:, 8:12, :],
      in0=PS(4)[0][:, 0:256].rearrange("p (i c) -> p i c", c=64),
      in1=pc("dt_bias").unsqueeze(1).to_broadcast([128, 4, 64]), op=ALU.add)
    V("act", "activation", [dtv_b], [t64_b], out=t64, in_=dtv, func=AF.Abs)
    V("act", "activation", [t64_b], [t64_b], out=t64, in_=t64, func=AF.Exp, scale=-1.0)
    V("dve", "tensor_scalar", [t64_b], [lpu_b], out=lpu, in0=t64, scalar1=1.0, scalar2=None, op0=ALU.add)
    V("act", "activation", [lpu_b], [lpl_b], out=lpl, in_=lpu, func=AF.Ln)
    V("dve", "tensor_scalar", [lpu_b], [lpu_b], out=lpu, in0=lpu, scalar1=-1.0, scalar2=1e-30, op0=ALU.add,
      op1=ALU.add)
    V("dve", "reciprocal", [lpu_b], [lpu_b], out=lpu, in_=lpu)
    V("dve", "scalar_tensor_tensor", [lpl_b, lpu_b], [lpl_b], out=lpl, in0=lpl, scalar=1e-30, in1=lpu,
      op0=ALU.add, op1=ALU.mult)
    V("dve", "tensor_tensor", [t64_b, lpl_b], [t64_b], out=t64, in0=t64, in1=lpl, op=ALU.mult)
    V("dve", "scalar_tensor_tensor", [dtv_b, t64_b], [dtv_b], out=dtv, in0=dtv, scalar=0.0, in1=t64,
      op0=ALU.max, op1=ALU.add)
    if dbg == "dt1":
        dump(dtv.rearrange("p a b -> p (a b)"), dtv_b, 768)
        return end_dbg()
    V("dve", "tensor_tensor", [dtv_b, msk_b], [dtv_b], out=dtv[:, 6:12, 0:32], in0=dtv[:, 6:12, 0:32],
      in1=msk[:, 8:14].unsqueeze(2).to_broadcast([128, 6, 32]), op=ALU.mult)
    V("dve", "tensor_tensor", [dtv_b, msk_b], [dtv_b], out=dtv[:, 6:12, 32:64], in0=dtv[:, 6:12, 32:64],
      in1=msk[:, 14:20].unsqueeze(2).to_broadcast([128, 6, 32]), op=ALU.mult)
    V("act", "activation", [par_b], [aneg_b], out=aneg, in_=pc("a_log"), func=AF.Exp)
    V("dve", "scalar_tensor_tensor", [dtv_b, aneg_b], [adt_b], out=adt, in0=dtv, scalar=-1.0,
      in1=aneg.unsqueeze(1).to_broadcast([128, 12, 64]), op0=ALU.mult, op1=ALU.mult)
    adt_hi, adt_hi_b = SLT.carve([128, 12, 64], BF16)
    adt_lo, adt_lo_b = SLT.carve([128, 12, 64], BF16)
    V("dve", "tensor_copy", [adt_b], [adt_hi_b], out=adt_hi, in_=adt)
    V("dve", "tensor_tensor", [adt_b, adt_hi_b], [adt_lo_b], out=adt_lo, in0=adt, in1=adt_hi, op=ALU.subtract)
    if dbg == "dt2":
        dump(adt.rearrange("p a b -> p (a b)"), adt_b, 768)
        dump(adt_lo.rearrange("p a b -> p (a b)"), adt_lo_b, 768)
        return end_dbg()
    for i in range(12):
        pt, pb = PS(5 + i % 2)
        for (c0, c1, tri, a0, a1) in ((0, 32, 1, 0, 32), (32, 64, 2, 32, 64), (64, 96, 3, 0, 32),
                                      (96, 128, 4, 32, 64), (128, 192, 5, 0, 64)):
            V("pe", "matmul", [adt_hi_b, cstb_b], [pb], pt[:, c0:c1], lhsT=cstb[:, tri, :],
              rhs=adt_hi[:, i, a0:a1], start=True, stop=False)
            V("pe", "matmul", [adt_lo_b, cstb_b], [pb], pt[:, c0:c1], lhsT=cstb[:, tri, :],
              rhs=adt_lo[:, i, a0:a1], start=False, stop=True)
        V("dve", "tensor_copy", [pb], [raw_b], out=raw[:, i, :], in_=pt[:, 0:192])
        if i < 6:
            V("act", "activation", [raw_b], [Eown_b], out=Eown[:, i, :], in_=raw[:, i, :], func=AF.Exp)
    if dbg == "dt3":
        dump(raw.rearrange("p a b -> p (a b)"), raw_b, 2304)
        dump(Eown.rearrange("p a b -> p (a b)"), Eown_b, 1152)
        return end_dbg()
    V("dve", "tensor_tensor", [dtv_b, Eown_b], [wdec_b], out=wdec, in0=dtv[:, 0:6, :], in1=Eown[:, :, 64:128],
      op=ALU.mult)
    V("dve", "tensor_scalar", [raw_b], [negcs_b], out=negcs, in0=raw[:, 0:6, 0:64], scalar1=-1.0, scalar2=None,
      op0=ALU.mult)
    V("dve", "tensor_scalar", [cst_b], [negm_b], out=negm, in0=cst[:, 3:5, :], scalar1=-65536.0, scalar2=None,
      op0=ALU.mult)
    V("dve", "tensor_copy", [raw_b], [cs_hi_b], out=cs_hi, in_=raw[:, 0:6, 0:64])
    V("dve", "tensor_tensor", [raw_b, cs_hi_b], [cs_lo_b], out=cs_lo, in0=raw[:, 0:6, 0:64], in1=cs_hi,
      op=ALU.subtract)
    V("dve", "memset", [], [lw_b], lw, 0.0)
    for i in range(10, 5, -1):
        V("dve", "tensor_tensor", [lw_b, raw_b], [lw_b], out=lw[:, i - 6, 0:32], in0=lw[:, i - 5, 0:32],
          in1=raw[:, i + 1, 128:160], op=ALU.add)
    for i in range(7, 12):
        V("dve", "tensor_tensor", [lw_b, raw_b], [lw_b], out=lw[:, i - 6, 32:64], in0=lw[:, i - 7, 32:64],
          in1=raw[:, i - 1, 160:192], op=ALU.add)
    V("dve", "tensor_tensor", [lw_b, raw_b], [ptot_b], out=ptot[:, 0:32], in0=lw[:, 0, 0:32],
      in1=raw[:, 6, 128:160], op=ALU.add)
    V("dve", "tensor_tensor", [lw_b, raw_b], [ptot_b], out=ptot[:, 32:64], in0=lw[:, 5, 32:64],
      in1=raw[:, 11, 160:192], op=ALU.add)
    V("act", "activation", [ptot_b], [ptot_b], out=ptot, in_=ptot, func=AF.Exp)
    V("dve", "tensor_tensor", [lw_b, raw_b], [wrest_b], out=wrest, in0=lw, in1=raw[:, 6:12, 64:128], op=ALU.add)
    V("act", "activation", [wrest_b], [wrest_b], out=wrest, in_=wrest, func=AF.Exp)
    V("dve", "tensor_tensor", [wrest_b, dtv_b], [wrest_b], out=wrest, in0=wrest, in1=dtv[:, 6:12, :], op=ALU.mult)
    for s in range(2):
        c0, c1 = 2 * s, 2 * s + 1
        V("dve", "tensor_tensor", [wdec_b, Eown_b], [wfin_b], out=wfin[:, 2 * s, :], in0=wdec[:, c0, 0:32],
          in1=Eown[:, c1, 128:160], op=ALU.mult)
        V("dve", "tensor_tensor", [wdec_b, Eown_b], [wfin_b], out=wfin[:, 2 * s + 1, :], in0=wdec[:, c1, 32:64],
          in1=Eown[:, c0, 160:192], op=ALU.mult)

    if dbg == "dt":
        dump(dtv.rearrange("p a b -> p (a b)"), dtv_b, 768)
        dump(raw.rearrange("p a b -> p (a b)"), raw_b, 2304)
        dump(wdec.rearrange("p a b -> p (a b)"), wdec_b, 384)
        dump(wrest.rearrange("p a b -> p (a b)"), wrest_b, 384)
        dump(ptot, ptot_b, 64)
        dump(wfin.rearrange("p a b -> p (a b)"), wfin_b, 128)
        return end_dbg()

    yT, yT_b = SL1.carve([128, 16, T], BF16)
    yTb = [Buf("yT_p%d" % p) for p in range(4)]
    for b_ in yTb:
        SL1.tiles.append(b_)
    st0v = st0_d.rearrange("(d g t p) n -> g p d t n", d=2, g=8, t=2, p=128)
    stg_i = [0]
    out_bufs = []

    for p in range(4):
        SL2.reset()
        SLT.reset()
        x_tok, x_tok_b = SL2.carve([128, 6, 512], BF16)
        B_tok, B_tok_b = SL2.carve([128, 6, 256], BF16)
        BT, BT_b = SL2.carve([128, 2, T], BF16)
        CT, CT_b = SL2.carve([128, 2, T], BF16)
        H, H_b = SL2.carve([128, 2, 512], F32)
        ents = [SL2.carve([128, 512], BF16) for _ in range(4)]
        pres = [SLT.carve([128, 1576], BF16) for _ in range(2)]
        fms = [SLT.carve([128, A], BF16) for _ in range(2)]
        pc_i = [0]
        diags = [SLT.carve([128, 5, 128], BF16) for _ in range(2)]
        dg_i = [0]
        x_rest, x_rest_b = SLT.carve([128, 6, 256], BF16)
        B_rest, B_rest_b = SLT.carve([128, 6, 256], BF16)
        xsr = [SLT.carve([128, 256], BF16) for _ in range(4)]
        h0t, h0t_b = SLT.carve([128, 2, 2, 128], F32, dma=True)
        htmp, htmp_b = SLT.carve([128, 512], F32)
        for (pre_, pre_b_) in pres:
            preP_ = pre_[:, 0:520].rearrange("p (s t) -> p s t", s=2)
            V("dve", "memset", [], [pre_b_], preP_[:, :, 0:2], 0.0)
            V("dve", "memset", [], [pre_b_], preP_[:, :, 258:260], 0.0)

        xu = load_unit(w_in_d, 2048 + 512 * p, 512)
        bu = load_unit(w_in_d, 4096 + 256 * p, 256)
        cu = load_unit(w_in_d, 5120 + 256 * p, 256)

        def proj_conv(unit, jt, cidx, sil):
            ut, ub = unit
            pre, pre_b = pres[pc_i[0] % 2]
            fm, fm_b = fms[pc_i[0] % 2]
            pc_i[0] += 1
            preP = pre[:, 0:520].rearrange("p (s t) -> p s t", s=2)
            preS = pre[:, 520:1576].rearrange("p (s t) -> p s t", s=8)
            for tb in range(3):
                pt, pb = PS((0, 1, 5)[tb])
                for k in range(8):
                    V("pe", "matmul", [ub] + hTb[4 * tb:4 * tb + 4], [pb], pt[:, :],
                      lhsT=ut[:, k, jt * 128:(jt + 1) * 128], rhs=hT[:, k, tb * 512:(tb + 1) * 512],
                      start=(k == 0), stop=(k == 7))
                if tb == 0:
                    dst = preP[:, :, 2:258]
                    src = pt[:, :].rearrange("p (s t) -> p s t", s=2)
                else:
                    dst = preS[:, 4 * (tb - 1):4 * tb, 2:130]
                    src = pt[:, :].rearrange("p (s t) -> p s t", s=4)
                V("act", "activation", [pb], [pre_b], out=dst, in_=src, func=AF.Copy)
            V("dve", "tensor_tensor", [pre_b, msk_b], [pre_b], out=preS[:, 1:8, 0:2], in0=preS[:, 0:7, 128:130],
              in1=msk[:, 1:8].unsqueeze(2).to_broadcast([128, 7, 2]), op=ALU.mult)
            V("dve", "tensor_scalar", [pre_b, msk_b], [pre_b], out=preS[:, 0, 0:2], in0=preS[:, 7, 128:130],
              scalar1=msk[:, 0:1], scalar2=None, op0=ALU.mult)
            V("dve", "tensor_tensor", [pre_b, msk_b], [pre_b], out=preS[:, 0:7, 130:132], in0=preS[:, 1:8, 2:4],
              in1=msk[:, 1:8].unsqueeze(2).to_broadcast([128, 7, 2]), op=ALU.mult)
            V("dve", "tensor_scalar", [pre_b, msk_b], [pre_b], out=preS[:, 7, 130:132], in0=preS[:, 0, 2:4],
              scalar1=msk[:, 0:1], scalar2=None, op0=ALU.mult)
            o, _w = PAR["conv_w"]
            dg, dgb = diags[dg_i[0] % 2]
            dg_i[0] += 1
            for k in range(5):
                V("dve", "tensor_scalar", [cstb_b, par_b], [dgb], out=dg[:, k, :], in0=IDENTB,
                  scalar1=par[:, o + cidx * 5 + k:o + cidx * 5 + k + 1], scalar2=None, op0=ALU.mult)
            for tb in range(3):
                pt, pb = PS((6, 7, 4)[tb])
                for k in range(5):
                    if tb == 0:
                        rhs = preP[:, :, k:k + 256]
                    else:
                        rhs = preS[:, 4 * (tb - 1):4 * tb, k:k + 128]
                    V("pe", "matmul", [pre_b, dgb], [pb], pt[:, :], lhsT=dg[:, k, :], rhs=rhs,
                      start=(k == 0), stop=(k == 4))
                sil(tb, pt, pb, fm, fm_b)
            return fm, fm_b

        def transposes_to(fm, fm_b, dst_own, dst_own_b, dst_rest, dst_rest_b):
            for half, (dst, dstb) in enumerate(((dst_own, dst_own_b), (dst_rest, dst_rest_b))):
                pt, pb = PS(2 + half)
                ptb = pt[:, :].bitcast(BF16)
                for cc in range(6):
                    c = half * 6 + cc
                    V("pe", "transpose", [fm_b, cstb_b], [pb], out=ptb[:, cc * 128:(cc + 1) * 128],
                      in_=fm[:, c * 128:(c + 1) * 128], identity=IDENTB)
                V("act", "activation", [pb], [dstb], out=dst,
                  in_=ptb[:, 0:768].rearrange("p (c n) -> p c n", c=6), func=AF.Copy)

        cb_o = PAR["conv_b"][0]
        for gl in range(2):
            cidx = 16 + 2 * p + gl
            def sil(tb, pt, pb, fm, fm_b, cidx=cidx):
                V("act", "activation", [pb, par_b], [fm_b], out=fm[:, tb * 512:(tb + 1) * 512], in_=pt[:, :],
                  func=AF.Silu, bias=par[:, cb_o + cidx:cb_o + cidx + 1], scale=1.0)
            fm, fm_b = proj_conv(bu, gl, cidx, sil)
            V("pool", "tensor_copy", [fm_b], [BT_b], out=BT[:, gl, :], in_=fm[:, 0:T])
            transposes_to(fm, fm_b, B_tok[:, :, gl * 128:(gl + 1) * 128], B_tok_b, B_rest[:, :, gl * 128:(gl + 1) * 128],
                          B_rest_b)
        for gl in range(2):
            cidx = 24 + 2 * p + gl
            def sil(tb, pt, pb, fm, fm_b, cidx=cidx, gl=gl):
                if tb == 0:
                    V("act", "activation", [pb, par_b], [CT_b], out=CT[:, gl, 0:512], in_=pt[:, :], func=AF.Silu,
                      bias=par[:, cb_o + cidx:cb_o + cidx + 1], scale=1.0)
                elif tb == 1:
                    V("act", "activation", [pb, par_b], [CT_b], out=CT[:, gl, 512:768], in_=pt[:, 0:256],
                      func=AF.Silu, bias=par[:, cb_o + cidx:cb_o + cidx + 1], scale=1.0)
            proj_conv(cu, gl, cidx, sil)
        for xl in range(4):
            cidx = 4 * p + xl
            gl = xl // 2
            g = 2 * p + gl
            def sil(tb, pt, pb, fm, fm_b, cidx=cidx):
                V("act", "activation", [pb, par_b], [fm_b], out=fm[:, tb * 512:(tb + 1) * 512], in_=pt[:, :],
                  func=AF.Silu, bias=par[:, cb_o + cidx:cb_o + cidx + 1], scale=1.0)
            fm, fm_b = proj_conv(xu, xl, cidx, sil)
            transposes_to(fm, fm_b, x_tok[:, :, xl * 128:(xl + 1) * 128], x_tok_b,
                          x_rest[:, :, (xl % 2) * 128:(xl % 2 + 1) * 128], x_rest_b)
            if xl % 2 == 1:
                pt4, pb4 = PS(4)
                ri = 0
                for d in range(2):
                    for i in range(6, 12):
                        xs_t, xs_b = xsr[ri % 4]
                        ri += 1
                        V("dve", "tensor_tensor", [x_rest_b, wrest_b], [xs_b],
                          out=xs_t.rearrange("p (h q) -> p h q", h=4),
                          in0=x_rest[:, i - 6, :].rearrange("p (h q) -> p h q", h=4),
                          in1=wrest[:, i - 6, d * 32 + 4 * g:d * 32 + 4 * g + 4].unsqueeze(2).to_broadcast(
                              [128, 4, 64]), op=ALU.mult)
                        V("pe", "matmul", [B_rest_b, xs_b], [pb4], pt4[:, d * 256:(d + 1) * 256],
                          lhsT=B_rest[:, i - 6, gl * 128:(gl + 1) * 128], rhs=xs_t, start=(i == 6), stop=(i == 11))
                for d in range(2):
                    dma_in(h0t[:, d], h0t_b, st0v[g][:, d])
                pt5, pb5 = PS(5)
                for d in range(2):
                    for t_ in range(2):
                        V("pe", "transpose", [h0t_b, cst_b], [pb5],
                          out=pt5[:, (d * 2 + t_) * 128:(d * 2 + t_ + 1) * 128], in_=h0t[:, d, t_, :],
                          identity=IDENT)
                V("dve", "tensor_tensor", [pb5, ptot_b], [htmp_b],
                  out=htmp.rearrange("p (d h q) -> p d h q", d=2, h=4),
                  in0=pt5[:, :].rearrange("p (d h q) -> p d h q", d=2, h=4),
                  in1=ptot.rearrange("p (d h) -> p d h", d=2)[:, :, 4 * g:4 * g + 4].unsqueeze(3).to_broadcast(
                      [128, 2, 4, 64]), op=ALU.mult)
                V("dve", "tensor_tensor", [pb4, htmp_b], [H_b], out=H[:, :, gl * 256:(gl + 1) * 256],
                  in0=pt4[:, :].rearrange("p (d c) -> p d c", d=2), in1=htmp.rearrange("p (d c) -> p d c", d=2),
                  op=ALU.add)

        if dbg == "chain" and p == 0:
            dump(x_tok.rearrange("p a b -> p (a b)"), x_tok_b, 3072)
            dump(B_tok.rearrange("p a b -> p (a b)"), B_tok_b, 1536)
            dump(H.rearrange("p a b -> p (a b)"), H_b, 1024)
            dump(CT[:, 0, :], CT_b, 768)
            dump(BT[:, 1, :], BT_b, 768)
            return end_dbg()

        SLT.reset()
        xs_pool = [SLT.carve([128, 512], BF16) for _ in range(4)]
        xs_i = [0]
        scss = [SLT.carve([128, 2, 128], F32) for _ in range(2)]
        lt4s = [SLT.carve([128, 4, 128], BF16) for _ in range(4)]
        lt4bufs = [[Buf("lt4_%d_%d" % (a_, b_)) for b_ in range(4)] for a_ in range(4)]
        for a_ in range(4):
            for b_ in lt4bufs[a_]:
                b_.t.rd = dict(lt4s[a_][1].t.rd)
                SLT.tiles.append(b_)
        mt4s = [SLT.carve([128, 4, 128], BF16) for _ in range(4)]
        yaccs = [SLT.carve([128, 512], F32) for _ in range(2)]
        ytmp, ytmp_b = SLT.carve([128, 512], F32)
        ybfs = [SLT.carve([128, 512], BF16) for _ in range(2)]
        yc_i = [0]
        stg = [SLT.carve([128, 4, 128], F32, dma=True) for _ in range(2)]

        def hb8(ap2d):
            return ap2d.rearrange("p (h q) -> p h q", h=8)

        def bc8(ap8):
            return ap8.unsqueeze(2).to_broadcast([128, 8, 64])

        def make_xs(c, wap, wbufs):
            t_, b_ = xs_pool[xs_i[0] % 4]
            xs_i[0] += 1
            V("dve", "tensor_tensor", [x_tok_b] + wbufs, [b_], out=hb8(t_), in0=hb8(x_tok[:, c, :]), in1=bc8(wap),
              op=ALU.mult)
            return t_, b_

        def state_mm(c, xs):
            pt, pb = PS(6)
            for gl in range(2):
                V("pe", "matmul", [B_tok_b, xs[1]], [pb], pt[:, gl * 256:(gl + 1) * 256],
                  lhsT=B_tok[:, c, gl * 128:(gl + 1) * 128], rhs=xs[0][:, gl * 256:(gl + 1) * 256],
                  start=True, stop=True)
            return pt, pb

        def wd(c, d):
            return wdec[:, c, d * 32 + 8 * p:d * 32 + 8 * p + 8]

        def ent_from_state(c, d, ent, hdir=None, etot_c=None):
            xs = make_xs(c, wd(c, d), [wdec_b])
            pt, pb = state_mm(c, xs)
            if hdir is None:
                V("act", "activation", [pb], [ent[1]], out=ent[0], in_=pt[:, :], func=AF.Copy)
            else:
                V("dve", "tensor_tensor", [H_b, Eown_b], [ytmp_b], out=hb8(ytmp), in0=hb8(H[:, hdir, :]),
                  in1=bc8(Eown[:, etot_c, 128 + hdir * 32 + 8 * p:128 + hdir * 32 + 8 * p + 8]), op=ALU.mult)
                V("dve", "tensor_tensor", [pb, ytmp_b], [ent[1]], out=ent[0], in0=pt[:, :], in1=ytmp, op=ALU.add)

        def finals(s, d):
            c0, c1 = 2 * s, 2 * s + 1
            if d == 0:
                xa = make_xs(c0, wfin[:, 2 * s, 8 * p:8 * p + 8], [wfin_b]); ca = c0
                xb = make_xs(c1, wd(c1, 0), [wdec_b]); cb = c1
            else:
                xa = make_xs(c1, wfin[:, 2 * s + 1, 8 * p:8 * p + 8], [wfin_b]); ca = c1
                xb = make_xs(c0, wd(c0, 1), [wdec_b]); cb = c0
            pt, pb = PS(5)
            for i in range(4):
                gl = i // 2
                V("pe", "matmul", [xa[1], B_tok_b], [pb], pt[:, i * 128:(i + 1) * 128],
                  lhsT=xa[0][:, i * 128:(i + 1) * 128], rhs=B_tok[:, ca, gl * 128:(gl + 1) * 128],
                  start=True, stop=False)
                V("pe", "matmul", [xb[1], B_tok_b], [pb], pt[:, i * 128:(i + 1) * 128],
                  lhsT=xb[0][:, i * 128:(i + 1) * 128], rhs=B_tok[:, cb, gl * 128:(gl + 1) * 128],
                  start=False, stop=True)
            st_, sb_ = stg[stg_i[0] % 2]
            stg_i[0] += 1
            V("act", "activation", [pb], [sb_], out=st_, in_=pt[:, :].rearrange("p (i n) -> p i n", i=4),
              func=AF.Copy)
            r0 = (s * 2 + d) * 2048 + 512 * p
            dst = ns_out[r0:r0 + 512, :].rearrange("(i r) n -> r i n", r=128)
            P.op("sp", (lambda E, dst=dst, st_=st_: E.dma_start(out=dst, in_=st_)), rd=[sb_], dma=sb_)
            if sb_ not in out_bufs:
                out_bufs.append(sb_)

        yn_i = [0]

        def y_front(c, ent_f, ent_b):
            scs, scs_b = scss[yc_i[0] % 2]
            yacc, yacc_b = yaccs[yc_i[0] % 2]
            ybf, ybf_b = ybfs[yc_i[0] % 2]
            pt3, pb3 = PS((3, 7)[yc_i[0] % 2])
            yc_i[0] += 1
            pt0, pb0 = PS(0)
            for gl in range(2):
                V("pe", "matmul", [BT_b, CT_b], [pb0], pt0[:, gl * 128:(gl + 1) * 128],
                  lhsT=BT[:, gl, c * 128:(c + 1) * 128], rhs=CT[:, gl, c * 128:(c + 1) * 128], start=True, stop=True)
            V("act", "activation", [pb0], [scs_b], out=scs, in_=pt0[:, 0:256].rearrange("p (g n) -> p g n", g=2),
              func=AF.Copy)
            xsd = [make_xs(c, dtv[:, c, d * 32 + 8 * p:d * 32 + 8 * p + 8], [dtv_b]) for d in range(2)]
            for gl in range(2):
                mts = []
                for d in range(2):
                    n = yn_i[0]
                    yn_i[0] += 1
                    ptS, pbS = PS((1, 2)[n % 2])
                    lt4, _ = lt4s[n % 4]
                    ltb = lt4bufs[n % 4]
                    mt4, mt4b = mt4s[n % 4]
                    mts.append((mt4, mt4b))
                    for hh in range(4):
                        hl = gl * 4 + hh
                        ci = d * 32 + 8 * p + hl
                        dst = ptS[:, hh * 128:(hh + 1) * 128]
                        V("pe", "matmul", [cs_hi_b, cstb_b], [pbS], dst,
                          lhsT=cs_hi[:, c, ci:ci + 1].to_broadcast([128, 128]), rhs=IDENTB, start=True, stop=False)
                        V("pe", "matmul", [cs_lo_b, cstb_b], [pbS], dst,
                          lhsT=cs_lo[:, c, ci:ci + 1].to_broadcast([128, 128]), rhs=IDENTB, start=False, stop=False)
                        V("pe", "matmul", [negm_b, cstb_b], [pbS], dst, lhsT=IDENTB, rhs=negm[:, d, :],
                          start=False, stop=True)
                    for hh in range(4):
                        hl = gl * 4 + hh
                        ci = d * 32 + 8 * p + hl
                        V("act", "activation", [pbS, negcs_b], [ltb[hh]], out=lt4[:, hh, :],
                          in_=ptS[:, hh * 128:(hh + 1) * 128], func=AF.Exp, bias=negcs[:, c, ci:ci + 1], scale=1.0)
                    V("dve", "tensor_tensor", ltb + [scs_b], [mt4b], out=mt4, in0=lt4,
                      in1=scs[:, gl, :].unsqueeze(1).to_broadcast([128, 4, 128]), op=ALU.mult)
                for hh in range(4):
                    hl = gl * 4 + hh
                    for d in range(2):
                        V("pe", "matmul", [mts[d][1], xsd[d][1]], [pb3], pt3[:, hl * 64:(hl + 1) * 64],
                          lhsT=mts[d][0][:, hh, :], rhs=xsd[d][0][:, hl * 64:(hl + 1) * 64],
                          start=(d == 0), stop=(d == 1))
            return (c, ent_f, ent_b, yacc, yacc_b, ybf, ybf_b, pt3, pb3)

        def y_tail(ctx):
            (c, ent_f, ent_b, yacc, yacc_b, ybf, ybf_b, pt3, pb3) = ctx
            for d, ent in ((0, ent_f), (1, ent_b)):
                if ent is None:
                    continue
                ptO, pbO = PS(4 + d)
                for gl in range(2):
                    V("pe", "matmul", [CT_b, ent[1]], [pbO], ptO[:, gl * 256:(gl + 1) * 256],
                      lhsT=CT[:, gl, c * 128:(c + 1) * 128], rhs=ent[0][:, gl * 256:(gl + 1) * 256],
                      start=True, stop=True)
            V("dve", "tensor_tensor", [x_tok_b, par_b], [yacc_b], out=hb8(yacc), in0=hb8(x_tok[:, c, :]),
              in1=bc8(pc("d_skip")[:, 8 * p:8 * p + 8]), op=ALU.mult)
            V("dve", "tensor_tensor", [pb3, yacc_b], [yacc_b], out=yacc, in0=pt3[:, :], in1=yacc, op=ALU.add)
            for d, ent in ((0, ent_f), (1, ent_b)):
                if ent is None:
                    continue
                ptO, pbO = PS(4 + d)
                V("dve", "tensor_tensor", [pbO, Eown_b], [ytmp_b], out=hb8(ytmp), in0=hb8(ptO[:, :]),
                  in1=bc8(Eown[:, c, d * 32 + 8 * p:d * 32 + 8 * p + 8]), op=ALU.mult)
                V("dve", "tensor_tensor", [ytmp_b, yacc_b], [yacc_b], out=yacc, in0=yacc, in1=ytmp, op=ALU.add)
            V("act", "activation", [yacc_b], [ybf_b], out=ybf, in_=yacc, func=AF.Copy)
            pt6, pb6 = PS(6)
            pt6b = pt6[:, :].bitcast(BF16)
            for j in range(4):
                V("pe", "transpose", [ybf_b, cstb_b], [pb6], out=pt6b[:, j * 128:(j + 1) * 128],
                  in_=ybf[:, j * 128:(j + 1) * 128], identity=IDENTB)
            V("act", "activation", [pb6], [yTb[p]], out=yT[:, 4 * p:4 * p + 4, c * 128:(c + 1) * 128],
              in_=pt6b[:, 0:512].rearrange("p (j n) -> p j n", j=4), func=AF.Copy)

        jobs = []
        for s in range(2):
            c0, c1 = 2 * s, 2 * s + 1
            ent_a, ent_bb = ents[2 * s], ents[2 * s + 1]

            def pre(s=s, c0=c0, c1=c1, ent_a=ent_a, ent_bb=ent_bb):
                ent_from_state(c0, 0, ent_a)
                ent_from_state(c1, 1, ent_bb)
                finals(s, 0)
                finals(s, 1)
            jobs.append((pre, c0, None, ent_bb))
            jobs.append((None, c1, ent_a, None))
        e0, e1, e2, e3 = ents

        def pre4():
            V("act", "activation", [H_b], [e0[1]], out=e0[0], in_=H[:, 0, :], func=AF.Copy)
            ent_from_state(5, 1, e1, hdir=1, etot_c=5)

        def pre5():
            ent_from_state(4, 0, e2, hdir=0, etot_c=4)
            V("act", "activation", [H_b], [e3[1]], out=e3[0], in_=H[:, 1, :], func=AF.Copy)
        jobs.append((pre4, 4, e0, e1))
        jobs.append((pre5, 5, e2, e3))
        prev_ctx = None
        for (pre, c, ef, eb_) in jobs:
            if pre is not None:
                pre()
            ctx = y_front(c, ef, eb_)
            if prev_ctx is not None:
                y_tail(prev_ctx)
            prev_ctx = ctx
        y_tail(prev_ctx)

    if dbg == "ssd":
        dump(yT[:, 0, :], yT_b, 768)
        dump(yT[:, 5, :], yT_b, 768)
        dump(yT[:, 15, :], yT_b, 768)
        P.ops["pool"][-1][1].update([b_.t.lw for b_ in yTb if b_.t.lw is not None])
        P.ops["pool"][-2][1].update([b_.t.lw for b_ in yTb if b_.t.lw is not None])
        P.ops["pool"][-3][1].update([b_.t.lw for b_ in yTb if b_.t.lw is not None])
        return end_dbg()

    SLT.reset(); SL2.reset(); SL3.reset()
    yrd = [yT_b] + yTb

    def hbufs(bi):
        return hTb[0:4] if bi == 0 else hTb[4:6]

    pf_i = [0]

    def proj_fm(units, jt, rhs_tile, rhs_bufs_fn, evac):
        par_ = pf_i[0] % 2
        pf_i[0] += 1
        nk = 8 * len(units)
        for bi, (t0, n, cnd) in enumerate(BLK):
            pt, pb = PS(par_ * 2 + bi)
            kk = 0
            for (ut, ub) in units:
                for k in range(8):
                    V("pe", "matmul", [ub] + rhs_bufs_fn(bi), [pb], pt[:, 0:n],
                      lhsT=ut[:, k, jt * 128:(jt + 1) * 128], rhs=rhs_tile[:, kk, t0:t0 + n],
                      start=(kk == 0), stop=(kk == nk - 1))
                    kk += 1
            evac(bi, t0, n, cnd, pt[:, 0:n], pb)

    gs, gs_b = SLT.carve([128, 8, T], BF16)
    gc, gc_b = SLT.carve([128, 8, T], BF16)
    bg_o = PAR["b_gate"][0]
    for (c0, gt, gb, joff) in ((8256, gs, gs_b, 0), (9280, gc, gc_b, 8)):
        gu_ = load_unit(w_in_d, c0, 1024)
        for j in range(8):
            def evac(bi, t0, n, cnd, pap, pb, j=j, gt=gt, gb=gb, joff=joff):
                V("act", "activation", [pb, par_b], [gb], out=gt[:, j, t0:t0 + n], in_=pap, func=AF.Sigmoid,
                  bias=par[:, bg_o + joff + j:bg_o + joff + j + 1], scale=1.0)
            proj_fm([gu_], j, hT, hbufs, evac)

    szs = [SL3.carve([128, T]) for _ in range(2)]
    sqs = [SL3.carve([128, T], BF16) for _ in range(2)]
    rgs = [SL3.carve([128, T]) for _ in range(2)]
    zus = [None, None]
    for j in range(16):
        if j % 8 == 0:
            zus[j // 8] = load_unit(w_in_d, (j // 8) * 1024, 1024)
        zu = zus[j // 8]
        sz, szb = szs[j % 2]
        sq, sqb = sqs[j % 2]

        def evac(bi, t0, n, cnd, pap, pb, sz=sz, szb=szb):
            V("act", "activation", [pb], [szb], out=sz[:, t0:t0 + n], in_=pap, func=AF.Silu)
        proj_fm([zu], j % 8, hT, hbufs, evac)
        V("dve", "tensor_tensor", yrd + [szb], [yT_b], out=yT[:, j, :], in0=yT[:, j, :], in1=sz, op=ALU.mult)
        V("act", "activation", [yT_b], [sqb], out=sq, in_=yT[:, j, :], func=AF.Square)
        for bi, (t0, n, cnd) in enumerate(BLK):
            V("pe", "matmul", [sqb, cstb_b], [PS(4 + bi)[1]], PS(4 + bi)[0][:, 0:n], lhsT=ONESB,
              rhs=sq[:, t0:t0 + n], start=(j % 2 == 0), stop=(j % 2 == 1))
        if j % 2 == 1:
            rg, rgb = rgs[(j // 2) % 2]
            for bi, (t0, n, cnd) in enumerate(BLK):
                rsqrt_from(rg[:, t0:t0 + n], rgb, PS(4 + bi)[0][:, 0:n], PS(4 + bi)[1], 1.0 / 256)
            for jj in (j - 1, j):
                V("dve", "scalar_tensor_tensor", [yT_b, rgb, par_b], [yT_b], out=yT[:, jj, :], in0=yT[:, jj, :],
                  scalar=pc("ssm_norm_w")[:, jj:jj + 1], in1=rg, op0=ALU.mult, op1=ALU.mult)

    if dbg == "yn":
        dump(yT[:, 0, :], yT_b, 768)
        dump(yT[:, 9, :], yT_b, 768)
        dump(gs[:, 3, :], gs_b, 768)
        dump(gc[:, 7, :], gc_b, 768)
        return end_dbg()

    mT, mT_b = SL2.carve([128, 8, T])
    so1 = load_unit(w_ssd_d, 0, 1024, k0=0)
    so2 = load_unit(w_ssd_d, 0, 1024, k0=8)
    for dd in range(8):
        def evac(bi, t0, n, cnd, pap, pb, dd=dd):
            V("dve", "tensor_tensor", [pb, gs_b], [mT_b], out=mT[:, dd, t0:t0 + n], in0=pap,
              in1=gs[:, dd, t0:t0 + n], op=ALU.mult)
        proj_fm([so1, so2], dd, yT, lambda bi: [yT_b], evac)

    SL1.reset()
    SL3.reset()
    uc, uc_b = SL3.carve([128, 8, T])
    upres = [SL1.carve([128, 948], BF16) for _ in range(2)]
    sg, sg_b = SL1.carve([128, T])
    dg31s = [SL1.carve([128, 31, 128], BF16) for _ in range(2)]
    for (up, upb) in upres:
        V("dve", "memset", [], [upb], up, 0.0)
    au = load_unit(w_in_d, 6208, 1024)
    bu_ = load_unit(w_in_d, 7232, 1024)
    cw_o = PAR["cdw_w"][0]
    cdb_o = PAR["cdw_b"][0]
    for j in range(8):
        up, upb = upres[j % 2]
        upP = up[:, 0:572].rearrange("p (s t) -> p s t", s=2)
        upS = up[:, 572:948].rearrange("p (s t) -> p s t", s=4)
        for bi, (t0, n, cnd) in enumerate(BLK):
            pa, pab = PS(bi)
            pbt, pbb = PS(2 + bi)
            for k in range(8):
                V("pe", "matmul", [au[1]] + hbufs(bi), [pab], pa[:, 0:n], lhsT=au[0][:, k, j * 128:(j + 1) * 128],
                  rhs=hT[:, k, t0:t0 + n], start=(k == 0), stop=(k == 7))
            for k in range(8):
                V("pe", "matmul", [bu_[1]] + hbufs(bi), [pbb], pbt[:, 0:n],
                  lhsT=bu_[0][:, k, j * 128:(j + 1) * 128], rhs=hT[:, k, t0:t0 + n], start=(k == 0), stop=(k == 7))
            V("act", "activation", [pbb], [sg_b], out=sg[:, t0:t0 + n], in_=pbt[:, 0:n], func=AF.Sigmoid)
            if bi == 0:
                V("dve", "tensor_tensor", [pab, sg_b], [upb], out=upP[:, :, 15:271],
                  in0=pa[:, 0:512].rearrange("p (s t) -> p s t", s=2),
                  in1=sg[:, 0:512].rearrange("p (s t) -> p s t", s=2), op=ALU.mult)
            else:
                V("dve", "tensor_tensor", [pab, sg_b], [upb], out=upS[:, :, 15:79],
                  in0=pa[:, 0:256].rearrange("p (s t) -> p s t", s=4),
                  in1=sg[:, 512:768].rearrange("p (s t) -> p s t", s=4), op=ALU.mult)
        dg, dgb = dg31s[j % 2]
        dgks = [Buf("dg31_%d_%d" % (j, k)) for k in range(31)]
        for k in range(31):
            dgks[k].t.rd = dict(dgb.t.rd)
            if dgb.t.lw is not None:
                dgks[k].t.rd[("lw", id(dgb))] = dgb.t.lw
            SL1.tiles.append(dgks[k])
            wc_ = par[:, cw_o + j * 31 + k:cw_o + j * 31 + k + 1]
            if k % 2 == 0:
                V("dve", "tensor_scalar", [cstb_b, par_b], [dgks[k]], out=dg[:, k, :], in0=IDENTB,
                  scalar1=wc_, scalar2=None, op0=ALU.mult)
            else:
                V("act", "activation", [cstb_b, par_b], [dgks[k]], out=dg[:, k, :], in_=IDENTB, func=AF.Copy,
                  scale=wc_)
        for bi, (t0, n, cnd) in enumerate(BLK):
            pt, pb = PS(4 + 2 * (j % 2) + bi)
            for k in range(31):
                rhs = upP[:, :, k:k + 256] if bi == 0 else upS[:, :, k:k + 64]
                V("pe", "matmul", [upb, dgks[k]], [pb], pt[:, 0:n], lhsT=dg[:, k, :], rhs=rhs,
                  start=(k == 0), stop=(k == 30))
                dgb.t.rd["pe"] = dgks[k].t.rd.get("pe", dgb.t.rd.get("pe"))
            V("act", "activation", [pb, par_b], [uc_b], out=uc[:, j, t0:t0 + n], in_=pt[:, 0:n], func=AF.Identity,
              bias=par[:, cdb_o + j:cdb_o + j + 1], scale=1.0)

    SL1.reset()
    un, un_b = SL1.carve([128, 8, T], BF16)
    lnr, lnr_b = SL1.carve([128, T])
    t1s = [SL1.carve([128, T]) for _ in range(2)]
    ucb_, ucbb = SLT.carve([128, T], BF16)
    sq2, sq2b = SLT.carve([128, T], BF16)
    mu, mu_b = SLT.carve([128, T])
    for j in range(8):
        V("act", "activation", [uc_b], [ucbb], out=ucb_, in_=uc[:, j, :], func=AF.Copy)
        V("act", "activation", [uc_b], [sq2b], out=sq2, in_=uc[:, j, :], func=AF.Square)
        for bi, (t0, n, cnd) in enumerate(BLK):
            V("pe", "matmul", [ucbb, cstb_b], [PS(4 + bi)[1]], PS(4 + bi)[0][:, 0:n], lhsT=ONESB,
              rhs=ucb_[:, t0:t0 + n], start=(j == 0), stop=(j == 7))
            V("pe", "matmul", [sq2b, cstb_b], [PS(6 + bi)[1]], PS(6 + bi)[0][:, 0:n], lhsT=ONESB,
              rhs=sq2[:, t0:t0 + n], start=(j == 0), stop=(j == 7))
    for bi, (t0, n, cnd) in enumerate(BLK):
        V("act", "activation", [PS(4 + bi)[1]], [mu_b], out=mu[:, t0:t0 + n], in_=PS(4 + bi)[0][:, 0:n],
          func=AF.Copy, scale=1.0 / 1024)
    V("dve", "tensor_tensor", [mu_b], [lnr_b], out=lnr, in0=mu, in1=mu, op=ALU.mult)
    for bi, (t0, n, cnd) in enumerate(BLK):
        V("dve", "scalar_tensor_tensor", [PS(6 + bi)[1], lnr_b], [lnr_b], out=lnr[:, t0:t0 + n],
          in0=PS(6 + bi)[0][:, 0:n], scalar=1.0 / 1024, in1=lnr[:, t0:t0 + n], op0=ALU.mult, op1=ALU.subtract)
    rsqrt_from(lnr, lnr_b, lnr, lnr_b, 1.0)
    for j in range(8):
        t1, t1b = t1s[j % 2]
        V("dve", "tensor_tensor", [uc_b, mu_b], [t1b], out=t1, in0=uc[:, j, :], in1=mu, op=ALU.subtract)
        V("dve", "tensor_tensor", [t1b, lnr_b], [t1b], out=t1, in0=t1, in1=lnr, op=ALU.mult)
        V("act", "activation", [t1b, par_b], [un_b], out=un[:, j, :], in_=t1, func=AF.Silu,
          scale=pc("ln_w")[:, j:j + 1], bias=pc("ln_b")[:, j:j + 1])
    cuo = load_unit(w_conf_d, 0, 1024)
    for dd in range(8):
        t1, t1b = t1s[dd % 2]

        def evac(bi, t0, n, cnd, pap, pb, dd=dd, t1=t1, t1b=t1b):
            V("dve", "scalar_tensor_tensor", [pb, gc_b, par_b], [t1b], out=t1[:, t0:t0 + n], in0=pap,
              scalar=pc("b_conf_out")[:, dd:dd + 1], in1=gc[:, dd, t0:t0 + n], op0=ALU.add, op1=ALU.mult)
            V("dve", "tensor_tensor", [t1b, mT_b], [mT_b], out=mT[:, dd, t0:t0 + n], in0=mT[:, dd, t0:t0 + n],
              in1=t1[:, t0:t0 + n], op=ALU.add)
        proj_fm([cuo], dd, un, lambda bi: [un_b], evac)

    SLT.reset()
    mb, mb_b = SLT.carve([128, 8, T], BF16)
    xts = [SLT.carve([128, 1024], F32, dma=True) for _ in range(2)]
    V("act", "activation", [mT_b], [mb_b], out=mb, in_=mT, func=AF.Copy)
    SL3.reset()
    x1T, x1T_b = SL3.carve([128, 8, T])
    for i in range(6):
        xtt, xtb = xts[i % 2]
        dma_in(xtt, xtb, x_all[i * 128:(i + 1) * 128, :])

        def evac(j, pap, pb, i=i):
            eng = "dve" if j < 4 else "act"
            if eng == "dve":
                V("dve", "tensor_copy", [pb], [x1T_b], out=x1T[:, j, i * 128:(i + 1) * 128], in_=pap)
            else:
                V("act", "activation", [pb], [x1T_b], out=x1T[:, j, i * 128:(i + 1) * 128], in_=pap, func=AF.Copy)
        transpose_x_tile(xtt, xtb, evac)
    wou = load_unit(w_o_d, 0, 1024)
    for dd in range(8):
        def evac(bi, t0, n, cnd, pap, pb, dd=dd):
            V("dve", "scalar_tensor_tensor", [pb, x1T_b, mod_b], [x1T_b], out=x1T[:, dd, t0:t0 + n], in0=pap,
              scalar=G1(dd, cnd), in1=x1T[:, dd, t0:t0 + n], op0=ALU.mult, op1=ALU.add)
        proj_fm([wou], dd, mb, lambda bi: [mb_b], evac)

    if dbg == "mix":
        for j in (0, 3, 7):
            dump(x1T[:, j, :], x1T_b, 768)
        return end_dbg()

    SL0.reset(); SL1.reset(); SL2.reset(); SLT.reset()
    acc, acc_b = SL0.carve([128, 8, T])
    acts = [SL2.carve([128, 8, T], BF16) for _ in range(2)]
    h2T, h2T_b = SLT.carve([128, 8, T], BF16)
    combT, combT_b = SLT.carve([128, T], BF16)
    rb, rb_b = SLT.carve([128, T])
    sqm, sqm_b = SLT.carve([128, T], BF16)
    lg, lg_b = SLT.carve([128, 6, 32])
    ex, ex_b = SLT.carve([128, 6, 32])
    mk, mk_b = SLT.carve([128, 6, 32])
    comb, comb_b = SLT.carve([128, 6, 32])
    cma_hi, cma_hi_b = SLT.carve([128, 6, 32], BF16)
    cma_lo, cma_lo_b = SLT.carve([128, 6, 32], BF16)
    m8, m8_b = SLT.carve([128, 6, 8])
    sm, sm_b = SLT.carve([128, 6])
    bdn, bdn_b = SLT.carve([128, 1024], BF16, dma=True)
    wrt, wrt_b = SLT.carve([128, 8, 32], F32, dma=True)
    wrt_hi, wrt_hi_b = SLT.carve([128, 8, 32], BF16)
    wrt_lo, wrt_lo_b = SLT.carve([128, 8, 32], BF16)
    h2lo, h2lo_b = SL1.carve([128, 8, T], BF16)
    h2fs = [SL1.carve([128, T]) for _ in range(2)]

    def rms_bc(src, src_b, scale):
        for j in range(8):
            V("act", "activation", [src_b], [sqm_b], out=sqm, in_=src[:, j, :], func=AF.Square)
            for bi, (t0, n, cnd) in enumerate(BLK):
                V("pe", "matmul", [sqm_b, cstb_b], [PS(4 + bi)[1]], PS(4 + bi)[0][:, 0:n], lhsT=ONESB,
                  rhs=sqm[:, t0:t0 + n], start=(j == 0), stop=(j == 7))
        for bi, (t0, n, cnd) in enumerate(BLK):
            rsqrt_from(rb[:, t0:t0 + n], rb_b, PS(4 + bi)[0][:, 0:n], PS(4 + bi)[1], scale)

    rms_bc(x1T, x1T_b, 1.0 / 1024)
    P.op("pool", lambda E: E.dma_start(out=bdn[0:32, :], in_=b_dn_d), wr=[bdn_b], dma=bdn_b)
    dma_in(wrt, wrt_b, w_rt_d.rearrange("p (k e) -> p k e", k=8))
    V("dve", "tensor_copy", [wrt_b], [wrt_hi_b], out=wrt_hi, in_=wrt)
    V("dve", "tensor_tensor", [wrt_b, wrt_hi_b], [wrt_lo_b], out=wrt_lo, in0=wrt, in1=wrt_hi, op=ALU.subtract)
    for j in range(8):
        h2f, h2fb = h2fs[j % 2]
        for bi, (t0, n, cnd) in enumerate(BLK):
            V("dve", "scalar_tensor_tensor", [x1T_b, rb_b, modA_b], [h2fb], out=h2f[:, t0:t0 + n],
              in0=x1T[:, j, t0:t0 + n], scalar=A2(j, cnd), in1=rb[:, t0:t0 + n], op0=ALU.mult, op1=ALU.mult)
            V("act", "activation", [h2fb, mod_b], [h2fb], out=h2f[:, t0:t0 + n], in_=h2f[:, t0:t0 + n],
              func=AF.Identity, bias=S2(j, cnd), scale=1.0)
        V("dve", "tensor_copy", [h2fb], [h2T_b], out=h2T[:, j, :], in_=h2f)
        V("dve", "tensor_tensor", [h2fb, h2T_b], [h2lo_b], out=h2lo[:, j, :], in0=h2f, in1=h2T[:, j, :],
          op=ALU.subtract)
    pt0, pb0 = PS(0)
    for i in range(6):
        n_mm = 0
        for k in range(8):
            for (lt_, ltb, rt_, rtb) in ((h2T, h2T_b, wrt_hi, wrt_hi_b), (h2T, h2T_b, wrt_lo, wrt_lo_b),
                                         (h2lo, h2lo_b, wrt_hi, wrt_hi_b)):
                V("pe", "matmul", [ltb, rtb], [pb0], pt0[:, i * 32:(i + 1) * 32],
                  lhsT=lt_[:, k, i * 128:(i + 1) * 128], rhs=rt_[:, k, :], start=(n_mm == 0), stop=(n_mm == 23))
                n_mm += 1
    V("dve", "tensor_tensor", [pb0, par_b], [lg_b], out=lg, in0=pt0[:, 0:192].rearrange("p (i e) -> p i e", i=6),
      in1=pc("b_router").unsqueeze(1).to_broadcast([128, 6, 32]), op=ALU.add)
    for i in range(6):
        V("dve", "max", [lg_b], [m8_b], out=m8[:, i, :], in_=lg[:, i, :])
    V("dve", "tensor_tensor", [lg_b, m8_b], [mk_b], out=mk, in0=lg, in1=m8[:, :, 3:4].to_broadcast([128, 6, 32]),
      op=ALU.is_ge)
    V("dve", "tensor_tensor", [lg_b, m8_b], [ex_b], out=ex, in0=lg, in1=m8[:, :, 0:1].to_broadcast([128, 6, 32]),
      op=ALU.subtract)
    V("act", "activation", [ex_b], [ex_b], out=ex, in_=ex, func=AF.Exp)
    V("dve", "tensor_tensor", [ex_b, mk_b], [ex_b], out=ex, in0=ex, in1=mk, op=ALU.mult)
    V("dve", "tensor_reduce", [ex_b], [sm_b], out=sm, in_=ex, axis=AX.X, op=ALU.add)
    V("dve", "reciprocal", [sm_b], [sm_b], out=sm, in_=sm)
    V("dve", "tensor_tensor", [ex_b, sm_b], [comb_b], out=comb, in0=ex,
      in1=sm.unsqueeze(2).to_broadcast([128, 6, 32]), op=ALU.mult)
    V("dve", "tensor_scalar", [comb_b], [ex_b], out=ex, in0=comb, scalar1=1.0 / SW_ALPHA, scalar2=None, op0=ALU.mult)
    V("dve", "tensor_copy", [ex_b], [cma_hi_b], out=cma_hi, in_=ex)
    V("dve", "tensor_tensor", [ex_b, cma_hi_b], [cma_lo_b], out=cma_lo, in0=ex, in1=cma_hi, op=ALU.subtract)
    for i in range(6):
        ptc, pbc = PS(1 + i // 4)
        V("pe", "transpose", [comb_b, cst_b], [pbc], out=ptc[0:32, (i % 4) * 128:(i % 4 + 1) * 128],
          in_=comb[:, i, :], identity=IDENT)
    V("act", "activation", [PS(1)[1]], [combT_b], out=combT[0:32, 0:512], in_=PS(1)[0][0:32, 0:512], func=AF.Copy)
    V("act", "activation", [PS(2)[1]], [combT_b], out=combT[0:32, 512:768], in_=PS(2)[0][0:32, 0:256], func=AF.Copy)
    for dd in range(8):
        for bi, (t0, n, cnd) in enumerate(BLK):
            pt, pb = PS(4 + (dd * 2 + bi) % 2)
            V("pe", "matmul", [bdn_b, combT_b], [pb], pt[:, 0:n], lhsT=bdn[0:32, dd * 128:(dd + 1) * 128],
              rhs=combT[0:32, t0:t0 + n], start=True, stop=True)
            V("act", "activation", [pb], [acc_b], out=acc[:, dd, t0:t0 + n], in_=pt[:, 0:n], func=AF.Copy)

    if dbg == "router":
        dump(comb.rearrange("p a b -> p (a b)"), comb_b, 192)
        dump(acc[:, 2, :], acc_b, 768)
        dump(h2T[:, 5, :], h2T_b, 768)
        return end_dbg()

    SL1.reset()
    gsbs = [SL1.carve([128, T]) for _ in range(2)]
    tts = [SL1.carve([128, T]) for _ in range(2)]
    ubs = [SL1.carve([128, T]) for _ in range(2)]
    cmbs = [SL1.carve([128, T]) for _ in range(2)]
    bgu_o = PAR["b_gu"][0]

    def make_cmb(e):
        cm_t, cm_b = cmbs[e % 2]
        for i in range(6):
            pt, pb = PS(6 + i // 4)
            dst = pt[:, (i % 4) * 128:(i % 4 + 1) * 128]
            V("pe", "matmul", [cma_hi_b, cstb_b], [pb], dst, lhsT=cma_hi[:, i, e:e + 1].to_broadcast([128, 128]),
              rhs=IDENTB, start=True, stop=False)
            V("pe", "matmul", [cma_lo_b, cstb_b], [pb], dst, lhsT=cma_lo[:, i, e:e + 1].to_broadcast([128, 128]),
              rhs=IDENTB, start=False, stop=True)
        V("act", "activation", [PS(6)[1]], [cm_b], out=cm_t[:, 0:512], in_=PS(6)[0][:, 0:512], func=AF.Copy)
        V("act", "activation", [PS(7)[1]], [cm_b], out=cm_t[:, 512:768], in_=PS(7)[0][:, 0:256], func=AF.Copy)
        return cm_t, cm_b

    jcount = [0]

    def expert_gu(e, jh, U, act, cm, down_iter=None):
        ut, ub_ = U
        act_t, act_b = act
        cm_t, cm_b = cm
        for jj in range(4):
            j = jh * 4 + jj
            s_ = jcount[0] % 2
            jcount[0] += 1
            gA, gAb = PS(3 * s_)
            uA, uAb = PS(3 * s_ + 1)
            gB, gBb = PS(3 * s_ + 2)
            if down_iter is not None:
                for _ in range(2):
                    next(down_iter, None)
            for k in range(8):
                w_ = ut[:, k, jj * 128:(jj + 1) * 128]
                V("pe", "matmul", [ub_, h2T_b], [gAb], gA[:, 0:512], lhsT=w_, rhs=h2T[:, k, 0:512],
                  start=(k == 0), stop=(k == 7))
                V("pe", "matmul", [ub_, h2T_b], [gBb], gB[:, 0:256], lhsT=w_, rhs=h2T[:, k, 512:768],
                  start=(k == 0), stop=(k == 7))
            for k in range(8):
                w_ = ut[:, k, 512 + jj * 128:512 + (jj + 1) * 128]
                V("pe", "matmul", [ub_, h2T_b], [uAb], uA[:, 0:512], lhsT=w_, rhs=h2T[:, k, 0:512],
                  start=(k == 0), stop=(k == 7))
                V("pe", "matmul", [ub_, h2T_b], [gBb], gB[:, 256:512], lhsT=w_, rhs=h2T[:, k, 512:768],
                  start=(k == 0), stop=(k == 7))
            bg = par[:, bgu_o + e * 16 + j:bgu_o + e * 16 + j + 1]
            bu = par[:, bgu_o + e * 16 + 8 + j:bgu_o + e * 16 + 8 + j + 1]
            gsb, gsbb = gsbs[s_]
            tt, ttb = tts[s_]
            ubt, ubb = ubs[s_]
            V("dve", "tensor_scalar", [gAb, par_b], [gsbb], out=gsb[:, 0:512], in0=gA[:, 0:512], scalar1=bg,
              scalar2=SW_LIM, op0=ALU.add, op1=ALU.min)
            V("dve", "tensor_scalar", [gBb, par_b], [gsbb], out=gsb[:, 512:768], in0=gB[:, 0:256], scalar1=bg,
              scalar2=SW_LIM, op0=ALU.add, op1=ALU.min)
            V("act", "activation", [uAb, par_b], [ubb], out=ubt[:, 0:512], in_=uA[:, 0:512], func=AF.Identity,
              bias=bu, scale=1.0)
            V("dve", "tensor_scalar", [gBb, par_b], [ubb], out=ubt[:, 512:768], in0=gB[:, 256:512], scalar1=bu,
              scalar2=None, op0=ALU.add)
            V("act", "activation", [gsbb], [ttb], out=tt, in_=gsb, func=AF.Silu, scale=SW_ALPHA)
            if down_iter is not None:
                for _ in range(2):
                    next(down_iter, None)
            V("dve", "tensor_scalar", [ubb], [ubb], out=ubt, in0=ubt, scalar1=SW_LIM,
              scalar2=-SW_LIM, op0=ALU.min, op1=ALU.max)
            V("dve", "scalar_tensor_tensor", [ubb, ttb], [ttb], out=tt, in0=ubt, scalar=1.0, in1=tt,
              op0=ALU.add, op1=ALU.mult)
            V("dve", "tensor_tensor", [ttb, cm_b], [act_b], out=act_t[:, j, :], in0=tt, in1=cm_t, op=ALU.mult)

    dcount = [0]

    accb = [[Buf("acc_%d_%d" % (dd, bi)) for bi in range(2)] for dd in range(8)]
    for dd in range(8):
        for bi in range(2):
            accb[dd][bi].t.lw = acc_b.t.lw
            accb[dd][bi].t.rd = dict(acc_b.t.rd)
            SL0.tiles.append(accb[dd][bi])

    def expert_down_gen(UD, act):
        ut, ub_ = UD
        act_t, act_b = act
        for bi, (t0, n, cnd) in enumerate(BLK):
            for dd in range(8):
                pt, pb = PS(6 + dcount[0] % 2)
                dcount[0] += 1
                for k in range(8):
                    V("pe", "matmul", [ub_, act_b], [pb], pt[:, 0:n], lhsT=ut[:, k, dd * 128:(dd + 1) * 128],
                      rhs=act_t[:, k, t0:t0 + n], start=(k == 0), stop=(k == 7))
                V("dve", "tensor_tensor", [pb, accb[dd][bi]], [accb[dd][bi]], out=acc[:, dd, t0:t0 + n],
                  in0=pt[:, 0:n], in1=acc[:, dd, t0:t0 + n], op=ALU.add)
                yield

    def expert_down(UD, act):
        for _ in expert_down_gen(UD, act):
            pass

    n_exp = N_EXP
    if dbg and dbg.startswith("moe"):
        n_exp = int(dbg[3:])
    UA = load_unit2(w_gu_d[0], 0, 1024)
    UB = load_unit2(w_gu_d[0], 512, 1536)
    UD = load_unit(w_dn_d[0], 0, 1024)
    prev = None
    for e in range(n_exp):
        cm = make_cmb(e)
        act = acts[e % 2]
        dit = expert_down_gen(*prev) if prev is not None else None
        expert_gu(e, 0, UA, act, cm, dit)
        if dit is not None:
            for _ in dit:
                pass
        if e + 1 < n_exp:
            UA_n = load_unit2(w_gu_d[e + 1], 0, 1024)
            UB_n = load_unit2(w_gu_d[e + 1], 512, 1536)
        expert_gu(e, 1, UB, act, cm)
        prev = (UD, act)
        if e + 1 < n_exp:
            UD = load_unit(w_dn_d[e + 1], 0, 1024)
            UA, UB = UA_n, UB_n
    expert_down(*prev)

    for j in range(8):
        for bi, (t0, n, cnd) in enumerate(BLK):
            V("dve", "scalar_tensor_tensor", [accb[j][bi], x1T_b, mod_b], [x1T_b], out=x1T[:, j, t0:t0 + n],
              in0=acc[:, j, t0:t0 + n], scalar=G2(j, cnd), in1=x1T[:, j, t0:t0 + n], op0=ALU.mult, op1=ALU.add)
    rms_bc(x1T, x1T_b, 1.0 / 1024)
    for j in range(8):
        V("dve", "scalar_tensor_tensor", [x1T_b, rb_b, par_b], [x1T_b], out=x1T[:, j, :], in0=x1T[:, j, :],
          scalar=pc("norm_final")[:, j:j + 1], in1=rb, op0=ALU.mult, op1=ALU.mult)
    SL2.reset()
    ystg = [SL2.carve([128, 1024], F32, dma=True) for _ in range(2)]
    for i in range(6):
        ys, ysb = ystg[i % 2]
        for hb in range(2):
            pt, pb = PS(1 + hb)
            for jj in range(4):
                j = hb * 4 + jj
                V("pe", "transpose", [x1T_b, cst_b], [pb], out=pt[:, jj * 128:(jj + 1) * 128],
                  in_=x1T[:, j, i * 128:(i + 1) * 128], identity=IDENT)
            if hb == 0:
                V("act", "activation", [pb], [ysb], out=ys[:, 0:512], in_=pt[:, :], func=AF.Copy)
            else:
                V("dve", "tensor_copy", [pb], [ysb], out=ys[:, 512:1024], in_=pt[:, :])
        dst = y_out[i * 128:(i + 1) * 128, :]
        P.op("sp", (lambda E, dst=dst, ys=ys: E.dma_start(out=dst, in_=ys)), rd=[ysb], dma=ysb)
        if ysb not in out_bufs:
            out_bufs.append(ysb)
    P.op("sp", None, wr=out_bufs)
    return finish(nc, P, es, sems)


def finish(nc, P, es, sems):
    P.finalize()
    with nc.Block() as block:
        @block.tensor
        def _(E):
            P.emit("pe", E, sems)

        @block.scalar
        def _(E):
            P.emit("act", E, sems)

        @block.vector
        def _(E):
            P.emit("dve", E, sems)

        @block.gpsimd
        def _(E):
            P.emit("pool", E, sems)

        @block.sync
        def _(E):
            P.emit("sp", E, sems)
    es.close()
    return nc


def core_inputs(inp, core, shared):
    b, q = core // 4, core % 4
    xp = np.asarray(inp["x_prompt"], np.float32)
    xs = np.asarray(inp["x_sample"], np.float32)
    xrot = np.roll(xs[b], -256 * q, axis=0)
    x_all = np.ascontiguousarray(np.concatenate([xp[2 * core], xp[2 * core + 1], xrot], axis=0))
    cond = np.stack([_fm(inp["c_ctx"], 8), _fm(np.asarray(inp["c"])[b], 8)], axis=2).reshape(128, 16)
    st0 = np.ascontiguousarray(np.asarray(inp["state_ssm"], np.float32)[b, 0].reshape(4096, 128))
    m = np.zeros((20,), np.float32)
    m[0:8] = 1.0
    m[(8 - 2 * q) % 8] = 0.0
    for s in range(2, 8):
        orig = (s + 2 * q) % 8
        m[8 + (s - 2)] = 1.0 if orig < 2 * q else 0.0
        m[14 + (s - 2)] = 0.0 if orig < 2 * q else 1.0
    d = dict(shared)
    d.update({"x_all": x_all, "cond_fm": np.ascontiguousarray(cond),
              "state0": st0, "masks": np.ascontiguousarray(np.broadcast_to(m[None, :], (128, 20)))})
    return d


def shared_inputs(inp):
    f = lambda a: np.ascontiguousarray(np.asarray(a, np.float32))
    wr = f(inp["w_router"])[0]
    return {
        "params": pack_params(inp), "consts": make_consts(),
        "w_ada": f(inp["w_ada"])[0], "w_in": f(inp["w_in"])[0], "w_ssd_out": f(inp["w_ssd_out"])[0],
        "w_conf_out": f(inp["w_conf_out"])[0], "w_o": f(inp["w_o"])[0],
        "w_router": np.ascontiguousarray(wr.reshape(8, 128, 32).transpose(1, 0, 2).reshape(128, 256)),
        "w_gu": f(inp["w_gu"])[0], "w_down": f(inp["w_down"])[0], "b_down": f(inp["b_down"])[0],
    }


def kernel(**inp):
    nc = build()
    shared = shared_inputs(inp)
    in_maps = [core_inputs(inp, c, shared) for c in range(8)]
    res = run_bass_kernel_spmd(nc, in_maps, core_ids=list(range(8)))
    y_prompt = np.zeros((16, 256, 1024), np.float32)
    y_sample = np.zeros((2, 1024, 1024), np.float32)
    new_state = np.zeros((16, 1, 2, 32, 64, 128), np.float32)
    for c in range(8):
        r = res.results[c]
        b, q = c // 4, c % 4
        y = r["y_own"]
        y_prompt[2 * c] = y[0:256]
        y_prompt[2 * c + 1] = y[256:512]
        y_sample[b, 256 * q:256 * q + 256] = y[512:768]
        ns = r["new_state"].reshape(2, 2, 32, 64, 128)
        new_state[2 * c, 0] = ns[0]
        new_state[2 * c + 1, 0] = ns[1]
    return (y_prompt, y_sample, new_state)
```

```python
import numpy as np
from contextlib import ExitStack
import concourse.bass as bass
import concourse.mybir as mybir
from concourse.bass_utils import run_bass_kernel_spmd

F32, BF16 = mybir.dt.float32, mybir.dt.bfloat16
AF = mybir.ActivationFunctionType
ALU = mybir.AluOpType
AX = mybir.AxisListType

T = 768
A = 1536
EPS = 1e-6
NU = 5
SAME_SYNC = True
N_EXP = 32
DBG = None


def _par_layout():
    off = {}
    n = 0
    def add(name, w):
        nonlocal n
        off[name] = (n, w)
        n += w
    add("norm_mix", 8); add("norm_ffn", 8); add("norm_final", 8); add("b_ada", 48)
    add("conv_w", 32 * 5); add("conv_b", 32); add("ssm_norm_w", 16)
    add("cdw_w", 8 * 31); add("cdw_b", 8); add("ln_w", 8); add("ln_b", 8); add("b_conf_out", 8)
    add("b_gate", 16); add("b_gu", 32 * 16)
    add("dt_bias", 64); add("a_log", 64); add("d_skip", 32); add("b_router", 32)
    return off, n

PAR, NPAR = _par_layout()


def _fm(v, ntile):
    return np.ascontiguousarray(np.asarray(v, np.float32).reshape(ntile, 128).T)


def pack_params(inp):
    P = np.zeros((128, NPAR), np.float32)
    def put(name, arr):
        o, w = PAR[name]
        assert arr.shape == (128, w), (name, arr.shape, w)
        P[:, o:o + w] = arr
    put("norm_mix", _fm(inp["norm_mix"][0], 8)); put("norm_ffn", _fm(inp["norm_ffn"][0], 8))
    put("norm_final", _fm(inp["norm_final"], 8)); put("b_ada", _fm(inp["b_ada"][0], 48))
    cw = np.asarray(inp["ssm_conv_w"][0], np.float32)
    put("conv_w", np.ascontiguousarray(cw.reshape(5, 32, 128).transpose(2, 1, 0).reshape(128, 160)))
    put("conv_b", _fm(inp["ssm_conv_b"][0], 32)); put("ssm_norm_w", _fm(inp["ssm_norm_w"][0], 16))
    dw = np.asarray(inp["conf_dw_w"][0], np.float32)
    put("cdw_w", np.ascontiguousarray(dw.reshape(31, 8, 128).transpose(2, 1, 0).reshape(128, 248)))
    put("cdw_b", _fm(inp["conf_dw_b"][0], 8)); put("ln_w", _fm(inp["conf_ln_w"][0], 8))
    put("ln_b", _fm(inp["conf_ln_b"][0], 8)); put("b_conf_out", _fm(inp["b_conf_out"][0], 8))
    put("b_gate", _fm(inp["b_gate"][0], 16))
    bg = np.asarray(inp["b_gu"][0], np.float32)
    put("b_gu", np.ascontiguousarray(bg.reshape(32, 16, 128).transpose(2, 0, 1).reshape(128, 512)))
    put("dt_bias", np.broadcast_to(np.asarray(inp["dt_bias"][0], np.float32).reshape(1, 64), (128, 64)))
    put("a_log", np.broadcast_to(np.asarray(inp["a_log"][0], np.float32).reshape(1, 64), (128, 64)))
    put("d_skip", np.broadcast_to(np.asarray(inp["d_skip"][0], np.float32).reshape(1, 32), (128, 32)))
    put("b_router", np.broadcast_to(np.asarray(inp["b_router"][0], np.float32).reshape(1, 32), (128, 32)))
    return P


def make_consts():
    k = np.arange(128)[:, None]
    s = np.arange(128)[None, :]
    C = np.zeros((128, 8, 128), np.float32)
    C[:, 0] = (k == s); C[:, 1] = (k <= s); C[:, 2] = (k >= s); C[:, 3] = (k > s); C[:, 4] = (k < s)
    C[:, 5] = 1.0
    C[:, 6] = (s < 64); C[:, 7] = (s >= 64)
    return C.reshape(128, 1024)


class _Trk:
    __slots__ = ("lw", "rd")
    def __init__(self):
        self.lw = None
        self.rd = {}


class Buf:
    def __init__(self, name, share=None):
        self.name = name
        self.t = share.t if share is not None else _Trk()
        self.dsem = None
        self.dn = 0


ENGS = ["pe", "act", "dve", "pool", "sp"]


class Prog:
    def __init__(self):
        self.ops = {e: [] for e in ENGS}

    def op(self, eng, fn, rd=(), wr=(), dma=None):
        deps = set()
        for b in rd:
            if b.t.lw is not None:
                deps.add(b.t.lw)
        for b in wr:
            if b.t.lw is not None:
                deps.add(b.t.lw)
            deps.update(b.t.rd.values())
        idx = len(self.ops[eng])
        if dma is not None:
            dma.dn += 1
            tok = ("dma", dma, dma.dn)
            key = ("dma", id(dma))
        else:
            tok = (eng, idx)
            key = eng
        for b in rd:
            if getattr(b, "is_psum", False) and eng != "pe":
                others = [k for k in b.t.rd if isinstance(k, str) and k not in ("pe", eng)]
                if others:
                    raise AssertionError("PSUM bank %s read by %s and %s between writes" % (b.name, eng, others))
            b.t.rd[key] = tok
        for b in wr:
            b.t.lw = tok
            b.t.rd = {}
        self.ops[eng].append((fn, deps, tok, dma))

    def finalize(self):
        needed = set()
        for e in ENGS:
            for fn, deps, tok, dma in self.ops[e]:
                for d in deps:
                    if d[0] != "dma":
                        if d[0] == e and (e == "pe" or not SAME_SYNC):
                            continue
                        needed.add(d)
        self.needed = needed
        self.sigval = {}
        for e in ENGS:
            c = 0
            for i, (fn, deps, tok, dma) in enumerate(self.ops[e]):
                if tok in needed:
                    c += 1
                    self.sigval[tok] = c

    def emit(self, e, E, sems):
        known = {}
        for fn, deps, tok, dma in self.ops[e]:
            waits = {}
            for d in deps:
                if d[0] == "dma":
                    sem, val = d[1].dsem, 16 * d[2]
                else:
                    if d[0] == e and (e == "pe" or not SAME_SYNC):
                        continue
                    sem, val = sems[d[0]], self.sigval[d]
                k = id(sem)
                if known.get(k, 0) >= val:
                    continue
                if k not in waits or waits[k][1] < val:
                    waits[k] = (sem, val)
            for k, (sem, val) in waits.items():
                E.wait_ge(sem, val)
                known[k] = val
            if fn is None:
                continue
            ins = fn(E)
            if dma is not None:
                ins.then_inc(dma.dsem, 16)
            elif tok in self.needed:
                ins.then_inc(sems[e], 1)


SW_ALPHA = 1.702
SW_LIM = 7.0
BLK = ((0, 512, 0), (512, 256, 1))


def build(dbg=None):
    nc = bass.Bass("TRN2", target_bir_lowering=False)
    P = Prog()
    es = ExitStack()

    def dram(name, shape, kind="ExternalInput", dt=F32):
        return nc.dram_tensor(name, list(shape), dt, kind=kind).ap()

    x_all = dram("x_all", [A, 1024])
    cond_d = dram("cond_fm", [128, 16])
    st0_d = dram("state0", [4096, 128])
    masks_d = dram("masks", [128, 20])
    params_d = dram("params", [128, NPAR])
    consts_d = dram("consts", [128, 1024])
    w_ada_d = dram("w_ada", [1024, 6144])
    w_in_d = dram("w_in", [1024, 10304])
    w_ssd_d = dram("w_ssd_out", [2048, 1024])
    w_conf_d = dram("w_conf_out", [1024, 1024])
    w_o_d = dram("w_o", [1024, 1024])
    w_rt_d = dram("w_router", [128, 256])
    w_gu_d = dram("w_gu", [N_EXP, 1024, 2048])
    w_dn_d = dram("w_down", [N_EXP, 1024, 1024])
    b_dn_d = dram("b_down", [N_EXP, 1024])
    y_out = dram("y_own", [T, 1024], kind="ExternalOutput")
    ns_out = dram("new_state", [2 * 2 * 2048, 128], kind="ExternalOutput")
    dbg_out = dram("dbg", [128, 8192], kind="ExternalOutput") if dbg else None

    def newsem(name):
        return es.enter_context(nc.semaphore(name))

    def sb(name, shape, dt=F32, dma=False):
        t = es.enter_context(nc.sbuf_tensor(name, list(shape), dt))
        b = Buf(name)
        if dma:
            b.dsem = newsem("d_" + name)
        return t, b

    sems = {e: newsem("s_" + e) for e in ENGS}

    def V(eng, method, rd, wr, *args, **kw):
        P.op(eng, lambda E: getattr(E, method)(*args, **kw), rd=rd, wr=wr)

    class Slab:
        def __init__(self, name, words):
            self.name = name
            self.words = words
            self.t = es.enter_context(nc.sbuf_tensor(name, [128, words], F32))
            self.ptr = 0
            self.tiles = []
            self.haz = {}
            self.n = 0

        def reset(self):
            for b in self.tiles:
                if b.t.lw is not None:
                    self.haz[("lw", id(b))] = b.t.lw
                for k, v in b.t.rd.items():
                    if k in self.haz and isinstance(k, str):
                        if self.haz[k][1] < v[1]:
                            self.haz[k] = v
                    else:
                        self.haz[k] = v
            self.tiles = []
            self.ptr = 0

        def carve(self, shape, dt=F32, dma=False):
            per = 1
            for d in shape[1:]:
                per *= d
            words = per if dt == F32 else (per + 1) // 2
            assert self.ptr + words <= self.words, (self.name, self.ptr, words, self.words)
            ap = self.t[:, self.ptr:self.ptr + words]
            self.ptr += words
            if dt != F32:
                ap = ap.bitcast(dt)
            if len(shape) == 3:
                ap = ap.rearrange("p (a b) -> p a b", a=shape[1])
            elif len(shape) == 4:
                ap = ap.rearrange("p (a b c) -> p a b c", a=shape[1], b=shape[2])
            self.n += 1
            b = Buf("%s_%d" % (self.name, self.n))
            b.t.rd = dict(self.haz)
            if dma:
                b.dsem = newsem("d_%s_%d" % (self.name, self.n))
            self.tiles.append(b)
            return ap, b

    cst, cst_b = sb("cst", [128, 8, 128], F32, dma=True)
    cstb, cstb_b = sb("cstb", [128, 8, 128], BF16)
    par, par_b = sb("par", [128, NPAR], F32, dma=True)
    msk, msk_b = sb("msk", [128, 20], F32, dma=True)
    small, small_b = sb("small", [128, 8], F32)
    negm_t, negm_b = sb("negm", [128, 2, 128], BF16)
    negm = negm_t[:]
    NUr = 4
    ring = [sb("ring%d" % i, [128, 8, 1024], BF16, dma=True) for i in range(NUr)]
    ring_i = [0]
    SL0 = Slab("SL0", 6144)
    SL1 = Slab("SL1", 6144)
    SL2 = Slab("SL2", 6144)
    SL3 = Slab("SL3", 6144)
    SLT = Slab("SLT", 7680)

    IDENT, TRI_LE, TRI_GE, TRI_GT, TRI_LT, ONES, HLO, HHI = [cst[:, i, :] for i in range(8)]
    IDENTB = cstb[:, 0, :]
    ONESB = cstb[:, 5, :]
    EPSC = small[:, 0:1]
    ONEC = small[:, 1:2]

    def pc(name, a=0, b=None):
        o, w = PAR[name]
        if b is None:
            b = w
        return par[:, o + a:o + b]

    psum = []
    for i in range(8):
        t = es.enter_context(nc.psum_tensor("ps%d" % i, [128, 512], F32))
        psum.append((t, Buf("ps%d" % i)))
        psum[-1][1].is_psum = True

    def PS(i):
        return psum[i]

    def next_ring():
        i = ring_i[0] % NUr
        ring_i[0] += 1
        return ring[i]

    def load_unit(w2d, c0, ncols, k0=0, nk=8):
        t, b = next_ring()
        src = w2d[k0 * 128:(k0 + nk) * 128, c0:c0 + ncols].rearrange("(kc p) c -> p kc c", p=128)
        P.op("pool", lambda E: E.dma_start(out=t[:, 0:nk, 0:ncols], in_=src), wr=[b], dma=b)
        return t, b

    def load_unit2(w2d, c0, c1, n=512):
        t, b = next_ring()
        for idx, cc in enumerate((c0, c1)):
            src = w2d[:, cc:cc + n].rearrange("(kc p) c -> p kc c", p=128)
            dst = t[:, :, idx * n:(idx + 1) * n]
            if idx == 0:
                P.op("pool", (lambda E, dst=dst, src=src: E.dma_start(out=dst, in_=src)), wr=[b], dma=b)
            else:
                P.op("pool", (lambda E, dst=dst, src=src: E.dma_start(out=dst, in_=src)), dma=b)
                b.t.lw = P.ops["pool"][-1][2]
        return t, b

    def dma_in(tile_ap, b, src, eng="sp"):
        P.op(eng, lambda E: E.dma_start(out=tile_ap, in_=src), wr=[b], dma=b)

    dbg_b = Buf("dbgout")
    if dbg:
        dbg_b.dsem = newsem("d_dbg")
    dbg_col = [0]

    def dump(ap2d, b, n):
        c0 = dbg_col[0]
        dbg_col[0] += n
        dst = dbg_out[:, c0:c0 + n]
        P.op("pool", lambda E: E.dma_start(out=dst, in_=ap2d), rd=[b], dma=dbg_b)
        return c0

    def end_dbg():
        P.op("sp", None, wr=[dbg_b])
        return finish(nc, P, es, sems)

    def rsqrt_from(out_ap, out_b, in_ap, in_b, scale, eng_rd=()):
        V("act", "activation", [in_b, small_b] + list(eng_rd), [out_b], out=out_ap, in_=in_ap, func=AF.Ln,
          scale=scale, bias=EPSC)
        V("act", "activation", [out_b], [out_b], out=out_ap, in_=out_ap, func=AF.Exp, scale=-0.5)

    dma_in(cst[:], cst_b, consts_d.rearrange("p (a b) -> p a b", a=8))
    dma_in(par[:], par_b, params_d)
    dma_in(msk[:], msk_b, masks_d)
    V("dve", "tensor_copy", [cst_b], [cstb_b], out=cstb[:], in_=cst[:])
    V("dve", "memset", [], [small_b], small[:, 0:1], EPS)
    V("dve", "memset", [], [small_b], small[:, 1:2], 1.0)

    cond, cond_b = sb("cond", [128, 8, 2], F32, dma=True)
    scond, scond_b = sb("scond", [128, 8, 2], BF16)
    mod, mod_b = sb("mod", [128, 48, 2], F32)
    modA, modA_b = sb("modA", [128, 2, 8, 2], F32)
    dma_in(cond[:], cond_b, cond_d.rearrange("p (j c) -> p j c", c=2))
    V("act", "activation", [cond_b], [scond_b], out=scond[:], in_=cond[:], func=AF.Silu)
    mod1_b = Buf("mod1")
    modA1_b = Buf("modA1")

    def ada_units(u0, u1, bank, mb_, which_list, mab_):
        pt, pb = PS(bank)
        for u in range(u0, u1):
            ut, ub = load_unit(w_ada_d, u * 1024, 1024)
            for jt in range(8):
                col = (u * 8 + jt) * 2
                for k in range(8):
                    V("pe", "matmul", [ub, scond_b], [pb], pt[:, col:col + 2],
                      lhsT=ut[:, k, jt * 128:(jt + 1) * 128], rhs=scond[:, k, :], start=(k == 0), stop=(k == 7))
        j0, j1 = u0 * 8, u1 * 8
        V("dve", "tensor_tensor", [pb, par_b], [mb_], out=mod[:, j0:j1, :],
          in0=pt[:, 2 * j0:2 * j1].rearrange("p (j c) -> p j c", c=2),
          in1=pc("b_ada", j0, j1).unsqueeze(2).to_broadcast([128, j1 - j0, 2]), op=ALU.add)
        for which, nm, jj0 in which_list:
            V("dve", "tensor_scalar", [mb_], [mab_], out=modA[:, which], in0=mod[:, jj0:jj0 + 8, :],
              scalar1=1.0, scalar2=None, op0=ALU.add)
            V("dve", "tensor_tensor", [mab_, par_b], [mab_], out=modA[:, which], in0=modA[:, which],
              in1=pc(nm).unsqueeze(2).to_broadcast([128, 8, 2]), op=ALU.mult)

    ada_units(0, 2, 0, mod1_b, [(0, "norm_mix", 8)], modA1_b)

    def A1(j, c): return modA[:, 0, j, c:c + 1]
    def S1(j, c): return mod[:, j, c:c + 1]
    def G1(j, c): return mod[:, 16 + j, c:c + 1]
    def A2(j, c): return modA[:, 1, j, c:c + 1]
    def S2(j, c): return mod[:, 24 + j, c:c + 1]
    def G2(j, c): return mod[:, 40 + j, c:c + 1]

    hT, hT_all = SL0.carve([128, 8, A], BF16)
    hTb = [Buf("hT_c%d" % i) for i in range(12)]
    for b_ in hTb:
        SL0.tiles.append(b_)
    xt = [SL2.carve([128, 1024], F32, dma=True) for _ in range(2)]
    xn = [SL2.carve([128, 1024], F32) for _ in range(2)]
    junk, junk_b = SL2.carve([128, 1024], BF16)
    ssq, ssq_b = sb("ssq", [128, 12], F32)
    rstd, rstd_b = sb("rstd", [128, 12], F32)
    V("dve", "memset", [], [ssq_b], ssq[:], 0.0)

    def transpose_x_tile(xnt, xnb, evac):
        for hb in range(2):
            pt, pb = PS(1 + hb)
            for jj in range(4):
                j = hb * 4 + jj
                V("pe", "transpose", [xnb, cst_b], [pb], out=pt[:, jj * 128:(jj + 1) * 128],
                  in_=xnt[:, j * 128:(j + 1) * 128], identity=IDENT)
            for jj in range(4):
                evac(hb * 4 + jj, pt[:, jj * 128:(jj + 1) * 128], pb)

    for i in range(12):
        xtt, xtb = xt[i % 2]
        xnt, xnb = xn[i % 2]
        cnd = 0 if i < 4 else 1
        dma_in(xtt, xtb, x_all[i * 128:(i + 1) * 128, :])
        V("act", "activation", [xtb], [junk_b, ssq_b], out=junk, in_=xtt, func=AF.Square,
          accum_out=ssq[:, i:i + 1])
        rsqrt_from(rstd[:, i:i + 1], rstd_b, ssq[:, i:i + 1], ssq_b, 1.0 / 1024)
        V("act", "activation", [xtb, rstd_b], [xnb], out=xnt, in_=xtt, func=AF.Copy, scale=rstd[:, i:i + 1])

        def evac(j, pap, pb, i=i, cnd=cnd):
            dst = hT[:, j, i * 128:(i + 1) * 128]
            if j < 4:
                V("dve", "tensor_scalar", [pb, modA1_b, mod1_b], [hTb[i]], out=dst, in0=pap,
                  scalar1=A1(j, cnd), scalar2=S1(j, cnd), op0=ALU.mult, op1=ALU.add)
            else:
                V("act", "activation", [pb, modA1_b, mod1_b], [hTb[i]], out=dst, in_=pap, func=AF.Identity,
                  scale=A1(j, cnd), bias=S1(j, cnd))
        transpose_x_tile(xnt, xnb, evac)

    ada_units(2, 6, 7, mod_b, [(1, "norm_ffn", 32)], modA_b)

    if dbg == "hT":
        dump(hT[:, 0, :], hT_all, 1536) if False else None
        for b_ in hTb:
            pass
        P.op("pool", lambda E: E.dma_start(out=dbg_out[:, 0:1536], in_=hT[:, 0, :]), rd=hTb, dma=dbg_b)
        P.op("pool", lambda E: E.dma_start(out=dbg_out[:, 1536:3072], in_=hT[:, 7, :]), rd=hTb, dma=dbg_b)
        P.op("pool", lambda E: E.dma_start(out=dbg_out[:, 3072:3168], in_=mod[:].rearrange("p j c -> p (j c)")),
             rd=[mod_b], dma=dbg_b)
        return end_dbg()

    SL2.reset()
    dtv, dtv_b = SL3.carve([128, 12, 64])
    raw, raw_b = SL3.carve([128, 12, 192])
    Eown, Eown_b = SL3.carve([128, 6, 192])
    wdec, wdec_b = SL3.carve([128, 6, 64])
    wrest, wrest_b = SL3.carve([128, 6, 64])
    lw, lw_b = SL3.carve([128, 6, 64])
    wfin, wfin_b = SL3.carve([128, 4, 32])
    ptot, ptot_b = SL3.carve([128, 64])
    aneg, aneg_b = SL3.carve([128, 64])
    negcs_t, negcs_b = sb("negcs", [128, 6, 64], F32)
    negcs = negcs_t[:]
    cs_hi, cs_hi_b = SL3.carve([128, 6, 64], BF16)
    cs_lo, cs_lo_b = SL3.carve([128, 6, 64], BF16)
    adt, adt_b = SLT.carve([128, 12, 64])
    t64, t64_b = SLT.carve([128, 12, 64])
    lpu, lpu_b = SLT.carve([128, 12, 64])
    lpl, lpl_b = SLT.carve([128, 12, 64])

    dtu, dtu_b = load_unit(w_in_d, 6144, 64)
    for i in range(12):
        pt, pb = PS(3 + (i // 8))
        col = (i % 8) * 64
        for k in range(8):
            V("pe", "matmul", [hTb[i], dtu_b], [pb], pt[:, col:col + 64], lhsT=hT[:, k, i * 128:(i + 1) * 128],
              rhs=dtu[:, k, 0:64], start=(k == 0), stop=(k == 7))
    V("dve", "tensor_tensor", [PS(3)[1], par_b], [dtv_b], out=dtv[:, 0:8, :],
      in0=PS(3)[0][:, 0:512].rearrange("p (i c) -> p i c", c=64),
      in1=pc("dt_bias").unsqueeze(1).to_broadcast([128, 8, 64]), op=ALU.add)
    V("dve", "tensor_tensor", [PS(4)[1], par_b], [dtv_b], out=dtv[:, 8:12, :],
      in0=PS(4)[0][:, 0:256].rearrange("p (i c) -> p i c", c=64),
      in1=pc("dt_bias").unsqueeze(1).to_broadcast([128, 4, 64]), op=ALU.add)
    V("act", "activation", [dtv_b], [t64_b], out=t64, in_=dtv, func=AF.Abs)
    V("act", "activation", [t64_b], [t64_b], out=t64, in_=t64, func=AF.Exp, scale=-1.0)
    V("dve", "tensor_scalar", [t64_b], [lpu_b], out=lpu, in0=t64, scalar1=1.0, scalar2=None, op0=ALU.add)
    V("act", "activation", [lpu_b], [lpl_b], out=lpl, in_=lpu, func=AF.Ln)
    V("dve", "tensor_scalar", [lpu_b], [lpu_b], out=lpu, in0=lpu, scalar1=-1.0, scalar2=1e-30, op0=ALU.add,
      op1=ALU.add)
    V("dve", "reciprocal", [lpu_b], [lpu_b], out=lpu, in_=lpu)
    V("dve", "scalar_tensor_tensor", [lpl_b, lpu_b], [lpl_b], out=lpl, in0=lpl, scalar=1e-30, in1=lpu,
      op0=ALU.add, op1=ALU.mult)
    V("dve", "tensor_tensor", [t64_b, lpl_b], [t64_b], out=t64, in0=t64, in1=lpl, op=ALU.mult)
    V("dve", "scalar_tensor_tensor", [dtv_b, t64_b], [dtv_b], out=dtv, in0=dtv, scalar=0.0, in1=t64,
      op0=ALU.max, op1=ALU.add)
    if dbg == "dt1":
        dump(dtv.rearrange("p a b -> p (a b)"), dtv_b, 768)
        return end_dbg()
    V("dve", "tensor_tensor", [dtv_b, msk_b], [dtv_b], out=dtv[:, 6:12, 0:32], in0=dtv[:, 6:12, 0:32],
      in1=msk[:, 8:14].unsqueeze(2).to_broadcast([128, 6, 32]), op=ALU.mult)
    V("dve", "tensor_tensor", [dtv_b, msk_b], [dtv_b], out=dtv[:, 6:12, 32:64], in0=dtv[:, 6:12, 32:64],
      in1=msk[:, 14:20].unsqueeze(2).to_broadcast([128, 6, 32]), op=ALU.mult)
    V("act", "activation", [par_b], [aneg_b], out=aneg, in_=pc("a_log"), func=AF.Exp)
    V("dve", "scalar_tensor_tensor", [dtv_b, aneg_b], [adt_b], out=adt, in0=dtv, scalar=-1.0,
      in1=aneg.unsqueeze(1).to_broadcast([128, 12, 64]), op0=ALU.mult, op1=ALU.mult)
    adt_hi, adt_hi_b = SLT.carve([128, 12, 64], BF16)
    adt_lo, adt_lo_b = SLT.carve([128, 12, 64], BF16)
    V("dve", "tensor_copy", [adt_b], [adt_hi_b], out=adt_hi, in_=adt)
    V("dve", "tensor_tensor", [adt_b, adt_hi_b], [adt_lo_b], out=adt_lo, in0=adt, in1=adt_hi, op=ALU.subtract)
    if dbg == "dt2":
        dump(adt.rearrange("p a b -> p (a b)"), adt_b, 768)
        dump(adt_lo.rearrange("p a b -> p (a b)"), adt_lo_b, 768)
        return end_dbg()
    for i in range(12):
        pt, pb = PS(5 + i % 2)
        for (c0, c1, tri, a0, a1) in ((0, 32, 1, 0, 32), (32, 64, 2, 32, 64), (64, 96, 3, 0, 32),
                                      (96, 128, 4, 32, 64), (128, 192, 5, 0, 64)):
            V("pe", "matmul", [adt_hi_b, cstb_b], [pb], pt[:, c0:c1], lhsT=cstb[:, tri, :],
              rhs=adt_hi[:, i, a0:a1], start=True, stop=False)
            V("pe", "matmul", [adt_lo_b, cstb_b], [pb], pt[:, c0:c1], lhsT=cstb[:, tri, :],
              rhs=adt_lo[:, i, a0:a1], start=False, stop=True)
        V("dve", "tensor_copy", [pb], [raw_b], out=raw[:, i, :], in_=pt[:, 0:192])
        if i < 6:
            V("act", "activation", [raw_b], [Eown_b], out=Eown[:, i, :], in_=raw[:, i, :], func=AF.Exp)
    if dbg == "dt3":
        dump(raw.rearrange("p a b -> p (a b)"), raw_b, 2304)
        dump(Eown.rearrange("p a b -> p (a b)"), Eown_b, 1152)
        return end_dbg()
    V("dve", "tensor_tensor", [dtv_b, Eown_b], [wdec_b], out=wdec, in0=dtv[:, 0:6, :], in1=Eown[:, :, 64:128],
      op=ALU.mult)
    V("dve", "tensor_scalar", [raw_b], [negcs_b], out=negcs, in0=raw[:, 0:6, 0:64], scalar1=-1.0, scalar2=None,
      op0=ALU.mult)
    V("dve", "tensor_scalar", [cst_b], [negm_b], out=negm, in0=cst[:, 3:5, :], scalar1=-65536.0, scalar2=None,
      op0=ALU.mult)
    V("dve", "tensor_copy", [raw_b], [cs_hi_b], out=cs_hi, in_=raw[:, 0:6, 0:64])
    V("dve", "tensor_tensor", [raw_b, cs_hi_b], [cs_lo_b], out=cs_lo, in0=raw[:, 0:6, 0:64], in1=cs_hi,
      op=ALU.subtract)
    V("dve", "memset", [], [lw_b], lw, 0.0)
    for i in range(10, 5, -1):
        V("dve", "tensor_tensor", [lw_b, raw_b], [lw_b], out=lw[:, i - 6, 0:32], in0=lw[:, i - 5, 0:32],
          in1=raw[:, i + 1, 128:160], op=ALU.add)
    for i in range(7, 12):
        V("dve", "tensor_tensor", [lw_b, raw_b], [lw_b], out=lw[:, i - 6, 32:64], in0=lw[:, i - 7, 32:64],
          in1=raw[:, i - 1, 160:192], op=ALU.add)
    V("dve", "tensor_tensor", [lw_b, raw_b], [ptot_b], out=ptot[:, 0:32], in0=lw[:, 0, 0:32],
      in1=raw[:, 6, 128:160], op=ALU.add)
    V("dve", "tensor_tensor", [lw_b, raw_b], [ptot_b], out=ptot[:, 32:64], in0=lw[:, 5, 32:64],
      in1=raw[:, 11, 160:192], op=ALU.add)
    V("act", "activation", [ptot_b], [ptot_b], out=ptot, in_=ptot, func=AF.Exp)
    V("dve", "tensor_tensor", [lw_b, raw_b], [wrest_b], out=wrest, in0=lw, in1=raw[:, 6:12, 64:128], op=ALU.add)
    V("act", "activation", [wrest_b], [wrest_b], out=wrest, in_=wrest, func=AF.Exp)
    V("dve", "tensor_tensor", [wrest_b, dtv_b], [wrest_b], out=wrest, in0=wrest, in1=dtv[:, 6:12, :], op=ALU.mult)
    for s in range(2):
        c0, c1 = 2 * s, 2 * s + 1
        V("dve", "tensor_tensor", [wdec_b, Eown_b], [wfin_b], out=wfin[:, 2 * s, :], in0=wdec[:, c0, 0:32],
          in1=Eown[:, c1, 128:160], op=ALU.mult)
        V("dve", "tensor_tensor", [wdec_b, Eown_b], [wfin_b], out=wfin[:, 2 * s + 1, :], in0=wdec[:, c1, 32:64],
          in1=Eown[:, c0, 160:192], op=ALU.mult)

    if dbg == "dt":
        dump(dtv.rearrange("p a b -> p (a b)"), dtv_b, 768)
        dump(raw.rearrange("p a b -> p (a b)"), raw_b, 2304)
        dump(wdec.rearrange("p a b -> p (a b)"), wdec_b, 384)
        dump(wrest.rearrange("p a b -> p (a b)"), wrest_b, 384)
        dump(ptot, ptot_b, 64)
        dump(wfin.rearrange("p a b -> p (a b)"), wfin_b, 128)
        return end_dbg()

    yT, yT_b = SL1.carve([128, 16, T], BF16)
    yTb = [Buf("yT_p%d" % p) for p in range(4)]
    for b_ in yTb:
        SL1.tiles.append(b_)
    st0v = st0_d.rearrange("(d g t p) n -> g p d t n", d=2, g=8, t=2, p=128)
    stg_i = [0]
    out_bufs = []

    for p in range(4):
        SL2.reset()
        SLT.reset()
        x_tok, x_tok_b = SL2.carve([128, 6, 512], BF16)
        B_tok, B_tok_b = SL2.carve([128, 6, 256], BF16)
        BT, BT_b = SL2.carve([128, 2, T], BF16)
        CT, CT_b = SL2.carve([128, 2, T], BF16)
        H, H_b = SL2.carve([128, 2, 512], F32)
        ents = [SL2.carve([128, 512], BF16) for _ in range(4)]
        pres = [SLT.carve([128, 1576], BF16) for _ in range(2)]
        fms = [SLT.carve([128, A], BF16) for _ in range(2)]
        pc_i = [0]
        diags = [SLT.carve([128, 5, 128], BF16) for _ in range(2)]
        dg_i = [0]
        x_rest, x_rest_b = SLT.carve([128, 6, 256], BF16)
        B_rest, B_rest_b = SLT.carve([128, 6, 256], BF16)
        xsr = [SLT.carve([128, 256], BF16) for _ in range(4)]
        h0t, h0t_b = SLT.carve([128, 2, 2, 128], F32, dma=True)
        htmp, htmp_b = SLT.carve([128, 512], F32)
        for (pre_, pre_b_) in pres:
            preP_ = pre_[:, 0:520].rearrange("p (s t) -> p s t", s=2)
            V("dve", "memset", [], [pre_b_], preP_[:, :, 0:2], 0.0)
            V("dve", "memset", [], [pre_b_], preP_[:, :, 258:260], 0.0)

        xu = load_unit(w_in_d, 2048 + 512 * p, 512)
        bu = load_unit(w_in_d, 4096 + 256 * p, 256)
        cu = load_unit(w_in_d, 5120 + 256 * p, 256)

        def proj_conv(unit, jt, cidx, sil):
            ut, ub = unit
            pre, pre_b = pres[pc_i[0] % 2]
            fm, fm_b = fms[pc_i[0] % 2]
            pc_i[0] += 1
            preP = pre[:, 0:520].rearrange("p (s t) -> p s t", s=2)
            preS = pre[:, 520:1576].rearrange("p (s t) -> p s t", s=8)
            for tb in range(3):
                pt, pb = PS((0, 1, 5)[tb])
                for k in range(8):
                    V("pe", "matmul", [ub] + hTb[4 * tb:4 * tb + 4], [pb], pt[:, :],
                      lhsT=ut[:, k, jt * 128:(jt + 1) * 128], rhs=hT[:, k, tb * 512:(tb + 1) * 512],
                      start=(k == 0), stop=(k == 7))
                if tb == 0:
                    dst = preP[:, :, 2:258]
                    src = pt[:, :].rearrange("p (s t) -> p s t", s=2)
                else:
                    dst = preS[:, 4 * (tb - 1):4 * tb, 2:130]
                    src = pt[:, :].rearrange("p (s t) -> p s t", s=4)
                V("act", "activation", [pb], [pre_b], out=dst, in_=src, func=AF.Copy)
            V("dve", "tensor_tensor", [pre_b, msk_b], [pre_b], out=preS[:, 1:8, 0:2], in0=preS[:, 0:7, 128:130],
              in1=msk[:, 1:8].unsqueeze(2).to_broadcast([128, 7, 2]), op=ALU.mult)
            V("dve", "tensor_scalar", [pre_b, msk_b], [pre_b], out=preS[:, 0, 0:2], in0=preS[:, 7, 128:130],
              scalar1=msk[:, 0:1], scalar2=None, op0=ALU.mult)
            V("dve", "tensor_tensor", [pre_b, msk_b], [pre_b], out=preS[:, 0:7, 130:132], in0=preS[:, 1:8, 2:4],
              in1=msk[:, 1:8].unsqueeze(2).to_broadcast([128, 7, 2]), op=ALU.mult)
            V("dve", "tensor_scalar", [pre_b, msk_b], [pre_b], out=preS[:, 7, 130:132], in0=preS[:, 0, 2:4],
              scalar1=msk[:, 0:1], scalar2=None, op0=ALU.mult)
            o, _w = PAR["conv_w"]
            dg, dgb = diags[dg_i[0] % 2]
            dg_i[0] += 1
            for k in range(5):
                V("dve", "tensor_scalar", [cstb_b, par_b], [dgb], out=dg[:, k, :], in0=IDENTB,
                  scalar1=par[:, o + cidx * 5 + k:o + cidx * 5 + k + 1], scalar2=None, op0=ALU.mult)
            for tb in range(3):
                pt, pb = PS((6, 7, 4)[tb])
                for k in range(5):
                    if tb == 0:
                        rhs = preP[:, :, k:k + 256]
                    else:
                        rhs = preS[:, 4 * (tb - 1):4 * tb, k:k + 128]
                    V("pe", "matmul", [pre_b, dgb], [pb], pt[:, :], lhsT=dg[:, k, :], rhs=rhs,
                      start=(k == 0), stop=(k == 4))
                sil(tb, pt, pb, fm, fm_b)
            return fm, fm_b

        def transposes_to(fm, fm_b, dst_own, dst_own_b, dst_rest, dst_rest_b):
            for half, (dst, dstb) in enumerate(((dst_own, dst_own_b), (dst_rest, dst_rest_b))):
                pt, pb = PS(2 + half)
                ptb = pt[:, :].bitcast(BF16)
                for cc in range(6):
                    c = half * 6 + cc
                    V("pe", "transpose", [fm_b, cstb_b], [pb], out=ptb[:, cc * 128:(cc + 1) * 128],
                      in_=fm[:, c * 128:(c + 1) * 128], identity=IDENTB)
                V("act", "activation", [pb], [dstb], out=dst,
                  in_=ptb[:, 0:768].rearrange("p (c n) -> p c n", c=6), func=AF.Copy)

        cb_o = PAR["conv_b"][0]
        for gl in range(2):
            cidx = 16 + 2 * p + gl
            def sil(tb, pt, pb, fm, fm_b, cidx=cidx):
                V("act", "activation", [pb, par_b], [fm_b], out=fm[:, tb * 512:(tb + 1) * 512], in_=pt[:, :],
                  func=AF.Silu, bias=par[:, cb_o + cidx:cb_o + cidx + 1], scale=1.0)
            fm, fm_b = proj_conv(bu, gl, cidx, sil)
            V("pool", "tensor_copy", [fm_b], [BT_b], out=BT[:, gl, :], in_=fm[:, 0:T])
            transposes_to(fm, fm_b, B_tok[:, :, gl * 128:(gl + 1) * 128], B_tok_b, B_rest[:, :, gl * 128:(gl + 1) * 128],
                          B_rest_b)
        for gl in range(2):
            cidx = 24 + 2 * p + gl
            def sil(tb, pt, pb, fm, fm_b, cidx=cidx, gl=gl):
                if tb == 0:
                    V("act", "activation", [pb, par_b], [CT_b], out=CT[:, gl, 0:512], in_=pt[:, :], func=AF.Silu,
                      bias=par[:, cb_o + cidx:cb_o + cidx + 1], scale=1.0)
                elif tb == 1:
                    V("act", "activation", [pb, par_b], [CT_b], out=CT[:, gl, 512:768], in_=pt[:, 0:256],
                      func=AF.Silu, bias=par[:, cb_o + cidx:cb_o + cidx + 1], scale=1.0)
            proj_conv(cu, gl, cidx, sil)
        for xl in range(4):
            cidx = 4 * p + xl
            gl = xl // 2
            g = 2 * p + gl
            def sil(tb, pt, pb, fm, fm_b, cidx=cidx):
                V("act", "activation", [pb, par_b], [fm_b], out=fm[:, tb * 512:(tb + 1) * 512], in_=pt[:, :],
                  func=AF.Silu, bias=par[:, cb_o + cidx:cb_o + cidx + 1], scale=1.0)
            fm, fm_b = proj_conv(xu, xl, cidx, sil)
            transposes_to(fm, fm_b, x_tok[:, :, xl * 128:(xl + 1) * 128], x_tok_b,
                          x_rest[:, :, (xl % 2) * 128:(xl % 2 + 1) * 128], x_rest_b)
            if xl % 2 == 1:
                pt4, pb4 = PS(4)
                ri = 0
                for d in range(2):
                    for i in range(6, 12):
                        xs_t, xs_b = xsr[ri % 4]
                        ri += 1
                        V("dve", "tensor_tensor", [x_rest_b, wrest_b], [xs_b],
                          out=xs_t.rearrange("p (h q) -> p h q", h=4),
                          in0=x_rest[:, i - 6, :].rearrange("p (h q) -> p h q", h=4),
                          in1=wrest[:, i - 6, d * 32 + 4 * g:d * 32 + 4 * g + 4].unsqueeze(2).to_broadcast(
                              [128, 4, 64]), op=ALU.mult)
                        V("pe", "matmul", [B_rest_b, xs_b], [pb4], pt4[:, d * 256:(d + 1) * 256],
                          lhsT=B_rest[:, i - 6, gl * 128:(gl + 1) * 128], rhs=xs_t, start=(i == 6), stop=(i == 11))
                for d in range(2):
                    dma_in(h0t[:, d], h0t_b, st0v[g][:, d])
                pt5, pb5 = PS(5)
                for d in range(2):
                    for t_ in range(2):
                        V("pe", "transpose", [h0t_b, cst_b], [pb5],
                          out=pt5[:, (d * 2 + t_) * 128:(d * 2 + t_ + 1) * 128], in_=h0t[:, d, t_, :],
                          identity=IDENT)
                V("dve", "tensor_tensor", [pb5, ptot_b], [htmp_b],
                  out=htmp.rearrange("p (d h q) -> p d h q", d=2, h=4),
                  in0=pt5[:, :].rearrange("p (d h q) -> p d h q", d=2, h=4),
                  in1=ptot.rearrange("p (d h) -> p d h", d=2)[:, :, 4 * g:4 * g + 4].unsqueeze(3).to_broadcast(
                      [128, 2, 4, 64]), op=ALU.mult)
                V("dve", "tensor_tensor", [pb4, htmp_b], [H_b], out=H[:, :, gl * 256:(gl + 1) * 256],
                  in0=pt4[:, :].rearrange("p (d c) -> p d c", d=2), in1=htmp.rearrange("p (d c) -> p d c", d=2),
                  op=ALU.add)

        if dbg == "chain" and p == 0:
            dump(x_tok.rearrange("p a b -> p (a b)"), x_tok_b, 3072)
            dump(B_tok.rearrange("p a b -> p (a b)"), B_tok_b, 1536)
            dump(H.rearrange("p a b -> p (a b)"), H_b, 1024)
            dump(CT[:, 0, :], CT_b, 768)
            dump(BT[:, 1, :], BT_b, 768)
            return end_dbg()

        SLT.reset()
        xs_pool = [SLT.carve([128, 512], BF16) for _ in range(4)]
        xs_i = [0]
        scss = [SLT.carve([128, 2, 128], F32) for _ in range(2)]
        lt4s = [SLT.carve([128, 4, 128], BF16) for _ in range(4)]
        lt4bufs = [[Buf("lt4_%d_%d" % (a_, b_)) for b_ in range(4)] for a_ in range(4)]
        for a_ in range(4):
            for b_ in lt4bufs[a_]:
                b_.t.rd = dict(lt4s[a_][1].t.rd)
                SLT.tiles.append(b_)
        mt4s = [SLT.carve([128, 4, 128], BF16) for _ in range(4)]
        yaccs = [SLT.carve([128, 512], F32) for _ in range(2)]
        ytmp, ytmp_b = SLT.carve([128, 512], F32)
        ybfs = [SLT.carve([128, 512], BF16) for _ in range(2)]
        yc_i = [0]
        stg = [SLT.carve([128, 4, 128], F32, dma=True) for _ in range(2)]

        def hb8(ap2d):
            return ap2d.rearrange("p (h q) -> p h q", h=8)

        def bc8(ap8):
            return ap8.unsqueeze(2).to_broadcast([128, 8, 64])

        def make_xs(c, wap, wbufs):
            t_, b_ = xs_pool[xs_i[0] % 4]
            xs_i[0] += 1
            V("dve", "tensor_tensor", [x_tok_b] + wbufs, [b_], out=hb8(t_), in0=hb8(x_tok[:, c, :]), in1=bc8(wap),
              op=ALU.mult)
            return t_, b_

        def state_mm(c, xs):
            pt, pb = PS(6)
            for gl in range(2):
                V("pe", "matmul", [B_tok_b, xs[1]], [pb], pt[:, gl * 256:(gl + 1) * 256],
                  lhsT=B_tok[:, c, gl * 128:(gl + 1) * 128], rhs=xs[0][:, gl * 256:(gl + 1) * 256],
                  start=True, stop=True)
            return pt, pb

        def wd(c, d):
            return wdec[:, c, d * 32 + 8 * p:d * 32 + 8 * p + 8]

        def ent_from_state(c, d, ent, hdir=None, etot_c=None):
            xs = make_xs(c, wd(c, d), [wdec_b])
            pt, pb = state_mm(c, xs)
            if hdir is None:
                V("act", "activation", [pb], [ent[1]], out=ent[0], in_=pt[:, :], func=AF.Copy)
            else:
                V("dve", "tensor_tensor", [H_b, Eown_b], [ytmp_b], out=hb8(ytmp), in0=hb8(H[:, hdir, :]),
                  in1=bc8(Eown[:, etot_c, 128 + hdir * 32 + 8 * p:128 + hdir * 32 + 8 * p + 8]), op=ALU.mult)
                V("dve", "tensor_tensor", [pb, ytmp_b], [ent[1]], out=ent[0], in0=pt[:, :], in1=ytmp, op=ALU.add)

        def finals(s, d):
            c0, c1 = 2 * s, 2 * s + 1
            if d == 0:
                xa = make_xs(c0, wfin[:, 2 * s, 8 * p:8 * p + 8], [wfin_b]); ca = c0
                xb = make_xs(c1, wd(c1, 0), [wdec_b]); cb = c1
            else:
                xa = make_xs(c1, wfin[:, 2 * s + 1, 8 * p:8 * p + 8], [wfin_b]); ca = c1
                xb = make_xs(c0, wd(c0, 1), [wdec_b]); cb = c0
            pt, pb = PS(5)
            for i in range(4):
                gl = i // 2
                V("pe", "matmul", [xa[1], B_tok_b], [pb], pt[:, i * 128:(i + 1) * 128],
                  lhsT=xa[0][:, i * 128:(i + 1) * 128], rhs=B_tok[:, ca, gl * 128:(gl + 1) * 128],
                  start=True, stop=False)
                V("pe", "matmul", [xb[1], B_tok_b], [pb], pt[:, i * 128:(i + 1) * 128],
                  lhsT=xb[0][:, i * 128:(i + 1) * 128], rhs=B_tok[:, cb, gl * 128:(gl + 1) * 128],
                  start=False, stop=True)
            st_, sb_ = stg[stg_i[0] % 2]
            stg_i[0] += 1
            V("act", "activation", [pb], [sb_], out=st_, in_=pt[:, :].rearrange("p (i n) -> p i n", i=4),
              func=AF.Copy)
            r0 = (s * 2 + d) * 2048 + 512 * p
            dst = ns_out[r0:r0 + 512, :].rearrange("(i r) n -> r i n", r=128)
            P.op("sp", (lambda E, dst=dst, st_=st_: E.dma_start(out=dst, in_=st_)), rd=[sb_], dma=sb_)
            if sb_ not in out_bufs:
                out_bufs.append(sb_)

        yn_i = [0]

        def y_front(c, ent_f, ent_b):
            scs, scs_b = scss[yc_i[0] % 2]
            yacc, yacc_b = yaccs[yc_i[0] % 2]
            ybf, ybf_b = ybfs[yc_i[0] % 2]
            pt3, pb3 = PS((3, 7)[yc_i[0] % 2])
            yc_i[0] += 1
            pt0, pb0 = PS(0)
            for gl in range(2):
                V("pe", "matmul", [BT_b, CT_b], [pb0], pt0[:, gl * 128:(gl + 1) * 128],
                  lhsT=BT[:, gl, c * 128:(c + 1) * 128], rhs=CT[:, gl, c * 128:(c + 1) * 128], start=True, stop=True)
            V("act", "activation", [pb0], [scs_b], out=scs, in_=pt0[:, 0:256].rearrange("p (g n) -> p g n", g=2),
              func=AF.Copy)
            xsd = [make_xs(c, dtv[:, c, d * 32 + 8 * p:d * 32 + 8 * p + 8], [dtv_b]) for d in range(2)]
            for gl in range(2):
                mts = []
                for d in range(2):
                    n = yn_i[0]
                    yn_i[0] += 1
                    ptS, pbS = PS((1, 2)[n % 2])
                    lt4, _ = lt4s[n % 4]
                    ltb = lt4bufs[n % 4]
                    mt4, mt4b = mt4s[n % 4]
                    mts.append((mt4, mt4b))
                    for hh in range(4):
                        hl = gl * 4 + hh
                        ci = d * 32 + 8 * p + hl
                        dst = ptS[:, hh * 128:(hh + 1) * 128]
                        V("pe", "matmul", [cs_hi_b, cstb_b], [pbS], dst,
                          lhsT=cs_hi[:, c, ci:ci + 1].to_broadcast([128, 128]), rhs=IDENTB, start=True, stop=False)
                        V("pe", "matmul", [cs_lo_b, cstb_b], [pbS], dst,
                          lhsT=cs_lo[:, c, ci:ci + 1].to_broadcast([128, 128]), rhs=IDENTB, start=False, stop=False)
                        V("pe", "matmul", [negm_b, cstb_b], [pbS], dst, lhsT=IDENTB, rhs=negm[:, d, :],
                          start=False, stop=True)
                    for hh in range(4):
                        hl = gl * 4 + hh
                        ci = d * 32 + 8 * p + hl
                        V("act", "activation", [pbS, negcs_b], [ltb[hh]], out=lt4[:, hh, :],
                          in_=ptS[:, hh * 128:(hh + 1) * 128], func=AF.Exp, bias=negcs[:, c, ci:ci + 1], scale=1.0)
                    V("dve", "tensor_tensor", ltb + [scs_b], [mt4b], out=mt4, in0=lt4,
                      in1=scs[:, gl, :].unsqueeze(1).to_broadcast([128, 4, 128]), op=ALU.mult)
                for hh in range(4):
                    hl = gl * 4 + hh
                    for d in range(2):
                        V("pe", "matmul", [mts[d][1], xsd[d][1]], [pb3], pt3[:, hl * 64:(hl + 1) * 64],
                          lhsT=mts[d][0][:, hh, :], rhs=xsd[d][0][:, hl * 64:(hl + 1) * 64],
                          start=(d == 0), stop=(d == 1))
            return (c, ent_f, ent_b, yacc, yacc_b, ybf, ybf_b, pt3, pb3)

        def y_tail(ctx):
            (c, ent_f, ent_b, yacc, yacc_b, ybf, ybf_b, pt3, pb3) = ctx
            for d, ent in ((0, ent_f), (1, ent_b)):
                if ent is None:
                    continue
                ptO, pbO = PS(4 + d)
                for gl in range(2):
                    V("pe", "matmul", [CT_b, ent[1]], [pbO], ptO[:, gl * 256:(gl + 1) * 256],
                      lhsT=CT[:, gl, c * 128:(c + 1) * 128], rhs=ent[0][:, gl * 256:(gl + 1) * 256],
                      start=True, stop=True)
            V("dve", "tensor_tensor", [x_tok_b, par_b], [yacc_b], out=hb8(yacc), in0=hb8(x_tok[:, c, :]),
              in1=bc8(pc("d_skip")[:, 8 * p:8 * p + 8]), op=ALU.mult)
            V("dve", "tensor_tensor", [pb3, yacc_b], [yacc_b], out=yacc, in0=pt3[:, :], in1=yacc, op=ALU.add)
            for d, ent in ((0, ent_f), (1, ent_b)):
                if ent is None:
                    continue
                ptO, pbO = PS(4 + d)
                V("dve", "tensor_tensor", [pbO, Eown_b], [ytmp_b], out=hb8(ytmp), in0=hb8(ptO[:, :]),
                  in1=bc8(Eown[:, c, d * 32 + 8 * p:d * 32 + 8 * p + 8]), op=ALU.mult)
                V("dve", "tensor_tensor", [ytmp_b, yacc_b], [yacc_b], out=yacc, in0=yacc, in1=ytmp, op=ALU.add)
            V("act", "activation", [yacc_b], [ybf_b], out=ybf, in_=yacc, func=AF.Copy)
            pt6, pb6 = PS(6)
            pt6b = pt6[:, :].bitcast(BF16)
            for j in range(4):
                V("pe", "transpose", [ybf_b, cstb_b], [pb6], out=pt6b[:, j * 128:(j + 1) * 128],
                  in_=ybf[:, j * 128:(j + 1) * 128], identity=IDENTB)
            V("act", "activation", [pb6], [yTb[p]], out=yT[:, 4 * p:4 * p + 4, c * 128:(c + 1) * 128],
              in_=pt6b[:, 0:512].rearrange("p (j n) -> p j n", j=4), func=AF.Copy)

        jobs = []
        for s in range(2):
            c0, c1 = 2 * s, 2 * s + 1
            ent_a, ent_bb = ents[2 * s], ents[2 * s + 1]

            def pre(s=s, c0=c0, c1=c1, ent_a=ent_a, ent_bb=ent_bb):
                ent_from_state(c0, 0, ent_a)
                ent_from_state(c1, 1, ent_bb)
                finals(s, 0)
                finals(s, 1)
            jobs.append((pre, c0, None, ent_bb))
            jobs.append((None, c1, ent_a, None))
        e0, e1, e2, e3 = ents

        def pre4():
            V("act", "activation", [H_b], [e0[1]], out=e0[0], in_=H[:, 0, :], func=AF.Copy)
            ent_from_state(5, 1, e1, hdir=1, etot_c=5)

        def pre5():
            ent_from_state(4, 0, e2, hdir=0, etot_c=4)
            V("act", "activation", [H_b], [e3[1]], out=e3[0], in_=H[:, 1, :], func=AF.Copy)
        jobs.append((pre4, 4, e0, e1))
        jobs.append((pre5, 5, e2, e3))
        prev_ctx = None
        for (pre, c, ef, eb_) in jobs:
            if pre is not None:
                pre()
            ctx = y_front(c, ef, eb_)
            if prev_ctx is not None:
                y_tail(prev_ctx)
            prev_ctx = ctx
        y_tail(prev_ctx)

    if dbg == "ssd":
        dump(yT[:, 0, :], yT_b, 768)
        dump(yT[:, 5, :], yT_b, 768)
        dump(yT[:, 15, :], yT_b, 768)
        P.ops["pool"][-1][1].update([b_.t.lw for b_ in yTb if b_.t.lw is not None])
        P.ops["pool"][-2][1].update([b_.t.lw for b_ in yTb if b_.t.lw is not None])
        P.ops["pool"][-3][1].update([b_.t.lw for b_ in yTb if b_.t.lw is not None])
        return end_dbg()

    SLT.reset(); SL2.reset(); SL3.reset()
    yrd = [yT_b] + yTb

    def hbufs(bi):
        return hTb[0:4] if bi == 0 else hTb[4:6]

    pf_i = [0]

    def proj_fm(units, jt, rhs_tile, rhs_bufs_fn, evac):
        par_ = pf_i[0] % 2
        pf_i[0] += 1
        nk = 8 * len(units)
        for bi, (t0, n, cnd) in enumerate(BLK):
            pt, pb = PS(par_ * 2 + bi)
            kk = 0
            for (ut, ub) in units:
                for k in range(8):
                    V("pe", "matmul", [ub] + rhs_bufs_fn(bi), [pb], pt[:, 0:n],
                      lhsT=ut[:, k, jt * 128:(jt + 1) * 128], rhs=rhs_tile[:, kk, t0:t0 + n],
                      start=(kk == 0), stop=(kk == nk - 1))
                    kk += 1
            evac(bi, t0, n, cnd, pt[:, 0:n], pb)

    gs, gs_b = SLT.carve([128, 8, T], BF16)
    gc, gc_b = SLT.carve([128, 8, T], BF16)
    bg_o = PAR["b_gate"][0]
    for (c0, gt, gb, joff) in ((8256, gs, gs_b, 0), (9280, gc, gc_b, 8)):
        gu_ = load_unit(w_in_d, c0, 1024)
        for j in range(8):
            def evac(bi, t0, n, cnd, pap, pb, j=j, gt=gt, gb=gb, joff=joff):
                V("act", "activation", [pb, par_b], [gb], out=gt[:, j, t0:t0 + n], in_=pap, func=AF.Sigmoid,
                  bias=par[:, bg_o + joff + j:bg_o + joff + j + 1], scale=1.0)
            proj_fm([gu_], j, hT, hbufs, evac)

    szs = [SL3.carve([128, T]) for _ in range(2)]
    sqs = [SL3.carve([128, T], BF16) for _ in range(2)]
    rgs = [SL3.carve([128, T]) for _ in range(2)]
    zus = [None, None]
    for j in range(16):
        if j % 8 == 0:
            zus[j // 8] = load_unit(w_in_d, (j // 8) * 1024, 1024)
        zu = zus[j // 8]
        sz, szb = szs[j % 2]
        sq, sqb = sqs[j % 2]

        def evac(bi, t0, n, cnd, pap, pb, sz=sz, szb=szb):
            V("act", "activation", [pb], [szb], out=sz[:, t0:t0 + n], in_=pap, func=AF.Silu)
        proj_fm([zu], j % 8, hT, hbufs, evac)
        V("dve", "tensor_tensor", yrd + [szb], [yT_b], out=yT[:, j, :], in0=yT[:, j, :], in1=sz, op=ALU.mult)
        V("act", "activation", [yT_b], [sqb], out=sq, in_=yT[:, j, :], func=AF.Square)
        for bi, (t0, n, cnd) in enumerate(BLK):
            V("pe", "matmul", [sqb, cstb_b], [PS(4 + bi)[1]], PS(4 + bi)[0][:, 0:n], lhsT=ONESB,
              rhs=sq[:, t0:t0 + n], start=(j % 2 == 0), stop=(j % 2 == 1))
        if j % 2 == 1:
            rg, rgb = rgs[(j // 2) % 2]
            for bi, (t0, n, cnd) in enumerate(BLK):
                rsqrt_from(rg[:, t0:t0 + n], rgb, PS(4 + bi)[0][:, 0:n], PS(4 + bi)[1], 1.0 / 256)
            for jj in (j - 1, j):
                V("dve", "scalar_tensor_tensor", [yT_b, rgb, par_b], [yT_b], out=yT[:, jj, :], in0=yT[:, jj, :],
                  scalar=pc("ssm_norm_w")[:, jj:jj + 1], in1=rg, op0=ALU.mult, op1=ALU.mult)

    if dbg == "yn":
        dump(yT[:, 0, :], yT_b, 768)
        dump(yT[:, 9, :], yT_b, 768)
        dump(gs[:, 3, :], gs_b, 768)
        dump(gc[:, 7, :], gc_b, 768)
        return end_dbg()

    mT, mT_b = SL2.carve([128, 8, T])
    so1 = load_unit(w_ssd_d, 0, 1024, k0=0)
    so2 = load_unit(w_ssd_d, 0, 1024, k0=8)
    for dd in range(8):
        def evac(bi, t0, n, cnd, pap, pb, dd=dd):
            V("dve", "tensor_tensor", [pb, gs_b], [mT_b], out=mT[:, dd, t0:t0 + n], in0=pap,
              in1=gs[:, dd, t0:t0 + n], op=ALU.mult)
        proj_fm([so1, so2], dd, yT, lambda bi: [yT_b], evac)

    SL1.reset()
    SL3.reset()
    uc, uc_b = SL3.carve([128, 8, T])
    upres = [SL1.carve([128, 948], BF16) for _ in range(2)]
    sg, sg_b = SL1.carve([128, T])
    dg31s = [SL1.carve([128, 31, 128], BF16) for _ in range(2)]
    for (up, upb) in upres:
        V("dve", "memset", [], [upb], up, 0.0)
    au = load_unit(w_in_d, 6208, 1024)
    bu_ = load_unit(w_in_d, 7232, 1024)
    cw_o = PAR["cdw_w"][0]
    cdb_o = PAR["cdw_b"][0]
    for j in range(8):
        up, upb = upres[j % 2]
        upP = up[:, 0:572].rearrange("p (s t) -> p s t", s=2)
        upS = up[:, 572:948].rearrange("p (s t) -> p s t", s=4)
        for bi, (t0, n, cnd) in enumerate(BLK):
            pa, pab = PS(bi)
            pbt, pbb = PS(2 + bi)
            for k in range(8):
                V("pe", "matmul", [au[1]] + hbufs(bi), [pab], pa[:, 0:n], lhsT=au[0][:, k, j * 128:(j + 1) * 128],
                  rhs=hT[:, k, t0:t0 + n], start=(k == 0), stop=(k == 7))
            for k in range(8):
                V("pe", "matmul", [bu_[1]] + hbufs(bi), [pbb], pbt[:, 0:n],
                  lhsT=bu_[0][:, k, j * 128:(j + 1) * 128], rhs=hT[:, k, t0:t0 + n], start=(k == 0), stop=(k == 7))
            V("act", "activation", [pbb], [sg_b], out=sg[:, t0:t0 + n], in_=pbt[:, 0:n], func=AF.Sigmoid)
            if bi == 0:
                V("dve", "tensor_tensor", [pab, sg_b], [upb], out=upP[:, :, 15:271],
                  in0=pa[:, 0:512].rearrange("p (s t) -> p s t", s=2),
                  in1=sg[:, 0:512].rearrange("p (s t) -> p s t", s=2), op=ALU.mult)
            else:
                V("dve", "tensor_tensor", [pab, sg_b], [upb], out=upS[:, :, 15:79],
                  in0=pa[:, 0:256].rearrange("p (s t) -> p s t", s=4),
                  in1=sg[:, 512:768].rearrange("p (s t) -> p s t", s=4), op=ALU.mult)
        dg, dgb = dg31s[j % 2]
        dgks = [Buf("dg31_%d_%d" % (j, k)) for k in range(31)]
        for k in range(31):
            dgks[k].t.rd = dict(dgb.t.rd)
            if dgb.t.lw is not None:
                dgks[k].t.rd[("lw", id(dgb))] = dgb.t.lw
            SL1.tiles.append(dgks[k])
            wc_ = par[:, cw_o + j * 31 + k:cw_o + j * 31 + k + 1]
            if k % 2 == 0:
                V("dve", "tensor_scalar", [cstb_b, par_b], [dgks[k]], out=dg[:, k, :], in0=IDENTB,
                  scalar1=wc_, scalar2=None, op0=ALU.mult)
            else:
                V("act", "activation", [cstb_b, par_b], [dgks[k]], out=dg[:, k, :], in_=IDENTB, func=AF.Copy,
                  scale=wc_)
        for bi, (t0, n, cnd) in enumerate(BLK):
            pt, pb = PS(4 + 2 * (j % 2) + bi)
            for k in range(31):
                rhs = upP[:, :, k:k + 256] if bi == 0 else upS[:, :, k:k + 64]
                V("pe", "matmul", [upb, dgks[k]], [pb], pt[:, 0:n], lhsT=dg[:, k, :], rhs=rhs,
                  start=(k == 0), stop=(k == 30))
                dgb.t.rd["pe"] = dgks[k].t.rd.get("pe", dgb.t.rd.get("pe"))
            V("act", "activation", [pb, par_b], [uc_b], out=uc[:, j, t0:t0 + n], in_=pt[:, 0:n], func=AF.Identity,
              bias=par[:, cdb_o + j:cdb_o + j + 1], scale=1.0)

    SL1.reset()
    un, un_b = SL1.carve([128, 8, T], BF16)
    lnr, lnr_b = SL1.carve([128, T])
    t1s = [SL1.carve([128, T]) for _ in range(2)]
    ucb_, ucbb = SLT.carve([128, T], BF16)
    sq2, sq2b = SLT.carve([128, T], BF16)
    mu, mu_b = SLT.carve([128, T])
    for j in range(8):
        V("act", "activation", [uc_b], [ucbb], out=ucb_, in_=uc[:, j, :], func=AF.Copy)
        V("act", "activation", [uc_b], [sq2b], out=sq2, in_=uc[:, j, :], func=AF.Square)
        for bi, (t0, n, cnd) in enumerate(BLK):
            V("pe", "matmul", [ucbb, cstb_b], [PS(4 + bi)[1]], PS(4 + bi)[0][:, 0:n], lhsT=ONESB,
              rhs=ucb_[:, t0:t0 + n], start=(j == 0), stop=(j == 7))
            V("pe", "matmul", [sq2b, cstb_b], [PS(6 + bi)[1]], PS(6 + bi)[0][:, 0:n], lhsT=ONESB,
              rhs=sq2[:, t0:t0 + n], start=(j == 0), stop=(j == 7))
    for bi, (t0, n, cnd) in enumerate(BLK):
        V("act", "activation", [PS(4 + bi)[1]], [mu_b], out=mu[:, t0:t0 + n], in_=PS(4 + bi)[0][:, 0:n],
          func=AF.Copy, scale=1.0 / 1024)
    V("dve", "tensor_tensor", [mu_b], [lnr_b], out=lnr, in0=mu, in1=mu, op=ALU.mult)
    for bi, (t0, n, cnd) in enumerate(BLK):
        V("dve", "scalar_tensor_tensor", [PS(6 + bi)[1], lnr_b], [lnr_b], out=lnr[:, t0:t0 + n],
          in0=PS(6 + bi)[0][:, 0:n], scalar=1.0 / 1024, in1=lnr[:, t0:t0 + n], op0=ALU.mult, op1=ALU.subtract)
    rsqrt_from(lnr, lnr_b, lnr, lnr_b, 1.0)
    for j in range(8):
        t1, t1b = t1s[j % 2]
        V("dve", "tensor_tensor", [uc_b, mu_b], [t1b], out=t1, in0=uc[:, j, :], in1=mu, op=ALU.subtract)
        V("dve", "tensor_tensor", [t1b, lnr_b], [t1b], out=t1, in0=t1, in1=lnr, op=ALU.mult)
        V("act", "activation", [t1b, par_b], [un_b], out=un[:, j, :], in_=t1, func=AF.Silu,
          scale=pc("ln_w")[:, j:j + 1], bias=pc("ln_b")[:, j:j + 1])
    cuo = load_unit(w_conf_d, 0, 1024)
    for dd in range(8):
        t1, t1b = t1s[dd % 2]

        def evac(bi, t0, n, cnd, pap, pb, dd=dd, t1=t1, t1b=t1b):
            V("dve", "scalar_tensor_tensor", [pb, gc_b, par_b], [t1b], out=t1[:, t0:t0 + n], in0=pap,
              scalar=pc("b_conf_out")[:, dd:dd + 1], in1=gc[:, dd, t0:t0 + n], op0=ALU.add, op1=ALU.mult)
            V("dve", "tensor_tensor", [t1b, mT_b], [mT_b], out=mT[:, dd, t0:t0 + n], in0=mT[:, dd, t0:t0 + n],
              in1=t1[:, t0:t0 + n], op=ALU.add)
        proj_fm([cuo], dd, un, lambda bi: [un_b], evac)

    SLT.reset()
    mb, mb_b = SLT.carve([128, 8, T], BF16)
    xts = [SLT.carve([128, 1024], F32, dma=True) for _ in range(2)]
    V("act", "activation", [mT_b], [mb_b], out=mb, in_=mT, func=AF.Copy)
    SL3.reset()
    x1T, x1T_b = SL3.carve([128, 8, T])
    for i in range(6):
        xtt, xtb = xts[i % 2]
        dma_in(xtt, xtb, x_all[i * 128:(i + 1) * 128, :])

        def evac(j, pap, pb, i=i):
            eng = "dve" if j < 4 else "act"
            if eng == "dve":
                V("dve", "tensor_copy", [pb], [x1T_b], out=x1T[:, j, i * 128:(i + 1) * 128], in_=pap)
            else:
                V("act", "activation", [pb], [x1T_b], out=x1T[:, j, i * 128:(i + 1) * 128], in_=pap, func=AF.Copy)
        transpose_x_tile(xtt, xtb, evac)
    wou = load_unit(w_o_d, 0, 1024)
    for dd in range(8):
        def evac(bi, t0, n, cnd, pap, pb, dd=dd):
            V("dve", "scalar_tensor_tensor", [pb, x1T_b, mod_b], [x1T_b], out=x1T[:, dd, t0:t0 + n], in0=pap,
              scalar=G1(dd, cnd), in1=x1T[:, dd, t0:t0 + n], op0=ALU.mult, op1=ALU.add)
        proj_fm([wou], dd, mb, lambda bi: [mb_b], evac)

    if dbg == "mix":
        for j in (0, 3, 7):
            dump(x1T[:, j, :], x1T_b, 768)
        return end_dbg()

    SL0.reset(); SL1.reset(); SL2.reset(); SLT.reset()
    acc, acc_b = SL0.carve([128, 8, T])
    acts = [SL2.carve([128, 8, T], BF16) for _ in range(2)]
    h2T, h2T_b = SLT.carve([128, 8, T], BF16)
    combT, combT_b = SLT.carve([128, T], BF16)
    rb, rb_b = SLT.carve([128, T])
    sqm, sqm_b = SLT.carve([128, T], BF16)
    lg, lg_b = SLT.carve([128, 6, 32])
    ex, ex_b = SLT.carve([128, 6, 32])
    mk, mk_b = SLT.carve([128, 6, 32])
    comb, comb_b = SLT.carve([128, 6, 32])
    cma_hi, cma_hi_b = SLT.carve([128, 6, 32], BF16)
    cma_lo, cma_lo_b = SLT.carve([128, 6, 32], BF16)
    m8, m8_b = SLT.carve([128, 6, 8])
    sm, sm_b = SLT.carve([128, 6])
    bdn, bdn_b = SLT.carve([128, 1024], BF16, dma=True)
    wrt, wrt_b = SLT.carve([128, 8, 32], F32, dma=True)
    wrt_hi, wrt_hi_b = SLT.carve([128, 8, 32], BF16)
    wrt_lo, wrt_lo_b = SLT.carve([128, 8, 32], BF16)
    h2lo, h2lo_b = SL1.carve([128, 8, T], BF16)
    h2fs = [SL1.carve([128, T]) for _ in range(2)]

    def rms_bc(src, src_b, scale):
        for j in range(8):
            V("act", "activation", [src_b], [sqm_b], out=sqm, in_=src[:, j, :], func=AF.Square)
            for bi, (t0, n, cnd) in enumerate(BLK):
                V("pe", "matmul", [sqm_b, cstb_b], [PS(4 + bi)[1]], PS(4 + bi)[0][:, 0:n], lhsT=ONESB,
                  rhs=sqm[:, t0:t0 + n], start=(j == 0), stop=(j == 7))
        for bi, (t0, n, cnd) in enumerate(BLK):
            rsqrt_from(rb[:, t0:t0 + n], rb_b, PS(4 + bi)[0][:, 0:n], PS(4 + bi)[1], scale)

    rms_bc(x1T, x1T_b, 1.0 / 1024)
    P.op("pool", lambda E: E.dma_start(out=bdn[0:32, :], in_=b_dn_d), wr=[bdn_b], dma=bdn_b)
    dma_in(wrt, wrt_b, w_rt_d.rearrange("p (k e) -> p k e", k=8))
    V("dve", "tensor_copy", [wrt_b], [wrt_hi_b], out=wrt_hi, in_=wrt)
    V("dve", "tensor_tensor", [wrt_b, wrt_hi_b], [wrt_lo_b], out=wrt_lo, in0=wrt, in1=wrt_hi, op=ALU.subtract)
    for j in range(8):
        h2f, h2fb = h2fs[j % 2]
        for bi, (t0, n, cnd) in enumerate(BLK):
            V("dve", "scalar_tensor_tensor", [x1T_b, rb_b, modA_b], [h2fb], out=h2f[:, t0:t0 + n],
              in0=x1T[:, j, t0:t0 + n], scalar=A2(j, cnd), in1=rb[:, t0:t0 + n], op0=ALU.mult, op1=ALU.mult)
            V("act", "activation", [h2fb, mod_b], [h2fb], out=h2f[:, t0:t0 + n], in_=h2f[:, t0:t0 + n],
              func=AF.Identity, bias=S2(j, cnd), scale=1.0)
        V("dve", "tensor_copy", [h2fb], [h2T_b], out=h2T[:, j, :], in_=h2f)
        V("dve", "tensor_tensor", [h2fb, h2T_b], [h2lo_b], out=h2lo[:, j, :], in0=h2f, in1=h2T[:, j, :],
          op=ALU.subtract)
    pt0, pb0 = PS(0)
    for i in range(6):
        n_mm = 0
        for k in range(8):
            for (lt_, ltb, rt_, rtb) in ((h2T, h2T_b, wrt_hi, wrt_hi_b), (h2T, h2T_b, wrt_lo, wrt_lo_b),
                                         (h2lo, h2lo_b, wrt_hi, wrt_hi_b)):
                V("pe", "matmul", [ltb, rtb], [pb0], pt0[:, i * 32:(i + 1) * 32],
                  lhsT=lt_[:, k, i * 128:(i + 1) * 128], rhs=rt_[:, k, :], start=(n_mm == 0), stop=(n_mm == 23))
                n_mm += 1
    V("dve", "tensor_tensor", [pb0, par_b], [lg_b], out=lg, in0=pt0[:, 0:192].rearrange("p (i e) -> p i e", i=6),
      in1=pc("b_router").unsqueeze(1).to_broadcast([128, 6, 32]), op=ALU.add)
    for i in range(6):
        V("dve", "max", [lg_b], [m8_b], out=m8[:, i, :], in_=lg[:, i, :])
    V("dve", "tensor_tensor", [lg_b, m8_b], [mk_b], out=mk, in0=lg, in1=m8[:, :, 3:4].to_broadcast([128, 6, 32]),
      op=ALU.is_ge)
    V("dve", "tensor_tensor", [lg_b, m8_b], [ex_b], out=ex, in0=lg, in1=m8[:, :, 0:1].to_broadcast([128, 6, 32]),
      op=ALU.subtract)
    V("act", "activation", [ex_b], [ex_b], out=ex, in_=ex, func=AF.Exp)
    V("dve", "tensor_tensor", [ex_b, mk_b], [ex_b], out=ex, in0=ex, in1=mk, op=ALU.mult)
    V("dve", "tensor_reduce", [ex_b], [sm_b], out=sm, in_=ex, axis=AX.X, op=ALU.add)
    V("dve", "reciprocal", [sm_b], [sm_b], out=sm, in_=sm)
    V("dve", "tensor_tensor", [ex_b, sm_b], [comb_b], out=comb, in0=ex,
      in1=sm.unsqueeze(2).to_broadcast([128, 6, 32]), op=ALU.mult)
    V("dve", "tensor_scalar", [comb_b], [ex_b], out=ex, in0=comb, scalar1=1.0 / SW_ALPHA, scalar2=None, op0=ALU.mult)
    V("dve", "tensor_copy", [ex_b], [cma_hi_b], out=cma_hi, in_=ex)
    V("dve", "tensor_tensor", [ex_b, cma_hi_b], [cma_lo_b], out=cma_lo, in0=ex, in1=cma_hi, op=ALU.subtract)
    for i in range(6):
        ptc, pbc = PS(1 + i // 4)
        V("pe", "transpose", [comb_b, cst_b], [pbc], out=ptc[0:32, (i % 4) * 128:(i % 4 + 1) * 128],
          in_=comb[:, i, :], identity=IDENT)
    V("act", "activation", [PS(1)[1]], [combT_b], out=combT[0:32, 0:512], in_=PS(1)[0][0:32, 0:512], func=AF.Copy)
    V("act", "activation", [PS(2)[1]], [combT_b], out=combT[0:32, 512:768], in_=PS(2)[0][0:32, 0:256], func=AF.Copy)
    for dd in range(8):
        for bi, (t0, n, cnd) in enumerate(BLK):
            pt, pb = PS(4 + (dd * 2 + bi) % 2)
            V("pe", "matmul", [bdn_b, combT_b], [pb], pt[:, 0:n], lhsT=bdn[0:32, dd * 128:(dd + 1) * 128],
              rhs=combT[0:32, t0:t0 + n], start=True, stop=True)
            V("act", "activation", [pb], [acc_b], out=acc[:, dd, t0:t0 + n], in_=pt[:, 0:n], func=AF.Copy)

    if dbg == "router":
        dump(comb.rearrange("p a b -> p (a b)"), comb_b, 192)
        dump(acc[:, 2, :], acc_b, 768)
        dump(h2T[:, 5, :], h2T_b, 768)
        return end_dbg()

    SL1.reset()
    gsbs = [SL1.carve([128, T]) for _ in range(2)]
    tts = [SL1.carve([128, T]) for _ in range(2)]
    ubs = [SL1.carve([128, T]) for _ in range(2)]
    cmbs = [SL1.carve([128, T]) for _ in range(2)]
    bgu_o = PAR["b_gu"][0]

    def make_cmb(e):
        cm_t, cm_b = cmbs[e % 2]
        for i in range(6):
            pt, pb = PS(6 + i // 4)
            dst = pt[:, (i % 4) * 128:(i % 4 + 1) * 128]
            V("pe", "matmul", [cma_hi_b, cstb_b], [pb], dst, lhsT=cma_hi[:, i, e:e + 1].to_broadcast([128, 128]),
              rhs=IDENTB, start=True, stop=False)
            V("pe", "matmul", [cma_lo_b, cstb_b], [pb], dst, lhsT=cma_lo[:, i, e:e + 1].to_broadcast([128, 128]),
              rhs=IDENTB, start=False, stop=True)
        V("act", "activation", [PS(6)[1]], [cm_b], out=cm_t[:, 0:512], in_=PS(6)[0][:, 0:512], func=AF.Copy)
        V("act", "activation", [PS(7)[1]], [cm_b], out=cm_t[:, 512:768], in_=PS(7)[0][:, 0:256], func=AF.Copy)
        return cm_t, cm_b

    jcount = [0]

    def expert_gu(e, jh, U, act, cm, down_iter=None):
        ut, ub_ = U
        act_t, act_b = act
        cm_t, cm_b = cm
        for jj in range(4):
            j = jh * 4 + jj
            s_ = jcount[0] % 2
            jcount[0] += 1
            gA, gAb = PS(3 * s_)
            uA, uAb = PS(3 * s_ + 1)
            gB, gBb = PS(3 * s_ + 2)
            if down_iter is not None and jj > 0:
                for _ in range(2):
                    next(down_iter, None)
            for k in range(8):
                w_ = ut[:, k, jj * 128:(jj + 1) * 128]
                V("pe", "matmul", [ub_, h2T_b], [gAb], gA[:, 0:512], lhsT=w_, rhs=h2T[:, k, 0:512],
                  start=(k == 0), stop=(k == 7))
                V("pe", "matmul", [ub_, h2T_b], [gBb], gB[:, 0:256], lhsT=w_, rhs=h2T[:, k, 512:768],
                  start=(k == 0), stop=(k == 7))
            for k in range(8):
                w_ = ut[:, k, 512 + jj * 128:512 + (jj + 1) * 128]
                V("pe", "matmul", [ub_, h2T_b], [uAb], uA[:, 0:512], lhsT=w_, rhs=h2T[:, k, 0:512],
                  start=(k == 0), stop=(k == 7))
                V("pe", "matmul", [ub_, h2T_b], [gBb], gB[:, 256:512], lhsT=w_, rhs=h2T[:, k, 512:768],
                  start=(k == 0), stop=(k == 7))
            bg = par[:, bgu_o + e * 16 + j:bgu_o + e * 16 + j + 1]
            bu = par[:, bgu_o + e * 16 + 8 + j:bgu_o + e * 16 + 8 + j + 1]
            gsb, gsbb = gsbs[s_]
            tt, ttb = tts[s_]
            ubt, ubb = ubs[s_]
            V("dve", "tensor_scalar", [gAb, par_b], [gsbb], out=gsb[:, 0:512], in0=gA[:, 0:512], scalar1=bg,
              scalar2=SW_LIM, op0=ALU.add, op1=ALU.min)
            V("dve", "tensor_scalar", [gBb, par_b], [gsbb], out=gsb[:, 512:768], in0=gB[:, 0:256], scalar1=bg,
              scalar2=SW_LIM, op0=ALU.add, op1=ALU.min)
            V("act", "activation", [uAb, par_b], [ubb], out=ubt[:, 0:512], in_=uA[:, 0:512], func=AF.Identity,
              bias=bu, scale=1.0)
            V("dve", "tensor_scalar", [gBb, par_b], [ubb], out=ubt[:, 512:768], in0=gB[:, 256:512], scalar1=bu,
              scalar2=None, op0=ALU.add)
            V("act", "activation", [gsbb], [ttb], out=tt, in_=gsb, func=AF.Silu, scale=SW_ALPHA)
            if down_iter is not None:
                for _ in range(2):
                    next(down_iter, None)
            V("dve", "tensor_scalar", [ubb], [ubb], out=ubt, in0=ubt, scalar1=SW_LIM,
              scalar2=-SW_LIM, op0=ALU.min, op1=ALU.max)
            V("dve", "scalar_tensor_tensor", [ubb, ttb], [ttb], out=tt, in0=ubt, scalar=1.0, in1=tt,
              op0=ALU.add, op1=ALU.mult)
            V("dve", "tensor_tensor", [ttb, cm_b], [act_b], out=act_t[:, j, :], in0=tt, in1=cm_t, op=ALU.mult)
            if down_iter is not None and jj == 0:
                for _ in range(2):
                    next(down_iter, None)

    dcount = [0]

    accb = [[Buf("acc_%d_%d" % (dd, bi)) for bi in range(2)] for dd in range(8)]
    for dd in range(8):
        for bi in range(2):
            accb[dd][bi].t.lw = acc_b.t.lw
            accb[dd][bi].t.rd = dict(acc_b.t.rd)
            SL0.tiles.append(accb[dd][bi])

    def expert_down_gen(UD, act):
        ut, ub_ = UD
        act_t, act_b = act
        for bi, (t0, n, cnd) in enumerate(BLK):
            for dd in range(8):
                pt, pb = PS(6 + dcount[0] % 2)
                dcount[0] += 1
                for k in range(8):
                    V("pe", "matmul", [ub_, act_b], [pb], pt[:, 0:n], lhsT=ut[:, k, dd * 128:(dd + 1) * 128],
                      rhs=act_t[:, k, t0:t0 + n], start=(k == 0), stop=(k == 7))
                V("dve", "tensor_tensor", [pb, accb[dd][bi]], [accb[dd][bi]], out=acc[:, dd, t0:t0 + n],
                  in0=pt[:, 0:n], in1=acc[:, dd, t0:t0 + n], op=ALU.add)
                yield

    def expert_down(UD, act):
        for _ in expert_down_gen(UD, act):
            pass

    n_exp = N_EXP
    if dbg and dbg.startswith("moe"):
        n_exp = int(dbg[3:])
    UA = load_unit2(w_gu_d[0], 0, 1024)
    UB = load_unit2(w_gu_d[0], 512, 1536)
    UD = load_unit(w_dn_d[0], 0, 1024)
    prev = None
    for e in range(n_exp):
        cm = make_cmb(e)
        act = acts[e % 2]
        dit = expert_down_gen(*prev) if prev is not None else None
        expert_gu(e, 0, UA, act, cm, dit)
        if dit is not None:
            for _ in dit:
                pass
        if e + 1 < n_exp:
            UA_n = load_unit2(w_gu_d[e + 1], 0, 1024)
            UB_n = load_unit2(w_gu_d[e + 1], 512, 1536)
        expert_gu(e, 1, UB, act, cm)
        prev = (UD, act)
        if e + 1 < n_exp:
            UD = load_unit(w_dn_d[e + 1], 0, 1024)
            UA, UB = UA_n, UB_n
    expert_down(*prev)

    for j in range(8):
        for bi, (t0, n, cnd) in enumerate(BLK):
            V("dve", "scalar_tensor_tensor", [accb[j][bi], x1T_b, mod_b], [x1T_b], out=x1T[:, j, t0:t0 + n],
              in0=acc[:, j, t0:t0 + n], scalar=G2(j, cnd), in1=x1T[:, j, t0:t0 + n], op0=ALU.mult, op1=ALU.add)
    rms_bc(x1T, x1T_b, 1.0 / 1024)
    for j in range(8):
        V("dve", "scalar_tensor_tensor", [x1T_b, rb_b, par_b], [x1T_b], out=x1T[:, j, :], in0=x1T[:, j, :],
          scalar=pc("norm_final")[:, j:j + 1], in1=rb, op0=ALU.mult, op1=ALU.mult)
    SL2.reset()
    ystg = [SL2.carve([128, 1024], F32, dma=True) for _ in range(2)]
    for i in range(6):
        ys, ysb = ystg[i % 2]
        for hb in range(2):
            pt, pb = PS(1 + hb)
            for jj in range(4):
                j = hb * 4 + jj
                V("pe", "transpose", [x1T_b, cst_b], [pb], out=pt[:, jj * 128:(jj + 1) * 128],
                  in_=x1T[:, j, i * 128:(i + 1) * 128], identity=IDENT)
            if hb == 0:
                V("act", "activation", [pb], [ysb], out=ys[:, 0:512], in_=pt[:, :], func=AF.Copy)
            else:
                V("dve", "tensor_copy", [pb], [ysb], out=ys[:, 512:1024], in_=pt[:, :])
        dst = y_out[i * 128:(i + 1) * 128, :]
        P.op("sp", (lambda E, dst=dst, ys=ys: E.dma_start(out=dst, in_=ys)), rd=[ysb], dma=ysb)
        if ysb not in out_bufs:
            out_bufs.append(ysb)
    P.op("sp", None, wr=out_bufs)
    return finish(nc, P, es, sems)


def finish(nc, P, es, sems):
    P.finalize()
    with nc.Block() as block:
        @block.tensor
        def _(E):
            P.emit("pe", E, sems)

        @block.scalar
        def _(E):
            P.emit("act", E, sems)

        @block.vector
        def _(E):
            P.emit("dve", E, sems)

        @block.gpsimd
        def _(E):
            P.emit("pool", E, sems)

        @block.sync
        def _(E):
            P.emit("sp", E, sems)
    es.close()
    return nc


def core_inputs(inp, core, shared):
    b, q = core // 4, core % 4
    xp = np.asarray(inp["x_prompt"], np.float32)
    xs = np.asarray(inp["x_sample"], np.float32)
    xrot = np.roll(xs[b], -256 * q, axis=0)
    x_all = np.ascontiguousarray(np.concatenate([xp[2 * core], xp[2 * core + 1], xrot], axis=0))
    cond = np.stack([_fm(inp["c_ctx"], 8), _fm(np.asarray(inp["c"])[b], 8)], axis=2).reshape(128, 16)
    st0 = np.ascontiguousarray(np.asarray(inp["state_ssm"], np.float32)[b, 0].reshape(4096, 128))
    m = np.zeros((20,), np.float32)
    m[0:8] = 1.0
    m[(8 - 2 * q) % 8] = 0.0
    for s in range(2, 8):
        orig = (s + 2 * q) % 8
        m[8 + (s - 2)] = 1.0 if orig < 2 * q else 0.0
        m[14 + (s - 2)] = 0.0 if orig < 2 * q else 1.0
    d = dict(shared)
    d.update({"x_all": x_all, "cond_fm": np.ascontiguousarray(cond),
              "state0": st0, "masks": np.ascontiguousarray(np.broadcast_to(m[None, :], (128, 20)))})
    return d


def shared_inputs(inp):
    f = lambda a: np.ascontiguousarray(np.asarray(a, np.float32))
    wr = f(inp["w_router"])[0]
    return {
        "params": pack_params(inp), "consts": make_consts(),
        "w_ada": f(inp["w_ada"])[0], "w_in": f(inp["w_in"])[0], "w_ssd_out": f(inp["w_ssd_out"])[0],
        "w_conf_out": f(inp["w_conf_out"])[0], "w_o": f(inp["w_o"])[0],
        "w_router": np.ascontiguousarray(wr.reshape(8, 128, 32).transpose(1, 0, 2).reshape(128, 256)),
        "w_gu": f(inp["w_gu"])[0], "w_down": f(inp["w_down"])[0], "b_down": f(inp["b_down"])[0],
    }


def kernel(**inp):
    nc = build()
    shared = shared_inputs(inp)
    in_maps = [core_inputs(inp, c, shared) for c in range(8)]
    res = run_bass_kernel_spmd(nc, in_maps, core_ids=list(range(8)))
    y_prompt = np.zeros((16, 256, 1024), np.float32)
    y_sample = np.zeros((2, 1024, 1024), np.float32)
    new_state = np.zeros((16, 1, 2, 32, 64, 128), np.float32)
    for c in range(8):
        r = res.results[c]
        b, q = c // 4, c % 4
        y = r["y_own"]
        y_prompt[2 * c] = y[0:256]
        y_prompt[2 * c + 1] = y[256:512]
        y_sample[b, 256 * q:256 * q + 256] = y[512:768]
        ns = r["new_state"].reshape(2, 2, 32, 64, 128)
        new_state[2 * c, 0] = ns[0]
        new_state[2 * c + 1, 0] = ns[1]
    return (y_prompt, y_sample, new_state)
```

```python
import numpy as np
from contextlib import ExitStack
import concourse.bass as bass
import concourse.mybir as mybir
from concourse.bass_utils import run_bass_kernel_spmd

F32, BF16 = mybir.dt.float32, mybir.dt.bfloat16
AF = mybir.ActivationFunctionType
ALU = mybir.AluOpType
AX = mybir.AxisListType

T = 768
A = 1536
EPS = 1e-6
NU = 5
SAME_SYNC = True
N_EXP = 32
DBG = None


def _par_layout():
    off = {}
    n = 0
    def add(name, w):
        nonlocal n
        off[name] = (n, w)
        n += w
    add("norm_mix", 8); add("norm_ffn", 8); add("norm_final", 8); add("b_ada", 48)
    add("conv_w", 32 * 5); add("conv_b", 32); add("ssm_norm_w", 16)
    add("cdw_w", 8 * 31); add("cdw_b", 8); add("ln_w", 8); add("ln_b", 8); add("b_conf_out", 8)
    add("b_gate", 16); add("b_gu", 32 * 16)
    add("dt_bias", 64); add("a_log", 64); add("d_skip", 32); add("b_router", 32)
    return off, n

PAR, NPAR = _par_layout()


def _fm(v, ntile):
    return np.ascontiguousarray(np.asarray(v, np.float32).reshape(ntile, 128).T)


def pack_params(inp):
    P = np.zeros((128, NPAR), np.float32)
    def put(name, arr):
        o, w = PAR[name]
        assert arr.shape == (128, w), (name, arr.shape, w)
        P[:, o:o + w] = arr
    put("norm_mix", _fm(inp["norm_mix"][0], 8)); put("norm_ffn", _fm(inp["norm_ffn"][0], 8))
    put("norm_final", _fm(inp["norm_final"], 8)); put("b_ada", _fm(inp["b_ada"][0], 48))
    cw = np.asarray(inp["ssm_conv_w"][0], np.float32)
    put("conv_w", np.ascontiguousarray(cw.reshape(5, 32, 128).transpose(2, 1, 0).reshape(128, 160)))
    put("conv_b", _fm(inp["ssm_conv_b"][0], 32)); put("ssm_norm_w", _fm(inp["ssm_norm_w"][0], 16))
    dw = np.asarray(inp["conf_dw_w"][0], np.float32)
    put("cdw_w", np.ascontiguousarray(dw.reshape(31, 8, 128).transpose(2, 1, 0).reshape(128, 248)))
    put("cdw_b", _fm(inp["conf_dw_b"][0], 8)); put("ln_w", _fm(inp["conf_ln_w"][0], 8))
    put("ln_b", _fm(inp["conf_ln_b"][0], 8)); put("b_conf_out", _fm(inp["b_conf_out"][0], 8))
    put("b_gate", _fm(inp["b_gate"][0], 16))
    bg = np.asarray(inp["b_gu"][0], np.float32)
    put("b_gu", np.ascontiguousarray(bg.reshape(32, 16, 128).transpose(2, 0, 1).reshape(128, 512)))
    put("dt_bias", np.broadcast_to(np.asarray(inp["dt_bias"][0], np.float32).reshape(1, 64), (128, 64)))
    put("a_log", np.broadcast_to(np.asarray(inp["a_log"][0], np.float32).reshape(1, 64), (128, 64)))
    put("d_skip", np.broadcast_to(np.asarray(inp["d_skip"][0], np.float32).reshape(1, 32), (128, 32)))
    put("b_router", np.broadcast_to(np.asarray(inp["b_router"][0], np.float32).reshape(1, 32), (128, 32)))
    return P


def make_consts():
    k = np.arange(128)[:, None]
    s = np.arange(128)[None, :]
    C = np.zeros((128, 8, 128), np.float32)
    C[:, 0] = (k == s); C[:, 1] = (k <= s); C[:, 2] = (k >= s); C[:, 3] = (k > s); C[:, 4] = (k < s)
    C[:, 5] = 1.0
    C[:, 6] = (s < 64); C[:, 7] = (s >= 64)
    return C.reshape(128, 1024)


class _Trk:
    __slots__ = ("lw", "rd")
    def __init__(self):
        self.lw = None
        self.rd = {}


class Buf:
    def __init__(self, name, share=None):
        self.name = name
        self.t = share.t if share is not None else _Trk()
        self.dsem = None
        self.dn = 0


ENGS = ["pe", "act", "dve", "pool", "sp"]


class Prog:
    def __init__(self):
        self.ops = {e: [] for e in ENGS}

    def op(self, eng, fn, rd=(), wr=(), dma=None):
        deps = set()
        for b in rd:
            if b.t.lw is not None:
                deps.add(b.t.lw)
        for b in wr:
            if b.t.lw is not None:
                deps.add(b.t.lw)
            deps.update(b.t.rd.values())
        idx = len(self.ops[eng])
        if dma is not None:
            dma.dn += 1
            tok = ("dma", dma, dma.dn)
            key = ("dma", id(dma))
        else:
            tok = (eng, idx)
            key = eng
        for b in rd:
            if getattr(b, "is_psum", False) and eng != "pe":
                others = [k for k in b.t.rd if isinstance(k, str) and k not in ("pe", eng)]
                if others:
                    raise AssertionError("PSUM bank %s read by %s and %s between writes" % (b.name, eng, others))
            b.t.rd[key] = tok
        for b in wr:
            b.t.lw = tok
            b.t.rd = {}
        self.ops[eng].append((fn, deps, tok, dma))

    def finalize(self):
        needed = set()
        for e in ENGS:
            for fn, deps, tok, dma in self.ops[e]:
                for d in deps:
                    if d[0] != "dma":
                        if d[0] == e and (e == "pe" or not SAME_SYNC):
                            continue
                        needed.add(d)
        self.needed = needed
        self.sigval = {}
        for e in ENGS:
            c = 0
            for i, (fn, deps, tok, dma) in enumerate(self.ops[e]):
                if tok in needed:
                    c += 1
                    self.sigval[tok] = c

    def emit(self, e, E, sems):
        known = {}
        for fn, deps, tok, dma in self.ops[e]:
            waits = {}
            for d in deps:
                if d[0] == "dma":
                    sem, val = d[1].dsem, 16 * d[2]
                else:
                    if d[0] == e and (e == "pe" or not SAME_SYNC):
                        continue
                    sem, val = sems[d[0]], self.sigval[d]
                k = id(sem)
                if known.get(k, 0) >= val:
                    continue
                if k not in waits or waits[k][1] < val:
                    waits[k] = (sem, val)
            for k, (sem, val) in waits.items():
                E.wait_ge(sem, val)
                known[k] = val
            if fn is None:
                continue
            ins = fn(E)
            if dma is not None:
                ins.then_inc(dma.dsem, 16)
            elif tok in self.needed:
                ins.then_inc(sems[e], 1)


SW_ALPHA = 1.702
SW_LIM = 7.0
BLK = ((0, 512, 0), (512, 256, 1))


def build(dbg=None):
    nc = bass.Bass("TRN2", target_bir_lowering=False)
    P = Prog()
    es = ExitStack()

    def dram(name, shape, kind="ExternalInput", dt=F32):
        return nc.dram_tensor(name, list(shape), dt, kind=kind).ap()

    x_all = dram("x_all", [A, 1024])
    cond_d = dram("cond_fm", [128, 16])
    st0_d = dram("state0", [4096, 128])
    masks_d = dram("masks", [128, 20])
    params_d = dram("params", [128, NPAR])
    consts_d = dram("consts", [128, 1024])
    w_ada_d = dram("w_ada", [1024, 6144])
    w_in_d = dram("w_in", [1024, 10304])
    w_ssd_d = dram("w_ssd_out", [2048, 1024])
    w_conf_d = dram("w_conf_out", [1024, 1024])
    w_o_d = dram("w_o", [1024, 1024])
    w_rt_d = dram("w_router", [128, 256])
    w_gu_d = dram("w_gu", [N_EXP, 1024, 2048])
    w_dn_d = dram("w_down", [N_EXP, 1024, 1024])
    b_dn_d = dram("b_down", [N_EXP, 1024])
    y_out = dram("y_own", [T, 1024], kind="ExternalOutput")
    ns_out = dram("new_state", [2 * 2 * 2048, 128], kind="ExternalOutput")
    dbg_out = dram("dbg", [128, 8192], kind="ExternalOutput") if dbg else None

    def newsem(name):
        return es.enter_context(nc.semaphore(name))

    def sb(name, shape, dt=F32, dma=False):
        t = es.enter_context(nc.sbuf_tensor(name, list(shape), dt))
        b = Buf(name)
        if dma:
            b.dsem = newsem("d_" + name)
        return t, b

    sems = {e: newsem("s_" + e) for e in ENGS}

    def V(eng, method, rd, wr, *args, **kw):
        P.op(eng, lambda E: getattr(E, method)(*args, **kw), rd=rd, wr=wr)

    class Slab:
        def __init__(self, name, words):
            self.name = name
            self.words = words
            self.t = es.enter_context(nc.sbuf_tensor(name, [128, words], F32))
            self.ptr = 0
            self.tiles = []
            self.haz = {}
            self.n = 0

        def reset(self):
            for b in self.tiles:
                if b.t.lw is not None:
                    self.haz[("lw", id(b))] = b.t.lw
                for k, v in b.t.rd.items():
                    if k in self.haz and isinstance(k, str):
                        if self.haz[k][1] < v[1]:
                            self.haz[k] = v
                    else:
                        self.haz[k] = v
            self.tiles = []
            self.ptr = 0

        def carve(self, shape, dt=F32, dma=False):
            per = 1
            for d in shape[1:]:
                per *= d
            words = per if dt == F32 else (per + 1) // 2
            assert self.ptr + words <= self.words, (self.name, self.ptr, words, self.words)
            ap = self.t[:, self.ptr:self.ptr + words]
            self.ptr += words
            if dt != F32:
                ap = ap.bitcast(dt)
            if len(shape) == 3:
                ap = ap.rearrange("p (a b) -> p a b", a=shape[1])
            elif len(shape) == 4:
                ap = ap.rearrange("p (a b c) -> p a b c", a=shape[1], b=shape[2])
            self.n += 1
            b = Buf("%s_%d" % (self.name, self.n))
            b.t.rd = dict(self.haz)
            if dma:
                b.dsem = newsem("d_%s_%d" % (self.name, self.n))
            self.tiles.append(b)
            return ap, b

    cst, cst_b = sb("cst", [128, 8, 128], F32, dma=True)
    cstb, cstb_b = sb("cstb", [128, 8, 128], BF16)
    par, par_b = sb("par", [128, NPAR], F32, dma=True)
    msk, msk_b = sb("msk", [128, 20], F32, dma=True)
    small, small_b = sb("small", [128, 8], F32)
    negm_t, negm_b = sb("negm", [128, 2, 128], BF16)
    negm = negm_t[:]
    NUr = 4
    ring = [sb("ring%d" % i, [128, 8, 1024], BF16, dma=True) for i in range(NUr)]
    ring_i = [0]
    SL0 = Slab("SL0", 6144)
    SL1 = Slab("SL1", 6144)
    SL2 = Slab("SL2", 6144)
    SL3 = Slab("SL3", 6144)
    SLT = Slab("SLT", 7680)

    IDENT, TRI_LE, TRI_GE, TRI_GT, TRI_LT, ONES, HLO, HHI = [cst[:, i, :] for i in range(8)]
    IDENTB = cstb[:, 0, :]
    ONESB = cstb[:, 5, :]
    EPSC = small[:, 0:1]
    ONEC = small[:, 1:2]

    def pc(name, a=0, b=None):
        o, w = PAR[name]
        if b is None:
            b = w
        return par[:, o + a:o + b]

    psum = []
    for i in range(8):
        t = es.enter_context(nc.psum_tensor("ps%d" % i, [128, 512], F32))
        psum.append((t, Buf("ps%d" % i)))
        psum[-1][1].is_psum = True

    def PS(i):
        return psum[i]

    def next_ring():
        i = ring_i[0] % NUr
        ring_i[0] += 1
        return ring[i]

    def load_unit(w2d, c0, ncols, k0=0, nk=8):
        t, b = next_ring()
        src = w2d[k0 * 128:(k0 + nk) * 128, c0:c0 + ncols].rearrange("(kc p) c -> p kc c", p=128)
        P.op("pool", lambda E: E.dma_start(out=t[:, 0:nk, 0:ncols], in_=src), wr=[b], dma=b)
        return t, b

    def load_unit2(w2d, c0, c1, n=512):
        t, b = next_ring()
        for idx, cc in enumerate((c0, c1)):
            src = w2d[:, cc:cc + n].rearrange("(kc p) c -> p kc c", p=128)
            dst = t[:, :, idx * n:(idx + 1) * n]
            if idx == 0:
                P.op("pool", (lambda E, dst=dst, src=src: E.dma_start(out=dst, in_=src)), wr=[b], dma=b)
            else:
                P.op("pool", (lambda E, dst=dst, src=src: E.dma_start(out=dst, in_=src)), dma=b)
                b.t.lw = P.ops["pool"][-1][2]
        return t, b

    def dma_in(tile_ap, b, src, eng="sp"):
        P.op(eng, lambda E: E.dma_start(out=tile_ap, in_=src), wr=[b], dma=b)

    dbg_b = Buf("dbgout")
    if dbg:
        dbg_b.dsem = newsem("d_dbg")
    dbg_col = [0]

    def dump(ap2d, b, n):
        c0 = dbg_col[0]
        dbg_col[0] += n
        dst = dbg_out[:, c0:c0 + n]
        P.op("pool", lambda E: E.dma_start(out=dst, in_=ap2d), rd=[b], dma=dbg_b)
        return c0

    def end_dbg():
        P.op("sp", None, wr=[dbg_b])
        return finish(nc, P, es, sems)

    def rsqrt_from(out_ap, out_b, in_ap, in_b, scale, eng_rd=()):
        V("act", "activation", [in_b, small_b] + list(eng_rd), [out_b], out=out_ap, in_=in_ap, func=AF.Ln,
          scale=scale, bias=EPSC)
        V("act", "activation", [out_b], [out_b], out=out_ap, in_=out_ap, func=AF.Exp, scale=-0.5)

    dma_in(cst[:], cst_b, consts_d.rearrange("p (a b) -> p a b", a=8))
    dma_in(par[:], par_b, params_d)
    dma_in(msk[:], msk_b, masks_d)
    V("dve", "tensor_copy", [cst_b], [cstb_b], out=cstb[:], in_=cst[:])
    V("dve", "memset", [], [small_b], small[:, 0:1], EPS)
    V("dve", "memset", [], [small_b], small[:, 1:2], 1.0)

    cond, cond_b = sb("cond", [128, 8, 2], F32, dma=True)
    scond, scond_b = sb("scond", [128, 8, 2], BF16)
    mod, mod_b = sb("mod", [128, 48, 2], F32)
    modA, modA_b = sb("modA", [128, 2, 8, 2], F32)
    dma_in(cond[:], cond_b, cond_d.rearrange("p (j c) -> p j c", c=2))
    V("act", "activation", [cond_b], [scond_b], out=scond[:], in_=cond[:], func=AF.Silu)
    mod1_b = Buf("mod1")
    modA1_b = Buf("modA1")

    def ada_units(u0, u1, bank, mb_, which_list, mab_):
        pt, pb = PS(bank)
        for u in range(u0, u1):
            ut, ub = load_unit(w_ada_d, u * 1024, 1024)
            for jt in range(8):
                col = (u * 8 + jt) * 2
                for k in range(8):
                    V("pe", "matmul", [ub, scond_b], [pb], pt[:, col:col + 2],
                      lhsT=ut[:, k, jt * 128:(jt + 1) * 128], rhs=scond[:, k, :], start=(k == 0), stop=(k == 7))
        j0, j1 = u0 * 8, u1 * 8
        V("dve", "tensor_tensor", [pb, par_b], [mb_], out=mod[:, j0:j1, :],
          in0=pt[:, 2 * j0:2 * j1].rearrange("p (j c) -> p j c", c=2),
          in1=pc("b_ada", j0, j1).unsqueeze(2).to_broadcast([128, j1 - j0, 2]), op=ALU.add)
        for which, nm, jj0 in which_list:
            V("dve", "tensor_scalar", [mb_], [mab_], out=modA[:, which], in0=mod[:, jj0:jj0 + 8, :],
              scalar1=1.0, scalar2=None, op0=ALU.add)
            V("dve", "tensor_tensor", [mab_, par_b], [mab_], out=modA[:, which], in0=modA[:, which],
              in1=pc(nm).unsqueeze(2).to_broadcast([128, 8, 2]), op=ALU.mult)

    ada_units(0, 2, 0, mod1_b, [(0, "norm_mix", 8)], modA1_b)

    def A1(j, c): return modA[:, 0, j, c:c + 1]
    def S1(j, c): return mod[:, j, c:c + 1]
    def G1(j, c): return mod[:, 16 + j, c:c + 1]
    def A2(j, c): return modA[:, 1, j, c:c + 1]
    def S2(j, c): return mod[:, 24 + j, c:c + 1]
    def G2(j, c): return mod[:, 40 + j, c:c + 1]

    hT, hT_all = SL0.carve([128, 8, A], BF16)
    hTb = [Buf("hT_c%d" % i) for i in range(12)]
    for b_ in hTb:
        SL0.tiles.append(b_)
    xt = [SL2.carve([128, 1024], F32, dma=True) for _ in range(2)]
    xn = [SL2.carve([128, 1024], F32) for _ in range(2)]
    junk, junk_b = SL2.carve([128, 1024], BF16)
    ssq, ssq_b = sb("ssq", [128, 12], F32)
    rstd, rstd_b = sb("rstd", [128, 12], F32)
    V("dve", "memset", [], [ssq_b], ssq[:], 0.0)

    def transpose_x_tile(xnt, xnb, evac):
        for hb in range(2):
            pt, pb = PS(1 + hb)
            for jj in range(4):
                j = hb * 4 + jj
                V("pe", "transpose", [xnb, cst_b], [pb], out=pt[:, jj * 128:(jj + 1) * 128],
                  in_=xnt[:, j * 128:(j + 1) * 128], identity=IDENT)
            for jj in range(4):
                evac(hb * 4 + jj, pt[:, jj * 128:(jj + 1) * 128], pb)

    for i in range(12):
        xtt, xtb = xt[i % 2]
        xnt, xnb = xn[i % 2]
        cnd = 0 if i < 4 else 1
        dma_in(xtt, xtb, x_all[i * 128:(i + 1) * 128, :])
        V("act", "activation", [xtb], [junk_b, ssq_b], out=junk, in_=xtt, func=AF.Square,
          accum_out=ssq[:, i:i + 1])
        rsqrt_from(rstd[:, i:i + 1], rstd_b, ssq[:, i:i + 1], ssq_b, 1.0 / 1024)
        V("act", "activation", [xtb, rstd_b], [xnb], out=xnt, in_=xtt, func=AF.Copy, scale=rstd[:, i:i + 1])

        def evac(j, pap, pb, i=i, cnd=cnd):
            dst = hT[:, j, i * 128:(i + 1) * 128]
            if j < 4:
                V("dve", "tensor_scalar", [pb, modA1_b, mod1_b], [hTb[i]], out=dst, in0=pap,
                  scalar1=A1(j, cnd), scalar2=S1(j, cnd), op0=ALU.mult, op1=ALU.add)
            else:
                V("act", "activation", [pb, modA1_b, mod1_b], [hTb[i]], out=dst, in_=pap, func=AF.Identity,
                  scale=A1(j, cnd), bias=S1(j, cnd))
        transpose_x_tile(xnt, xnb, evac)

    ada_units(2, 6, 7, mod_b, [(1, "norm_ffn", 32)], modA_b)

    if dbg == "hT":
        dump(hT[:, 0, :], hT_all, 1536) if False else None
        for b_ in hTb:
            pass
        P.op("pool", lambda E: E.dma_start(out=dbg_out[:, 0:1536], in_=hT[:, 0, :]), rd=hTb, dma=dbg_b)
        P.op("pool", lambda E: E.dma_start(out=dbg_out[:, 1536:3072], in_=hT[:, 7, :]), rd=hTb, dma=dbg_b)
        P.op("pool", lambda E: E.dma_start(out=dbg_out[:, 3072:3168], in_=mod[:].rearrange("p j c -> p (j c)")),
             rd=[mod_b], dma=dbg_b)
        return end_dbg()

    SL2.reset()
    dtv, dtv_b = SL3.carve([128, 12, 64])
    raw, raw_b = SL3.carve([128, 12, 192])
    Eown, Eown_b = SL3.carve([128, 6, 192])
    wdec, wdec_b = SL3.carve([128, 6, 64])
    wrest, wrest_b = SL3.carve([128, 6, 64])
    lw, lw_b = SL3.carve([128, 6, 64])
    wfin, wfin_b = SL3.carve([128, 4, 32])
    ptot, ptot_b = SL3.carve([128, 64])
    aneg, aneg_b = SL3.carve([128, 64])
    negcs_t, negcs_b = sb("negcs", [128, 6, 64], F32)
    negcs = negcs_t[:]
    cs_hi, cs_hi_b = SL3.carve([128, 6, 64], BF16)
    cs_lo, cs_lo_b = SL3.carve([128, 6, 64], BF16)
    adt, adt_b = SLT.carve([128, 12, 64])
    t64, t64_b = SLT.carve([128, 12, 64])
    lpu, lpu_b = SLT.carve([128, 12, 64])
    lpl, lpl_b = SLT.carve([128, 12, 64])

    dtu, dtu_b = load_unit(w_in_d, 6144, 64)
    for i in range(12):
        pt, pb = PS(3 + (i // 8))
        col = (i % 8) * 64
        for k in range(8):
            V("pe", "matmul", [hTb[i], dtu_b], [pb], pt[:, col:col + 64], lhsT=hT[:, k, i * 128:(i + 1) * 128],
              rhs=dtu[:, k, 0:64], start=(k == 0), stop=(k == 7))
    V("dve", "tensor_tensor", [PS(3)[1], par_b], [dtv_b], out=dtv[:, 0:8, :],
      in0=PS(3)[0][:, 0:512].rearrange("p (i c) -> p i c", c=64),
      in1=pc("dt_bias").unsqueeze(1).to_broadcast([128, 8, 64]), op=ALU.add)
    V("dve", "tensor_tensor", [PS(4)[1], par_b], [dtv_b], out=dtv[:, 8:12, :],
      in0=PS(4)[0][:, 0:256].rearrange("p (i c) -> p i c", c=64),
      in1=pc("dt_bias").unsqueeze(1).to_broadcast([128, 4, 64]), op=ALU.add)
    V("act", "activation", [dtv_b], [t64_b], out=t64, in_=dtv, func=AF.Abs)
    V("act", "activation", [t64_b], [t64_b], out=t64, in_=t64, func=AF.Exp, scale=-1.0)
    V("dve", "tensor_scalar", [t64_b], [lpu_b], out=lpu, in0=t64, scalar1=1.0, scalar2=None, op0=ALU.add)
    V("act", "activation", [lpu_b], [lpl_b], out=lpl, in_=lpu, func=AF.Ln)
    V("dve", "tensor_scalar", [lpu_b], [lpu_b], out=lpu, in0=lpu, scalar1=-1.0, scalar2=1e-30, op0=ALU.add,
      op1=ALU.add)
    V("dve", "reciprocal", [lpu_b], [lpu_b], out=lpu, in_=lpu)
    V("dve", "scalar_tensor_tensor", [lpl_b, lpu_b], [lpl_b], out=lpl, in0=lpl, scalar=1e-30, in1=lpu,
      op0=ALU.add, op1=ALU.mult)
    V("dve", "tensor_tensor", [t64_b, lpl_b], [t64_b], out=t64, in0=t64, in1=lpl, op=ALU.mult)
    V("dve", "scalar_tensor_tensor", [dtv_b, t64_b], [dtv_b], out=dtv, in0=dtv, scalar=0.0, in1=t64,
      op0=ALU.max, op1=ALU.add)
    if dbg == "dt1":
        dump(dtv.rearrange("p a b -> p (a b)"), dtv_b, 768)
        return end_dbg()
    V("dve", "tensor_tensor", [dtv_b, msk_b], [dtv_b], out=dtv[:, 6:12, 0:32], in0=dtv[:, 6:12, 0:32],
      in1=msk[:, 8:14].unsqueeze(2).to_broadcast([128, 6, 32]), op=ALU.mult)
    V("dve", "tensor_tensor", [dtv_b, msk_b], [dtv_b], out=dtv[:, 6:12, 32:64], in0=dtv[:, 6:12, 32:64],
      in1=msk[:, 14:20].unsqueeze(2).to_broadcast([128, 6, 32]), op=ALU.mult)
    V("act", "activation", [par_b], [aneg_b], out=aneg, in_=pc("a_log"), func=AF.Exp)
    V("dve", "scalar_tensor_tensor", [dtv_b, aneg_b], [adt_b], out=adt, in0=dtv, scalar=-1.0,
      in1=aneg.unsqueeze(1).to_broadcast([128, 12, 64]), op0=ALU.mult, op1=ALU.mult)
    adt_hi, adt_hi_b = SLT.carve([128, 12, 64], BF16)
    adt_lo, adt_lo_b = SLT.carve([128, 12, 64], BF16)
    V("dve", "tensor_copy", [adt_b], [adt_hi_b], out=adt_hi, in_=adt)
    V("dve", "tensor_tensor", [adt_b, adt_hi_b], [adt_lo_b], out=adt_lo, in0=adt, in1=adt_hi, op=ALU.subtract)
    if dbg == "dt2":
        dump(adt.rearrange("p a b -> p (a b)"), adt_b, 768)
        dump(adt_lo.rearrange("p a b -> p (a b)"), adt_lo_b, 768)
        return end_dbg()
    for i in range(12):
        pt, pb = PS(5 + i % 2)
        for (c0, c1, tri, a0, a1) in ((0, 32, 1, 0, 32), (32, 64, 2, 32, 64), (64, 96, 3, 0, 32),
                                      (96, 128, 4, 32, 64), (128, 192, 5, 0, 64)):
            V("pe", "matmul", [adt_hi_b, cstb_b], [pb], pt[:, c0:c1], lhsT=cstb[:, tri, :],
              rhs=adt_hi[:, i, a0:a1], start=True, stop=False)
            V("pe", "matmul", [adt_lo_b, cstb_b], [pb], pt[:, c0:c1], lhsT=cstb[:, tri, :],
              rhs=adt_lo[:, i, a0:a1], start=False, stop=True)
        V("dve", "tensor_copy", [pb], [raw_b], out=raw[:, i, :], in_=pt[:, 0:192])
        if i < 6:
            V("act", "activation", [raw_b], [Eown_b], out=Eown[:, i, :], in_=raw[:, i, :], func=AF.Exp)
    if dbg == "dt3":
        dump(raw.rearrange("p a b -> p (a b)"), raw_b, 2304)
        dump(Eown.rearrange("p a b -> p (a b)"), Eown_b, 1152)
        return end_dbg()
    V("dve", "tensor_tensor", [dtv_b, Eown_b], [wdec_b], out=wdec, in0=dtv[:, 0:6, :], in1=Eown[:, :, 64:128],
      op=ALU.mult)
    V("dve", "tensor_scalar", [raw_b], [negcs_b], out=negcs, in0=raw[:, 0:6, 0:64], scalar1=-1.0, scalar2=None,
      op0=ALU.mult)
    V("dve", "tensor_scalar", [cst_b], [negm_b], out=negm, in0=cst[:, 3:5, :], scalar1=-65536.0, scalar2=None,
      op0=ALU.mult)
    V("dve", "tensor_copy", [raw_b], [cs_hi_b], out=cs_hi, in_=raw[:, 0:6, 0:64])
    V("dve", "tensor_tensor", [raw_b, cs_hi_b], [cs_lo_b], out=cs_lo, in0=raw[:, 0:6, 0:64], in1=cs_hi,
      op=ALU.subtract)
    V("dve", "memset", [], [lw_b], lw, 0.0)
    for i in range(10, 5, -1):
        V("dve", "tensor_tensor", [lw_b, raw_b], [lw_b], out=lw[:, i - 6, 0:32], in0=lw[:, i - 5, 0:32],
          in1=raw[:, i + 1, 128:160], op=ALU.add)
    for i in range(7, 12):
        V("dve", "tensor_tensor", [lw_b, raw_b], [lw_b], out=lw[:, i - 6, 32:64], in0=lw[:, i - 7, 32:64],
          in1=raw[:, i - 1, 160:192], op=ALU.add)
    V("dve", "tensor_tensor", [lw_b, raw_b], [ptot_b], out=ptot[:, 0:32], in0=lw[:, 0, 0:32],
      in1=raw[:, 6, 128:160], op=ALU.add)
    V("dve", "tensor_tensor", [lw_b, raw_b], [ptot_b], out=ptot[:, 32:64], in0=lw[:, 5, 32:64],
      in1=raw[:, 11, 160:192], op=ALU.add)
    V("act", "activation", [ptot_b], [ptot_b], out=ptot, in_=ptot, func=AF.Exp)
    V("dve", "tensor_tensor", [lw_b, raw_b], [wrest_b], out=wrest, in0=lw, in1=raw[:, 6:12, 64:128], op=ALU.add)
    V("act", "activation", [wrest_b], [wrest_b], out=wrest, in_=wrest, func=AF.Exp)
    V("dve", "tensor_tensor", [wrest_b, dtv_b], [wrest_b], out=wrest, in0=wrest, in1=dtv[:, 6:12, :], op=ALU.mult)
    for s in range(2):
        c0, c1 = 2 * s, 2 * s + 1
        V("dve", "tensor_tensor", [wdec_b, Eown_b], [wfin_b], out=wfin[:, 2 * s, :], in0=wdec[:, c0, 0:32],
          in1=Eown[:, c1, 128:160], op=ALU.mult)
        V("dve", "tensor_tensor", [wdec_b, Eown_b], [wfin_b], out=wfin[:, 2 * s + 1, :], in0=wdec[:, c1, 32:64],
          in1=Eown[:, c0, 160:192], op=ALU.mult)

    if dbg == "dt":
        dump(dtv.rearrange("p a b -> p (a b)"), dtv_b, 768)
        dump(raw.rearrange("p a b -> p (a b)"), raw_b, 2304)
        dump(wdec.rearrange("p a b -> p (a b)"), wdec_b, 384)
        dump(wrest.rearrange("p a b -> p (a b)"), wrest_b, 384)
        dump(ptot, ptot_b, 64)
        dump(wfin.rearrange("p a b -> p (a b)"), wfin_b, 128)
        return end_dbg()

    yT, yT_b = SL1.carve([128, 16, T], BF16)
    yTb = [Buf("yT_p%d" % p) for p in range(4)]
    for b_ in yTb:
        SL1.tiles.append(b_)
    st0v = st0_d.rearrange("(d g t p) n -> g p d t n", d=2, g=8, t=2, p=128)
    stg_i = [0]
    out_bufs = []

    for p in range(4):
        SL2.reset()
        SLT.reset()
        x_tok, x_tok_b = SL2.carve([128, 6, 512], BF16)
        B_tok, B_tok_b = SL2.carve([128, 6, 256], BF16)
        BT, BT_b = SL2.carve([128, 2, T], BF16)
        CT, CT_b = SL2.carve([128, 2, T], BF16)
        H, H_b = SL2.carve([128, 2, 512], F32)
        ents = [SL2.carve([128, 512], BF16) for _ in range(4)]
        pres = [SLT.carve([128, 1576], BF16) for _ in range(2)]
        fms = [SLT.carve([128, A], BF16) for _ in range(2)]
        pc_i = [0]
        diags = [SLT.carve([128, 5, 128], BF16) for _ in range(2)]
        dg_i = [0]
        x_rest, x_rest_b = SLT.carve([128, 6, 256], BF16)
        B_rest, B_rest_b = SLT.carve([128, 6, 256], BF16)
        xsr = [SLT.carve([128, 256], BF16) for _ in range(4)]
        h0t, h0t_b = SLT.carve([128, 2, 2, 128], F32, dma=True)
        htmp, htmp_b = SLT.carve([128, 512], F32)
        for (pre_, pre_b_) in pres:
            preP_ = pre_[:, 0:520].rearrange("p (s t) -> p s t", s=2)
            V("dve", "memset", [], [pre_b_], preP_[:, :, 0:2], 0.0)
            V("dve", "memset", [], [pre_b_], preP_[:, :, 258:260], 0.0)

        xu = load_unit(w_in_d, 2048 + 512 * p, 512)
        bu = load_unit(w_in_d, 4096 + 256 * p, 256)
        cu = load_unit(w_in_d, 5120 + 256 * p, 256)

        def proj_conv(unit, jt, cidx, sil):
            ut, ub = unit
            pre, pre_b = pres[pc_i[0] % 2]
            fm, fm_b = fms[pc_i[0] % 2]
            pc_i[0] += 1
            preP = pre[:, 0:520].rearrange("p (s t) -> p s t", s=2)
            preS = pre[:, 520:1576].rearrange("p (s t) -> p s t", s=8)
            for tb in range(3):
                pt, pb = PS((0, 1, 5)[tb])
                for k in range(8):
                    V("pe", "matmul", [ub] + hTb[4 * tb:4 * tb + 4], [pb], pt[:, :],
                      lhsT=ut[:, k, jt * 128:(jt + 1) * 128], rhs=hT[:, k, tb * 512:(tb + 1) * 512],
                      start=(k == 0), stop=(k == 7))
                if tb == 0:
                    dst = preP[:, :, 2:258]
                    src = pt[:, :].rearrange("p (s t) -> p s t", s=2)
                else:
                    dst = preS[:, 4 * (tb - 1):4 * tb, 2:130]
                    src = pt[:, :].rearrange("p (s t) -> p s t", s=4)
                V("act", "activation", [pb], [pre_b], out=dst, in_=src, func=AF.Copy)
            V("dve", "tensor_tensor", [pre_b, msk_b], [pre_b], out=preS[:, 1:8, 0:2], in0=preS[:, 0:7, 128:130],
              in1=msk[:, 1:8].unsqueeze(2).to_broadcast([128, 7, 2]), op=ALU.mult)
            V("dve", "tensor_scalar", [pre_b, msk_b], [pre_b], out=preS[:, 0, 0:2], in0=preS[:, 7, 128:130],
              scalar1=msk[:, 0:1], scalar2=None, op0=ALU.mult)
            V("dve", "tensor_tensor", [pre_b, msk_b], [pre_b], out=preS[:, 0:7, 130:132], in0=preS[:, 1:8, 2:4],
              in1=msk[:, 1:8].unsqueeze(2).to_broadcast([128, 7, 2]), op=ALU.mult)
            V("dve", "tensor_scalar", [pre_b, msk_b], [pre_b], out=preS[:, 7, 130:132], in0=preS[:, 0, 2:4],
              scalar1=msk[:, 0:1], scalar2=None, op0=ALU.mult)
            o, _w = PAR["conv_w"]
            dg, dgb = diags[dg_i[0] % 2]
            dg_i[0] += 1
            for k in range(5):
                V("dve", "tensor_scalar", [cstb_b, par_b], [dgb], out=dg[:, k, :], in0=IDENTB,
                  scalar1=par[:, o + cidx * 5 + k:o + cidx * 5 + k + 1], scalar2=None, op0=ALU.mult)
            for tb in range(3):
                pt, pb = PS((6, 7, 4)[tb])
                for k in range(5):
                    if tb == 0:
                        rhs = preP[:, :, k:k + 256]
                    else:
                        rhs = preS[:, 4 * (tb - 1):4 * tb, k:k + 128]
                    V("pe", "matmul", [pre_b, dgb], [pb], pt[:, :], lhsT=dg[:, k, :], rhs=rhs,
                      start=(k == 0), stop=(k == 4))
                sil(tb, pt, pb, fm, fm_b)
            return fm, fm_b

        def transposes_to(fm, fm_b, dst_own, dst_own_b, dst_rest, dst_rest_b):
            for half, (dst, dstb) in enumerate(((dst_own, dst_own_b), (dst_rest, dst_rest_b))):
                pt, pb = PS(2 + half)
                ptb = pt[:, :].bitcast(BF16)
                for cc in range(6):
                    c = half * 6 + cc
                    V("pe", "transpose", [fm_b, cstb_b], [pb], out=ptb[:, cc * 128:(cc + 1) * 128],
                      in_=fm[:, c * 128:(c + 1) * 128], identity=IDENTB)
                V("act", "activation", [pb], [dstb], out=dst,
                  in_=ptb[:, 0:768].rearrange("p (c n) -> p c n", c=6), func=AF.Copy)

        cb_o = PAR["conv_b"][0]
        for gl in range(2):
            cidx = 16 + 2 * p + gl
            def sil(tb, pt, pb, fm, fm_b, cidx=cidx):
                V("act", "activation", [pb, par_b], [fm_b], out=fm[:, tb * 512:(tb + 1) * 512], in_=pt[:, :],
                  func=AF.Silu, bias=par[:, cb_o + cidx:cb_o + cidx + 1], scale=1.0)
            fm, fm_b = proj_conv(bu, gl, cidx, sil)
            V("pool", "tensor_copy", [fm_b], [BT_b], out=BT[:, gl, :], in_=fm[:, 0:T])
            transposes_to(fm, fm_b, B_tok[:, :, gl * 128:(gl + 1) * 128], B_tok_b, B_rest[:, :, gl * 128:(gl + 1) * 128],
                          B_rest_b)
        for gl in range(2):
            cidx = 24 + 2 * p + gl
            def sil(tb, pt, pb, fm, fm_b, cidx=cidx, gl=gl):
                if tb == 0:
                    V("act", "activation", [pb, par_b], [CT_b], out=CT[:, gl, 0:512], in_=pt[:, :], func=AF.Silu,
                      bias=par[:, cb_o + cidx:cb_o + cidx + 1], scale=1.0)
                elif tb == 1:
                    V("act", "activation", [pb, par_b], [CT_b], out=CT[:, gl, 512:768], in_=pt[:, 0:256],
                      func=AF.Silu, bias=par[:, cb_o + cidx:cb_o + cidx + 1], scale=1.0)
            proj_conv(cu, gl, cidx, sil)
        for xl in range(4):
            cidx = 4 * p + xl
            gl = xl // 2
            g = 2 * p + gl
            def sil(tb, pt, pb, fm, fm_b, cidx=cidx):
                V("act", "activation", [pb, par_b], [fm_b], out=fm[:, tb * 512:(tb + 1) * 512], in_=pt[:, :],
                  func=AF.Silu, bias=par[:, cb_o + cidx:cb_o + cidx + 1], scale=1.0)
            fm, fm_b = proj_conv(xu, xl, cidx, sil)
            transposes_to(fm, fm_b, x_tok[:, :, xl * 128:(xl + 1) * 128], x_tok_b,
                          x_rest[:, :, (xl % 2) * 128:(xl % 2 + 1) * 128], x_rest_b)
            if xl % 2 == 1:
                pt4, pb4 = PS(4)
                ri = 0
                for d in range(2):
                    for i in range(6, 12):
                        xs_t, xs_b = xsr[ri % 4]
                        ri += 1
                        V("dve", "tensor_tensor", [x_rest_b, wrest_b], [xs_b],
                          out=xs_t.rearrange("p (h q) -> p h q", h=4),
                          in0=x_rest[:, i - 6, :].rearrange("p (h q) -> p h q", h=4),
                          in1=wrest[:, i - 6, d * 32 + 4 * g:d * 32 + 4 * g + 4].unsqueeze(2).to_broadcast(
                              [128, 4, 64]), op=ALU.mult)
                        V("pe", "matmul", [B_rest_b, xs_b], [pb4], pt4[:, d * 256:(d + 1) * 256],
                          lhsT=B_rest[:, i - 6, gl * 128:(gl + 1) * 128], rhs=xs_t, start=(i == 6), stop=(i == 11))
                for d in range(2):
                    dma_in(h0t[:, d], h0t_b, st0v[g][:, d])
                pt5, pb5 = PS(5)
                for d in range(2):
                    for t_ in range(2):
                        V("pe", "transpose", [h0t_b, cst_b], [pb5],
                          out=pt5[:, (d * 2 + t_) * 128:(d * 2 + t_ + 1) * 128], in_=h0t[:, d, t_, :],
                          identity=IDENT)
                V("dve", "tensor_tensor", [pb5, ptot_b], [htmp_b],
                  out=htmp.rearrange("p (d h q) -> p d h q", d=2, h=4),
                  in0=pt5[:, :].rearrange("p (d h q) -> p d h q", d=2, h=4),
                  in1=ptot.rearrange("p (d h) -> p d h", d=2)[:, :, 4 * g:4 * g + 4].unsqueeze(3).to_broadcast(
                      [128, 2, 4, 64]), op=ALU.mult)
                V("dve", "tensor_tensor", [pb4, htmp_b], [H_b], out=H[:, :, gl * 256:(gl + 1) * 256],
                  in0=pt4[:, :].rearrange("p (d c) -> p d c", d=2), in1=htmp.rearrange("p (d c) -> p d c", d=2),
                  op=ALU.add)

        if dbg == "chain" and p == 0:
            dump(x_tok.rearrange("p a b -> p (a b)"), x_tok_b, 3072)
            dump(B_tok.rearrange("p a b -> p (a b)"), B_tok_b, 1536)
            dump(H.rearrange("p a b -> p (a b)"), H_b, 1024)
            dump(CT[:, 0, :], CT_b, 768)
            dump(BT[:, 1, :], BT_b, 768)
            return end_dbg()

        SLT.reset()
        xs_pool = [SLT.carve([128, 512], BF16) for _ in range(4)]
        xs_i = [0]
        scss = [SLT.carve([128, 2, 128], F32) for _ in range(2)]
        lt4s = [SLT.carve([128, 4, 128], BF16) for _ in range(4)]
        lt4bufs = [[Buf("lt4_%d_%d" % (a_, b_)) for b_ in range(4)] for a_ in range(4)]
        for a_ in range(4):
            for b_ in lt4bufs[a_]:
                b_.t.rd = dict(lt4s[a_][1].t.rd)
                SLT.tiles.append(b_)
        mt4s = [SLT.carve([128, 4, 128], BF16) for _ in range(4)]
        yaccs = [SLT.carve([128, 512], F32) for _ in range(2)]
        ytmp, ytmp_b = SLT.carve([128, 512], F32)
        ybfs = [SLT.carve([128, 512], BF16) for _ in range(2)]
        yc_i = [0]
        stg = [SLT.carve([128, 4, 128], F32, dma=True) for _ in range(2)]

        def hb8(ap2d):
            return ap2d.rearrange("p (h q) -> p h q", h=8)

        def bc8(ap8):
            return ap8.unsqueeze(2).to_broadcast([128, 8, 64])

        def make_xs(c, wap, wbufs):
            t_, b_ = xs_pool[xs_i[0] % 4]
            xs_i[0] += 1
            V("dve", "tensor_tensor", [x_tok_b] + wbufs, [b_], out=hb8(t_), in0=hb8(x_tok[:, c, :]), in1=bc8(wap),
              op=ALU.mult)
            return t_, b_

        def state_mm(c, xs):
            pt, pb = PS(6)
            for gl in range(2):
                V("pe", "matmul", [B_tok_b, xs[1]], [pb], pt[:, gl * 256:(gl + 1) * 256],
                  lhsT=B_tok[:, c, gl * 128:(gl + 1) * 128], rhs=xs[0][:, gl * 256:(gl + 1) * 256],
                  start=True, stop=True)
            return pt, pb

        def wd(c, d):
            return wdec[:, c, d * 32 + 8 * p:d * 32 + 8 * p + 8]

        def ent_from_state(c, d, ent, hdir=None, etot_c=None):
            xs = make_xs(c, wd(c, d), [wdec_b])
            pt, pb = state_mm(c, xs)
            if hdir is None:
                V("act", "activation", [pb], [ent[1]], out=ent[0], in_=pt[:, :], func=AF.Copy)
            else:
                V("dve", "tensor_tensor", [H_b, Eown_b], [ytmp_b], out=hb8(ytmp), in0=hb8(H[:, hdir, :]),
                  in1=bc8(Eown[:, etot_c, 128 + hdir * 32 + 8 * p:128 + hdir * 32 + 8 * p + 8]), op=ALU.mult)
                V("dve", "tensor_tensor", [pb, ytmp_b], [ent[1]], out=ent[0], in0=pt[:, :], in1=ytmp, op=ALU.add)

        def finals(s, d):
            c0, c1 = 2 * s, 2 * s + 1
            if d == 0:
                xa = make_xs(c0, wfin[:, 2 * s, 8 * p:8 * p + 8], [wfin_b]); ca = c0
                xb = make_xs(c1, wd(c1, 0), [wdec_b]); cb = c1
            else:
                xa = make_xs(c1, wfin[:, 2 * s + 1, 8 * p:8 * p + 8], [wfin_b]); ca = c1
                xb = make_xs(c0, wd(c0, 1), [wdec_b]); cb = c0
            pt, pb = PS(5)
            for i in range(4):
                gl = i // 2
                V("pe", "matmul", [xa[1], B_tok_b], [pb], pt[:, i * 128:(i + 1) * 128],
                  lhsT=xa[0][:, i * 128:(i + 1) * 128], rhs=B_tok[:, ca, gl * 128:(gl + 1) * 128],
                  start=True, stop=False)
                V("pe", "matmul", [xb[1], B_tok_b], [pb], pt[:, i * 128:(i + 1) * 128],
                  lhsT=xb[0][:, i * 128:(i + 1) * 128], rhs=B_tok[:, cb, gl * 128:(gl + 1) * 128],
                  start=False, stop=True)
            st_, sb_ = stg[stg_i[0] % 2]
            stg_i[0] += 1
            V("act", "activation", [pb], [sb_], out=st_, in_=pt[:, :].rearrange("p (i n) -> p i n", i=4),
              func=AF.Copy)
            r0 = (s * 2 + d) * 2048 + 512 * p
            dst = ns_out[r0:r0 + 512, :].rearrange("(i r) n -> r i n", r=128)
            P.op("sp", (lambda E, dst=dst, st_=st_: E.dma_start(out=dst, in_=st_)), rd=[sb_], dma=sb_)
            if sb_ not in out_bufs:
                out_bufs.append(sb_)

        yn_i = [0]

        def y_front(c, ent_f, ent_b):
            scs, scs_b = scss[yc_i[0] % 2]
            yacc, yacc_b = yaccs[yc_i[0] % 2]
            ybf, ybf_b = ybfs[yc_i[0] % 2]
            pt3, pb3 = PS((3, 7)[yc_i[0] % 2])
            yc_i[0] += 1
            pt0, pb0 = PS(0)
            for gl in range(2):
                V("pe", "matmul", [BT_b, CT_b], [pb0], pt0[:, gl * 128:(gl + 1) * 128],
                  lhsT=BT[:, gl, c * 128:(c + 1) * 128], rhs=CT[:, gl, c * 128:(c + 1) * 128], start=True, stop=True)
            V("act", "activation", [pb0], [scs_b], out=scs, in_=pt0[:, 0:256].rearrange("p (g n) -> p g n", g=2),
              func=AF.Copy)
            xsd = [make_xs(c, dtv[:, c, d * 32 + 8 * p:d * 32 + 8 * p + 8], [dtv_b]) for d in range(2)]
            for gl in range(2):
                mts = []
                for d in range(2):
                    n = yn_i[0]
                    yn_i[0] += 1
                    ptS, pbS = PS((1, 2)[n % 2])
                    lt4, _ = lt4s[n % 4]
                    ltb = lt4bufs[n % 4]
                    mt4, mt4b = mt4s[n % 4]
                    mts.append((mt4, mt4b))
                    for hh in range(4):
                        hl = gl * 4 + hh
                        ci = d * 32 + 8 * p + hl
                        dst = ptS[:, hh * 128:(hh + 1) * 128]
                        V("pe", "matmul", [cs_hi_b, cstb_b], [pbS], dst,
                          lhsT=cs_hi[:, c, ci:ci + 1].to_broadcast([128, 128]), rhs=IDENTB, start=True, stop=False)
                        V("pe", "matmul", [cs_lo_b, cstb_b], [pbS], dst,
                          lhsT=cs_lo[:, c, ci:ci + 1].to_broadcast([128, 128]), rhs=IDENTB, start=False, stop=False)
                        V("pe", "matmul", [negm_b, cstb_b], [pbS], dst, lhsT=IDENTB, rhs=negm[:, d, :],
                          start=False, stop=True)
                    for hh in range(4):
                        hl = gl * 4 + hh
                        ci = d * 32 + 8 * p + hl
                        V("act", "activation", [pbS, negcs_b], [ltb[hh]], out=lt4[:, hh, :],
                          in_=ptS[:, hh * 128:(hh + 1) * 128], func=AF.Exp, bias=negcs[:, c, ci:ci + 1], scale=1.0)
                    V("dve", "tensor_tensor", ltb + [scs_b], [mt4b], out=mt4, in0=lt4,
                      in1=scs[:, gl, :].unsqueeze(1).to_broadcast([128, 4, 128]), op=ALU.mult)
                for hh in range(4):
                    hl = gl * 4 + hh
                    for d in range(2):
                        V("pe", "matmul", [mts[d][1], xsd[d][1]], [pb3], pt3[:, hl * 64:(hl + 1) * 64],
                          lhsT=mts[d][0][:, hh, :], rhs=xsd[d][0][:, hl * 64:(hl + 1) * 64],
                          start=(d == 0), stop=(d == 1))
            return (c, ent_f, ent_b, yacc, yacc_b, ybf, ybf_b, pt3, pb3)

        def y_tail(ctx):
            (c, ent_f, ent_b, yacc, yacc_b, ybf, ybf_b, pt3, pb3) = ctx
            for d, ent in ((0, ent_f), (1, ent_b)):
                if ent is None:
                    continue
                ptO, pbO = PS(4 + d)
                for gl in range(2):
                    V("pe", "matmul", [CT_b, ent[1]], [pbO], ptO[:, gl * 256:(gl + 1) * 256],
                      lhsT=CT[:, gl, c * 128:(c + 1) * 128], rhs=ent[0][:, gl * 256:(gl + 1) * 256],
                      start=True, stop=True)
            V("dve", "tensor_tensor", [x_tok_b, par_b], [yacc_b], out=hb8(yacc), in0=hb8(x_tok[:, c, :]),
              in1=bc8(pc("d_skip")[:, 8 * p:8 * p + 8]), op=ALU.mult)
            V("dve", "tensor_tensor", [pb3, yacc_b], [yacc_b], out=yacc, in0=pt3[:, :], in1=yacc, op=ALU.add)
            for d, ent in ((0, ent_f), (1, ent_b)):
                if ent is None:
                    continue
                ptO, pbO = PS(4 + d)
                V("dve", "tensor_tensor", [pbO, Eown_b], [ytmp_b], out=hb8(ytmp), in0=hb8(ptO[:, :]),
                  in1=bc8(Eown[:, c, d * 32 + 8 * p:d * 32 + 8 * p + 8]), op=ALU.mult)
                V("dve", "tensor_tensor", [ytmp_b, yacc_b], [yacc_b], out=yacc, in0=yacc, in1=ytmp, op=ALU.add)
            V("act", "activation", [yacc_b], [ybf_b], out=ybf, in_=yacc, func=AF.Copy)
            pt6, pb6 = PS(6)
            pt6b = pt6[:, :].bitcast(BF16)
            for j in range(4):
                V("pe", "transpose", [ybf_b, cstb_b], [pb6], out=pt6b[:, j * 128:(j + 1) * 128],
                  in_=ybf[:, j * 128:(j + 1) * 128], identity=IDENTB)
            V("act", "activation", [pb6], [yTb[p]], out=yT[:, 4 * p:4 * p + 4, c * 128:(c + 1) * 128],
              in_=pt6b[:, 0:512].rearrange("p (j n) -> p j n", j=4), func=AF.Copy)

        jobs = []
        for s in range(2):
            c0, c1 = 2 * s, 2 * s + 1
            ent_a, ent_bb = ents[2 * s], ents[2 * s + 1]

            def pre(s=s, c0=c0, c1=c1, ent_a=ent_a, ent_bb=ent_bb):
                ent_from_state(c0, 0, ent_a)
                ent_from_state(c1, 1, ent_bb)
                finals(s, 0)
                finals(s, 1)
            jobs.append((pre, c0, None, ent_bb))
            jobs.append((None, c1, ent_a, None))
        e0, e1, e2, e3 = ents

        def pre4():
            V("act", "activation", [H_b], [e0[1]], out=e0[0], in_=H[:, 0, :], func=AF.Copy)
            ent_from_state(5, 1, e1, hdir=1, etot_c=5)

        def pre5():
            ent_from_state(4, 0, e2, hdir=0, etot_c=4)
            V("act", "activation", [H_b], [e3[1]], out=e3[0], in_=H[:, 1, :], func=AF.Copy)
        jobs.append((pre4, 4, e0, e1))
        jobs.append((pre5, 5, e2, e3))
        prev_ctx = None
        for (pre, c, ef, eb_) in jobs:
            if pre is not None:
                pre()
            ctx = y_front(c, ef, eb_)
            if prev_ctx is not None:
                y_tail(prev_ctx)
            prev_ctx = ctx
        y_tail(prev_ctx)

    if dbg == "ssd":
        dump(yT[:, 0, :], yT_b, 768)
        dump(yT[:, 5, :], yT_b, 768)
        dump(yT[:, 15, :], yT_b, 768)
        P.ops["pool"][-1][1].update([b_.t.lw for b_ in yTb if b_.t.lw is not None])
        P.ops["pool"][-2][1].update([b_.t.lw for b_ in yTb if b_.t.lw is not None])
        P.ops["pool"][-3][1].update([b_.t.lw for b_ in yTb if b_.t.lw is not None])
        return end_dbg()

    SLT.reset(); SL2.reset(); SL3.reset()
    yrd = [yT_b] + yTb

    def hbufs(bi):
        return hTb[0:4] if bi == 0 else hTb[4:6]

    pf_i = [0]

    def proj_fm(units, jt, rhs_tile, rhs_bufs_fn, evac):
        par_ = pf_i[0] % 2
        pf_i[0] += 1
        nk = 8 * len(units)
        for bi, (t0, n, cnd) in enumerate(BLK):
            pt, pb = PS(par_ * 2 + bi)
            kk = 0
            for (ut, ub) in units:
                for k in range(8):
                    V("pe", "matmul", [ub] + rhs_bufs_fn(bi), [pb], pt[:, 0:n],
                      lhsT=ut[:, k, jt * 128:(jt + 1) * 128], rhs=rhs_tile[:, kk, t0:t0 + n],
                      start=(kk == 0), stop=(kk == nk - 1))
                    kk += 1
            evac(bi, t0, n, cnd, pt[:, 0:n], pb)

    gs, gs_b = SLT.carve([128, 8, T], BF16)
    gc, gc_b = SLT.carve([128, 8, T], BF16)
    bg_o = PAR["b_gate"][0]
    for (c0, gt, gb, joff) in ((8256, gs, gs_b, 0), (9280, gc, gc_b, 8)):
        gu_ = load_unit(w_in_d, c0, 1024)
        for j in range(8):
            def evac(bi, t0, n, cnd, pap, pb, j=j, gt=gt, gb=gb, joff=joff):
                V("act", "activation", [pb, par_b], [gb], out=gt[:, j, t0:t0 + n], in_=pap, func=AF.Sigmoid,
                  bias=par[:, bg_o + joff + j:bg_o + joff + j + 1], scale=1.0)
            proj_fm([gu_], j, hT, hbufs, evac)

    szs = [SL3.carve([128, T]) for _ in range(2)]
    sqs = [SL3.carve([128, T], BF16) for _ in range(2)]
    rgs = [SL3.carve([128, T]) for _ in range(2)]
    zus = [None, None]
    for j in range(16):
        if j % 8 == 0:
            zus[j // 8] = load_unit(w_in_d, (j // 8) * 1024, 1024)
        zu = zus[j // 8]
        sz, szb = szs[j % 2]
        sq, sqb = sqs[j % 2]

        def evac(bi, t0, n, cnd, pap, pb, sz=sz, szb=szb):
            V("act", "activation", [pb], [szb], out=sz[:, t0:t0 + n], in_=pap, func=AF.Silu)
        proj_fm([zu], j % 8, hT, hbufs, evac)
        V("dve", "tensor_tensor", yrd + [szb], [yT_b], out=yT[:, j, :], in0=yT[:, j, :], in1=sz, op=ALU.mult)
        V("act", "activation", [yT_b], [sqb], out=sq, in_=yT[:, j, :], func=AF.Square)
        for bi, (t0, n, cnd) in enumerate(BLK):
            V("pe", "matmul", [sqb, cstb_b], [PS(4 + bi)[1]], PS(4 + bi)[0][:, 0:n], lhsT=ONESB,
              rhs=sq[:, t0:t0 + n], start=(j % 2 == 0), stop=(j % 2 == 1))
        if j % 2 == 1:
            rg, rgb = rgs[(j // 2) % 2]
            for bi, (t0, n, cnd) in enumerate(BLK):
                rsqrt_from(rg[:, t0:t0 + n], rgb, PS(4 + bi)[0][:, 0:n], PS(4 + bi)[1], 1.0 / 256)
            for jj in (j - 1, j):
                V("dve", "scalar_tensor_tensor", [yT_b, rgb, par_b], [yT_b], out=yT[:, jj, :], in0=yT[:, jj, :],
                  scalar=pc("ssm_norm_w")[:, jj:jj + 1], in1=rg, op0=ALU.mult, op1=ALU.mult)

    if dbg == "yn":
        dump(yT[:, 0, :], yT_b, 768)
        dump(yT[:, 9, :], yT_b, 768)
        dump(gs[:, 3, :], gs_b, 768)
        dump(gc[:, 7, :], gc_b, 768)
        return end_dbg()

    mT, mT_b = SL2.carve([128, 8, T])
    so1 = load_unit(w_ssd_d, 0, 1024, k0=0)
    so2 = load_unit(w_ssd_d, 0, 1024, k0=8)
    for dd in range(8):
        def evac(bi, t0, n, cnd, pap, pb, dd=dd):
            V("dve", "tensor_tensor", [pb, gs_b], [mT_b], out=mT[:, dd, t0:t0 + n], in0=pap,
              in1=gs[:, dd, t0:t0 + n], op=ALU.mult)
        proj_fm([so1, so2], dd, yT, lambda bi: [yT_b], evac)

    SL1.reset()
    SL3.reset()
    uc, uc_b = SL3.carve([128, 8, T])
    upres = [SL1.carve([128, 948], BF16) for _ in range(2)]
    sg, sg_b = SL1.carve([128, T])
    dg31s = [SL1.carve([128, 31, 128], BF16) for _ in range(2)]
    for (up, upb) in upres:
        V("dve", "memset", [], [upb], up, 0.0)
    au = load_unit(w_in_d, 6208, 1024)
    bu_ = load_unit(w_in_d, 7232, 1024)
    cw_o = PAR["cdw_w"][0]
    cdb_o = PAR["cdw_b"][0]
    for j in range(8):
        up, upb = upres[j % 2]
        upP = up[:, 0:572].rearrange("p (s t) -> p s t", s=2)
        upS = up[:, 572:948].rearrange("p (s t) -> p s t", s=4)
        for bi, (t0, n, cnd) in enumerate(BLK):
            pa, pab = PS(bi)
            pbt, pbb = PS(2 + bi)
            for k in range(8):
                V("pe", "matmul", [au[1]] + hbufs(bi), [pab], pa[:, 0:n], lhsT=au[0][:, k, j * 128:(j + 1) * 128],
                  rhs=hT[:, k, t0:t0 + n], start=(k == 0), stop=(k == 7))
            for k in range(8):
                V("pe", "matmul", [bu_[1]] + hbufs(bi), [pbb], pbt[:, 0:n],
                  lhsT=bu_[0][:, k, j * 128:(j + 1) * 128], rhs=hT[:, k, t0:t0 + n], start=(k == 0), stop=(k == 7))
            V("act", "activation", [pbb], [sg_b], out=sg[:, t0:t0 + n], in_=pbt[:, 0:n], func=AF.Sigmoid)
            if bi == 0:
                V("dve", "tensor_tensor", [pab, sg_b], [upb], out=upP[:, :, 15:271],
                  in0=pa[:, 0:512].rearrange("p (s t) -> p s t", s=2),
                  in1=sg[:, 0:512].rearrange("p (s t) -> p s t", s=2), op=ALU.mult)
            else:
                V("dve", "tensor_tensor", [pab, sg_b], [upb], out=upS[:, :, 15:79],
                  in0=pa[:, 0:256].rearrange("p (s t) -> p s t", s=4),
                  in1=sg[:, 512:768].rearrange("p (s t) -> p s t", s=4), op=ALU.mult)
        dg, dgb = dg31s[j % 2]
        dgks = [Buf("dg31_%d_%d" % (j, k)) for k in range(31)]
        for k in range(31):
            dgks[k].t.rd = dict(dgb.t.rd)
            if dgb.t.lw is not None:
                dgks[k].t.rd[("lw", id(dgb))] = dgb.t.lw
            SL1.tiles.append(dgks[k])
            wc_ = par[:, cw_o + j * 31 + k:cw_o + j * 31 + k + 1]
            if k % 2 == 0:
                V("dve", "tensor_scalar", [cstb_b, par_b], [dgks[k]], out=dg[:, k, :], in0=IDENTB,
                  scalar1=wc_, scalar2=None, op0=ALU.mult)
            else:
                V("act", "activation", [cstb_b, par_b], [dgks[k]], out=dg[:, k, :], in_=IDENTB, func=AF.Copy,
                  scale=wc_)
        for bi, (t0, n, cnd) in enumerate(BLK):
            pt, pb = PS(4 + 2 * (j % 2) + bi)
            for k in range(31):
                rhs = upP[:, :, k:k + 256] if bi == 0 else upS[:, :, k:k + 64]
                V("pe", "matmul", [upb, dgks[k]], [pb], pt[:, 0:n], lhsT=dg[:, k, :], rhs=rhs,
                  start=(k == 0), stop=(k == 30))
                dgb.t.rd["pe"] = dgks[k].t.rd.get("pe", dgb.t.rd.get("pe"))
            V("act", "activation", [pb, par_b], [uc_b], out=uc[:, j, t0:t0 + n], in_=pt[:, 0:n], func=AF.Identity,
              bias=par[:, cdb_o + j:cdb_o + j + 1], scale=1.0)

    SL1.reset()
    un, un_b = SL1.carve([128, 8, T], BF16)
    lnr, lnr_b = SL1.carve([128, T])
    t1s = [SL1.carve([128, T]) for _ in range(2)]
    ucb_, ucbb = SLT.carve([128, T], BF16)
    sq2, sq2b = SLT.carve([128, T], BF16)
    mu, mu_b = SLT.carve([128, T])
    for j in range(8):
        V("act", "activation", [uc_b], [ucbb], out=ucb_, in_=uc[:, j, :], func=AF.Copy)
        V("act", "activation", [uc_b], [sq2b], out=sq2, in_=uc[:, j, :], func=AF.Square)
        for bi, (t0, n, cnd) in enumerate(BLK):
            V("pe", "matmul", [ucbb, cstb_b], [PS(4 + bi)[1]], PS(4 + bi)[0][:, 0:n], lhsT=ONESB,
              rhs=ucb_[:, t0:t0 + n], start=(j == 0), stop=(j == 7))
            V("pe", "matmul", [sq2b, cstb_b], [PS(6 + bi)[1]], PS(6 + bi)[0][:, 0:n], lhsT=ONESB,
              rhs=sq2[:, t0:t0 + n], start=(j == 0), stop=(j == 7))
    for bi, (t0, n, cnd) in enumerate(BLK):
        V("act", "activation", [PS(4 + bi)[1]], [mu_b], out=mu[:, t0:t0 + n], in_=PS(4 + bi)[0][:, 0:n],
          func=AF.Copy, scale=1.0 / 1024)
    V("dve", "tensor_tensor", [mu_b], [lnr_b], out=lnr, in0=mu, in1=mu, op=ALU.mult)
    for bi, (t0, n, cnd) in enumerate(BLK):
        V("dve", "scalar_tensor_tensor", [PS(6 + bi)[1], lnr_b], [lnr_b], out=lnr[:, t0:t0 + n],
          in0=PS(6 + bi)[0][:, 0:n], scalar=1.0 / 1024, in1=lnr[:, t0:t0 + n], op0=ALU.mult, op1=ALU.subtract)
    rsqrt_from(lnr, lnr_b, lnr, lnr_b, 1.0)
    for j in range(8):
        t1, t1b = t1s[j % 2]
        V("dve", "tensor_tensor", [uc_b, mu_b], [t1b], out=t1, in0=uc[:, j, :], in1=mu, op=ALU.subtract)
        V("dve", "tensor_tensor", [t1b, lnr_b], [t1b], out=t1, in0=t1, in1=lnr, op=ALU.mult)
        V("act", "activation", [t1b, par_b], [un_b], out=un[:, j, :], in_=t1, func=AF.Silu,
          scale=pc("ln_w")[:, j:j + 1], bias=pc("ln_b")[:, j:j + 1])
    cuo = load_unit(w_conf_d, 0, 1024)
    for dd in range(8):
        t1, t1b = t1s[dd % 2]

        def evac(bi, t0, n, cnd, pap, pb, dd=dd, t1=t1, t1b=t1b):
            V("dve", "scalar_tensor_tensor", [pb, gc_b, par_b], [t1b], out=t1[:, t0:t0 + n], in0=pap,
              scalar=pc("b_conf_out")[:, dd:dd + 1], in1=gc[:, dd, t0:t0 + n], op0=ALU.add, op1=ALU.mult)
            V("dve", "tensor_tensor", [t1b, mT_b], [mT_b], out=mT[:, dd, t0:t0 + n], in0=mT[:, dd, t0:t0 + n],
              in1=t1[:, t0:t0 + n], op=ALU.add)
        proj_fm([cuo], dd, un, lambda bi: [un_b], evac)

    SLT.reset()
    mb, mb_b = SLT.carve([128, 8, T], BF16)
    xts = [SLT.carve([128, 1024], F32, dma=True) for _ in range(2)]
    V("act", "activation", [mT_b], [mb_b], out=mb, in_=mT, func=AF.Copy)
    SL3.reset()
    x1T, x1T_b = SL3.carve([128, 8, T])
    for i in range(6):
        xtt, xtb = xts[i % 2]
        dma_in(xtt, xtb, x_all[i * 128:(i + 1) * 128, :])

        def evac(j, pap, pb, i=i):
            eng = "dve" if j < 4 else "act"
            if eng == "dve":
                V("dve", "tensor_copy", [pb], [x1T_b], out=x1T[:, j, i * 128:(i + 1) * 128], in_=pap)
            else:
                V("act", "activation", [pb], [x1T_b], out=x1T[:, j, i * 128:(i + 1) * 128], in_=pap, func=AF.Copy)
        transpose_x_tile(xtt, xtb, evac)
    wou = load_unit(w_o_d, 0, 1024)
    for dd in range(8):
        def evac(bi, t0, n, cnd, pap, pb, dd=dd):
            V("dve", "scalar_tensor_tensor", [pb, x1T_b, mod_b], [x1T_b], out=x1T[:, dd, t0:t0 + n], in0=pap,
              scalar=G1(dd, cnd), in1=x1T[:, dd, t0:t0 + n], op0=ALU.mult, op1=ALU.add)
        proj_fm([wou], dd, mb, lambda bi: [mb_b], evac)

    if dbg == "mix":
        for j in (0, 3, 7):
            dump(x1T[:, j, :], x1T_b, 768)
        return end_dbg()

    SL0.reset(); SL1.reset(); SL2.reset(); SLT.reset()
    acc, acc_b = SL0.carve([128, 8, T])
    acts = [SL2.carve([128, 8, T], BF16) for _ in range(2)]
    h2T, h2T_b = SLT.carve([128, 8, T], BF16)
    combT, combT_b = SLT.carve([128, T], BF16)
    rb, rb_b = SLT.carve([128, T])
    sqm, sqm_b = SLT.carve([128, T], BF16)
    lg, lg_b = SLT.carve([128, 6, 32])
    ex, ex_b = SLT.carve([128, 6, 32])
    mk, mk_b = SLT.carve([128, 6, 32])
    comb, comb_b = SLT.carve([128, 6, 32])
    cma_hi, cma_hi_b = SLT.carve([128, 6, 32], BF16)
    cma_lo, cma_lo_b = SLT.carve([128, 6, 32], BF16)
    m8, m8_b = SLT.carve([128, 6, 8])
    sm, sm_b = SLT.carve([128, 6])
    bdn, bdn_b = SLT.carve([128, 1024], BF16, dma=True)
    wrt, wrt_b = SLT.carve([128, 8, 32], F32, dma=True)
    wrt_hi, wrt_hi_b = SLT.carve([128, 8, 32], BF16)
    wrt_lo, wrt_lo_b = SLT.carve([128, 8, 32], BF16)
    h2lo, h2lo_b = SL1.carve([128, 8, T], BF16)
    h2fs = [SL1.carve([128, T]) for _ in range(2)]

    def rms_bc(src, src_b, scale):
        for j in range(8):
            V("act", "activation", [src_b], [sqm_b], out=sqm, in_=src[:, j, :], func=AF.Square)
            for bi, (t0, n, cnd) in enumerate(BLK):
                V("pe", "matmul", [sqm_b, cstb_b], [PS(4 + bi)[1]], PS(4 + bi)[0][:, 0:n], lhsT=ONESB,
                  rhs=sqm[:, t0:t0 + n], start=(j == 0), stop=(j == 7))
        for bi, (t0, n, cnd) in enumerate(BLK):
            rsqrt_from(rb[:, t0:t0 + n], rb_b, PS(4 + bi)[0][:, 0:n], PS(4 + bi)[1], scale)

    rms_bc(x1T, x1T_b, 1.0 / 1024)
    P.op("pool", lambda E: E.dma_start(out=bdn[0:32, :], in_=b_dn_d), wr=[bdn_b], dma=bdn_b)
    dma_in(wrt, wrt_b, w_rt_d.rearrange("p (k e) -> p k e", k=8))
    V("dve", "tensor_copy", [wrt_b], [wrt_hi_b], out=wrt_hi, in_=wrt)
    V("dve", "tensor_tensor", [wrt_b, wrt_hi_b], [wrt_lo_b], out=wrt_lo, in0=wrt, in1=wrt_hi, op=ALU.subtract)
    for j in range(8):
        h2f, h2fb = h2fs[j % 2]
        for bi, (t0, n, cnd) in enumerate(BLK):
            V("dve", "scalar_tensor_tensor", [x1T_b, rb_b, modA_b], [h2fb], out=h2f[:, t0:t0 + n],
              in0=x1T[:, j, t0:t0 + n], scalar=A2(j, cnd), in1=rb[:, t0:t0 + n], op0=ALU.mult, op1=ALU.mult)
            V("act", "activation", [h2fb, mod_b], [h2fb], out=h2f[:, t0:t0 + n], in_=h2f[:, t0:t0 + n],
              func=AF.Identity, bias=S2(j, cnd), scale=1.0)
        V("dve", "tensor_copy", [h2fb], [h2T_b], out=h2T[:, j, :], in_=h2f)
        V("dve", "tensor_tensor", [h2fb, h2T_b], [h2lo_b], out=h2lo[:, j, :], in0=h2f, in1=h2T[:, j, :],
          op=ALU.subtract)
    pt0, pb0 = PS(0)
    for i in range(6):
        n_mm = 0
        for k in range(8):
            for (lt_, ltb, rt_, rtb) in ((h2T, h2T_b, wrt_hi, wrt_hi_b), (h2T, h2T_b, wrt_lo, wrt_lo_b),
                                         (h2lo, h2lo_b, wrt_hi, wrt_hi_b)):
                V("pe", "matmul", [ltb, rtb], [pb0], pt0[:, i * 32:(i + 1) * 32],
                  lhsT=lt_[:, k, i * 128:(i + 1) * 128], rhs=rt_[:, k, :], start=(n_mm == 0), stop=(n_mm == 23))
                n_mm += 1
    V("dve", "tensor_tensor", [pb0, par_b], [lg_b], out=lg, in0=pt0[:, 0:192].rearrange("p (i e) -> p i e", i=6),
      in1=pc("b_router").unsqueeze(1).to_broadcast([128, 6, 32]), op=ALU.add)
    for i in range(6):
        V("dve", "max", [lg_b], [m8_b], out=m8[:, i, :], in_=lg[:, i, :])
    V("dve", "tensor_tensor", [lg_b, m8_b], [mk_b], out=mk, in0=lg, in1=m8[:, :, 3:4].to_broadcast([128, 6, 32]),
      op=ALU.is_ge)
    V("dve", "tensor_tensor", [lg_b, m8_b], [ex_b], out=ex, in0=lg, in1=m8[:, :, 0:1].to_broadcast([128, 6, 32]),
      op=ALU.subtract)
    V("act", "activation", [ex_b], [ex_b], out=ex, in_=ex, func=AF.Exp)
    V("dve", "tensor_tensor", [ex_b, mk_b], [ex_b], out=ex, in0=ex, in1=mk, op=ALU.mult)
    V("dve", "tensor_reduce", [ex_b], [sm_b], out=sm, in_=ex, axis=AX.X, op=ALU.add)
    V("dve", "reciprocal", [sm_b], [sm_b], out=sm, in_=sm)
    V("dve", "tensor_tensor", [ex_b, sm_b], [comb_b], out=comb, in0=ex,
      in1=sm.unsqueeze(2).to_broadcast([128, 6, 32]), op=ALU.mult)
    V("dve", "tensor_scalar", [comb_b], [ex_b], out=ex, in0=comb, scalar1=1.0 / SW_ALPHA, scalar2=None, op0=ALU.mult)
    V("dve", "tensor_copy", [ex_b], [cma_hi_b], out=cma_hi, in_=ex)
    V("dve", "tensor_tensor", [ex_b, cma_hi_b], [cma_lo_b], out=cma_lo, in0=ex, in1=cma_hi, op=ALU.subtract)
    for i in range(6):
        ptc, pbc = PS(1 + i // 4)
        V("pe", "transpose", [comb_b, cst_b], [pbc], out=ptc[0:32, (i % 4) * 128:(i % 4 + 1) * 128],
          in_=comb[:, i, :], identity=IDENT)
    V("act", "activation", [PS(1)[1]], [combT_b], out=combT[0:32, 0:512], in_=PS(1)[0][0:32, 0:512], func=AF.Copy)
    V("act", "activation", [PS(2)[1]], [combT_b], out=combT[0:32, 512:768], in_=PS(2)[0][0:32, 0:256], func=AF.Copy)
    for dd in range(8):
        for bi, (t0, n, cnd) in enumerate(BLK):
            pt, pb = PS(4 + (dd * 2 + bi) % 2)
            V("pe", "matmul", [bdn_b, combT_b], [pb], pt[:, 0:n], lhsT=bdn[0:32, dd * 128:(dd + 1) * 128],
              rhs=combT[0:32, t0:t0 + n], start=True, stop=True)
            V("act", "activation", [pb], [acc_b], out=acc[:, dd, t0:t0 + n], in_=pt[:, 0:n], func=AF.Copy)

    if dbg == "router":
        dump(comb.rearrange("p a b -> p (a b)"), comb_b, 192)
        dump(acc[:, 2, :], acc_b, 768)
        dump(h2T[:, 5, :], h2T_b, 768)
        return end_dbg()

    SL1.reset()
    gsbs = [SL1.carve([128, T]) for _ in range(2)]
    tts = [SL1.carve([128, T]) for _ in range(2)]
    ubs = [SL1.carve([128, T]) for _ in range(2)]
    cmbs = [SL1.carve([128, T]) for _ in range(2)]
    bgu_o = PAR["b_gu"][0]

    def make_cmb(e):
        cm_t, cm_b = cmbs[e % 2]
        for i in range(6):
            pt, pb = PS(6 + i // 4)
            dst = pt[:, (i % 4) * 128:(i % 4 + 1) * 128]
            V("pe", "matmul", [cma_hi_b, cstb_b], [pb], dst, lhsT=cma_hi[:, i, e:e + 1].to_broadcast([128, 128]),
              rhs=IDENTB, start=True, stop=False)
            V("pe", "matmul", [cma_lo_b, cstb_b], [pb], dst, lhsT=cma_lo[:, i, e:e + 1].to_broadcast([128, 128]),
              rhs=IDENTB, start=False, stop=True)
        V("act", "activation", [PS(6)[1]], [cm_b], out=cm_t[:, 0:512], in_=PS(6)[0][:, 0:512], func=AF.Copy)
        V("act", "activation", [PS(7)[1]], [cm_b], out=cm_t[:, 512:768], in_=PS(7)[0][:, 0:256], func=AF.Copy)
        return cm_t, cm_b

    jcount = [0]

    def expert_gu(e, jh, U, act, cm, down_iter=None):
        ut, ub_ = U
        act_t, act_b = act
        cm_t, cm_b = cm
        for jj in range(4):
            j = jh * 4 + jj
            s_ = jcount[0] % 2
            jcount[0] += 1
            gA, gAb = PS(3 * s_)
            uA, uAb = PS(3 * s_ + 1)
            gB, gBb = PS(3 * s_ + 2)
            if down_iter is not None and jj > 0:
                for _ in range(2):
                    next(down_iter, None)
            for k in range(8):
                w_ = ut[:, k, jj * 128:(jj + 1) * 128]
                V("pe", "matmul", [ub_, h2T_b], [gAb], gA[:, 0:512], lhsT=w_, rhs=h2T[:, k, 0:512],
                  start=(k == 0), stop=(k == 7))
                V("pe", "matmul", [ub_, h2T_b], [gBb], gB[:, 0:256], lhsT=w_, rhs=h2T[:, k, 512:768],
                  start=(k == 0), stop=(k == 7))
            for k in range(8):
                w_ = ut[:, k, 512 + jj * 128:512 + (jj + 1) * 128]
                V("pe", "matmul", [ub_, h2T_b], [uAb], uA[:, 0:512], lhsT=w_, rhs=h2T[:, k, 0:512],
                  start=(k == 0), stop=(k == 7))
                V("pe", "matmul", [ub_, h2T_b], [gBb], gB[:, 256:512], lhsT=w_, rhs=h2T[:, k, 512:768],
                  start=(k == 0), stop=(k == 7))
            bg = par[:, bgu_o + e * 16 + j:bgu_o + e * 16 + j + 1]
            bu = par[:, bgu_o + e * 16 + 8 + j:bgu_o + e * 16 + 8 + j + 1]
            gsb, gsbb = gsbs[s_]
            tt, ttb = tts[s_]
            ubt, ubb = ubs[s_]
            V("dve", "tensor_scalar", [gAb, par_b], [gsbb], out=gsb[:, 0:512], in0=gA[:, 0:512], scalar1=bg,
              scalar2=SW_LIM, op0=ALU.add, op1=ALU.min)
            V("dve", "tensor_scalar", [gBb, par_b], [gsbb], out=gsb[:, 512:768], in0=gB[:, 0:256], scalar1=bg,
              scalar2=SW_LIM, op0=ALU.add, op1=ALU.min)
            V("act", "activation", [uAb, par_b], [ubb], out=ubt[:, 0:512], in_=uA[:, 0:512], func=AF.Identity,
              bias=bu, scale=1.0)
            V("dve", "tensor_scalar", [gBb, par_b], [ubb], out=ubt[:, 512:768], in0=gB[:, 256:512], scalar1=bu,
              scalar2=None, op0=ALU.add)
            V("act", "activation", [gsbb], [ttb], out=tt, in_=gsb, func=AF.Silu, scale=SW_ALPHA)
            if down_iter is not None:
                for _ in range(4 if jj == 0 else 2):
                    next(down_iter, None)
            V("dve", "tensor_scalar", [ubb], [ubb], out=ubt, in0=ubt, scalar1=SW_LIM,
              scalar2=-SW_LIM, op0=ALU.min, op1=ALU.max)
            V("dve", "scalar_tensor_tensor", [ubb, ttb], [ttb], out=tt, in0=ubt, scalar=1.0, in1=tt,
              op0=ALU.add, op1=ALU.mult)
            V("dve", "tensor_tensor", [ttb, cm_b], [act_b], out=act_t[:, j, :], in0=tt, in1=cm_t, op=ALU.mult)

    dcount = [0]

    accb = [[Buf("acc_%d_%d" % (dd, bi)) for bi in range(2)] for dd in range(8)]
    for dd in range(8):
        for bi in range(2):
            accb[dd][bi].t.lw = acc_b.t.lw
            accb[dd][bi].t.rd = dict(acc_b.t.rd)
            SL0.tiles.append(accb[dd][bi])

    def expert_down_gen(UD, act):
        ut, ub_ = UD
        act_t, act_b = act
        for bi, (t0, n, cnd) in enumerate(BLK):
            for dd in range(8):
                pt, pb = PS(6 + dcount[0] % 2)
                dcount[0] += 1
                for k in range(8):
                    V("pe", "matmul", [ub_, act_b], [pb], pt[:, 0:n], lhsT=ut[:, k, dd * 128:(dd + 1) * 128],
                      rhs=act_t[:, k, t0:t0 + n], start=(k == 0), stop=(k == 7))
                V("dve", "tensor_tensor", [pb, accb[dd][bi]], [accb[dd][bi]], out=acc[:, dd, t0:t0 + n],
                  in0=pt[:, 0:n], in1=acc[:, dd, t0:t0 + n], op=ALU.add)
                yield

    def expert_down(UD, act):
        for _ in expert_down_gen(UD, act):
            pass

    n_exp = N_EXP
    if dbg and dbg.startswith("moe"):
        n_exp = int(dbg[3:])
    UA = load_unit2(w_gu_d[0], 0, 1024)
    UB = load_unit2(w_gu_d[0], 512, 1536)
    UD = load_unit(w_dn_d[0], 0, 1024)
    prev = None
    for e in range(n_exp):
        cm = make_cmb(e)
        act = acts[e % 2]
        dit = expert_down_gen(*prev) if prev is not None else None
        expert_gu(e, 0, UA, act, cm, dit)
        if dit is not None:
            for _ in dit:
                pass
        if e + 1 < n_exp:
            UA_n = load_unit2(w_gu_d[e + 1], 0, 1024)
            UB_n = load_unit2(w_gu_d[e + 1], 512, 1536)
        expert_gu(e, 1, UB, act, cm)
        prev = (UD, act)
        if e + 1 < n_exp:
            UD = load_unit(w_dn_d[e + 1], 0, 1024)
            UA, UB = UA_n, UB_n
    expert_down(*prev)

    for j in range(8):
        for bi, (t0, n, cnd) in enumerate(BLK):
            V("dve", "scalar_tensor_tensor", [accb[j][bi], x1T_b, mod_b], [x1T_b], out=x1T[:, j, t0:t0 + n],
              in0=acc[:, j, t0:t0 + n], scalar=G2(j, cnd), in1=x1T[:, j, t0:t0 + n], op0=ALU.mult, op1=ALU.add)
    rms_bc(x1T, x1T_b, 1.0 / 1024)
    for j in range(8):
        V("dve", "scalar_tensor_tensor", [x1T_b, rb_b, par_b], [x1T_b], out=x1T[:, j, :], in0=x1T[:, j, :],
          scalar=pc("norm_final")[:, j:j + 1], in1=rb, op0=ALU.mult, op1=ALU.mult)
    SL2.reset()
    ystg = [SL2.carve([128, 1024], F32, dma=True) for _ in range(2)]
    for i in range(6):
        ys, ysb = ystg[i % 2]
        for hb in range(2):
            pt, pb = PS(1 + hb)
            for jj in range(4):
                j = hb * 4 + jj
                V("pe", "transpose", [x1T_b, cst_b], [pb], out=pt[:, jj * 128:(jj + 1) * 128],
                  in_=x1T[:, j, i * 128:(i + 1) * 128], identity=IDENT)
            if hb == 0:
                V("act", "activation", [pb], [ysb], out=ys[:, 0:512], in_=pt[:, :], func=AF.Copy)
            else:
                V("dve", "tensor_copy", [pb], [ysb], out=ys[:, 512:1024], in_=pt[:, :])
        dst = y_out[i * 128:(i + 1) * 128, :]
        P.op("sp", (lambda E, dst=dst, ys=ys: E.dma_start(out=dst, in_=ys)), rd=[ysb], dma=ysb)
        if ysb not in out_bufs:
            out_bufs.append(ysb)
    P.op("sp", None, wr=out_bufs)
    return finish(nc, P, es, sems)


def finish(nc, P, es, sems):
    P.finalize()
    with nc.Block() as block:
        @block.tensor
        def _(E):
            P.emit("pe", E, sems)

        @block.scalar
        def _(E):
            P.emit("act", E, sems)

        @block.vector
        def _(E):
            P.emit("dve", E, sems)

        @block.gpsimd
        def _(E):
            P.emit("pool", E, sems)

        @block.sync
        def _(E):
            P.emit("sp", E, sems)
    es.close()
    return nc


def core_inputs(inp, core, shared):
    b, q = core // 4, core % 4
    xp = np.asarray(inp["x_prompt"], np.float32)
    xs = np.asarray(inp["x_sample"], np.float32)
    xrot = np.roll(xs[b], -256 * q, axis=0)
    x_all = np.ascontiguousarray(np.concatenate([xp[2 * core], xp[2 * core + 1], xrot], axis=0))
    cond = np.stack([_fm(inp["c_ctx"], 8), _fm(np.asarray(inp["c"])[b], 8)], axis=2).reshape(128, 16)
    st0 = np.ascontiguousarray(np.asarray(inp["state_ssm"], np.float32)[b, 0].reshape(4096, 128))
    m = np.zeros((20,), np.float32)
    m[0:8] = 1.0
    m[(8 - 2 * q) % 8] = 0.0
    for s in range(2, 8):
        orig = (s + 2 * q) % 8
        m[8 + (s - 2)] = 1.0 if orig < 2 * q else 0.0
        m[14 + (s - 2)] = 0.0 if orig < 2 * q else 1.0
    d = dict(shared)
    d.update({"x_all": x_all, "cond_fm": np.ascontiguousarray(cond),
              "state0": st0, "masks": np.ascontiguousarray(np.broadcast_to(m[None, :], (128, 20)))})
    return d


def shared_inputs(inp):
    f = lambda a: np.ascontiguousarray(np.asarray(a, np.float32))
    wr = f(inp["w_router"])[0]
    return {
        "params": pack_params(inp), "consts": make_consts(),
        "w_ada": f(inp["w_ada"])[0], "w_in": f(inp["w_in"])[0], "w_ssd_out": f(inp["w_ssd_out"])[0],
        "w_conf_out": f(inp["w_conf_out"])[0], "w_o": f(inp["w_o"])[0],
        "w_router": np.ascontiguousarray(wr.reshape(8, 128, 32).transpose(1, 0, 2).reshape(128, 256)),
        "w_gu": f(inp["w_gu"])[0], "w_down": f(inp["w_down"])[0], "b_down": f(inp["b_down"])[0],
    }


def kernel(**inp):
    nc = build()
    shared = shared_inputs(inp)
    in_maps = [core_inputs(inp, c, shared) for c in range(8)]
    res = run_bass_kernel_spmd(nc, in_maps, core_ids=list(range(8)))
    y_prompt = np.zeros((16, 256, 1024), np.float32)
    y_sample = np.zeros((2, 1024, 1024), np.float32)
    new_state = np.zeros((16, 1, 2, 32, 64, 128), np.float32)
    for c in range(8):
        r = res.results[c]
        b, q = c // 4, c % 4
        y = r["y_own"]
        y_prompt[2 * c] = y[0:256]
        y_prompt[2 * c + 1] = y[256:512]
        y_sample[b, 256 * q:256 * q + 256] = y[512:768]
        ns = r["new_state"].reshape(2, 2, 32, 64, 128)
        new_state[2 * c, 0] = ns[0]
        new_state[2 * c + 1, 0] = ns[1]
    return (y_prompt, y_sample, new_state)
```
